# Optimizing a Trainium2 kernel written in Bass

```python
import math
import jax
import jax.numpy as jnp
from jax import lax
import numpy as np

D_MODEL = 1024
BATCH = 2
SEQ = 8192
DEPTH = 2

GRID_W = 64
CTX_LEN = 256
EPS = 1e-6

HEAD_DIM = 128
ATT_HEADS = 8
ATT_KV_HEADS = 2
GQA_REP = ATT_HEADS // ATT_KV_HEADS
ATT_WIDTH = ATT_HEADS * HEAD_DIM
KV_WIDTH = ATT_KV_HEADS * HEAD_DIM
ATT_SCALE = HEAD_DIM ** -0.5
ROPE_THETA = 10000.0
Q_BLOCK = 128
WINDOW = 128

SSD_HEADS = 16
SSD_HEAD_DIM = 64
SSD_WIDTH = SSD_HEADS * SSD_HEAD_DIM
SSD_GROUPS = 2
SSD_STATE = 64
SSD_CHUNK = 128
SSD_CONV_CH = SSD_WIDTH + 2 * SSD_GROUPS * SSD_STATE

LRU_WIDTH = 1024
LRU_BLOCKS = 16
LRU_BLOCK_DIM = LRU_WIDTH // LRU_BLOCKS
LRU_C = 8.0

CONV_W = 5

MIX_WIDTH = SSD_WIDTH + ATT_WIDTH
AB_SPLITS = (SSD_WIDTH,
             SSD_WIDTH + SSD_CONV_CH,
             SSD_WIDTH + SSD_CONV_CH + 2 * SSD_HEADS,
             SSD_WIDTH + SSD_CONV_CH + 2 * SSD_HEADS + ATT_WIDTH,
             SSD_WIDTH + SSD_CONV_CH + 2 * SSD_HEADS + ATT_WIDTH + KV_WIDTH)
AB_IN = AB_SPLITS[-1] + KV_WIDTH
CD_SPLITS = (LRU_WIDTH, 2 * LRU_WIDTH, 2 * LRU_WIDTH + ATT_WIDTH, 2 * LRU_WIDTH + ATT_WIDTH + KV_WIDTH)
CD_IN = CD_SPLITS[-1] + KV_WIDTH

MOE_GROUPS = 4
EXPERTS_PER_GROUP = 4
N_EXPERTS = MOE_GROUPS * EXPERTS_PER_GROUP
TOP_K = 2
D_EXPERT = 256

N_EVEN = (DEPTH + 1) // 2
N_ODD = DEPTH // 2

kernel_name = 'hybrid_ssd_gqa_rglru_swa_hmoe_dit'

F32 = jnp.float32


def rmsnorm(x, g):
    xf = x.astype(F32)
    y = xf * lax.rsqrt(jnp.mean(xf * xf, axis=-1, keepdims=True) + EPS)
    return (y * g.astype(F32)).astype(x.dtype)


def dwconv_centred(x, w, b):
    pad = CONV_W // 2
    y = lax.conv_general_dilated(x, w[:, None, :].astype(x.dtype), window_strides=(1,),
                                 padding=[(pad, pad)], dimension_numbers=('NWC', 'WIO', 'NWC'),
                                 feature_group_count=x.shape[-1])
    return y + b


def axial_rope(L):
    rows = L // GRID_W
    row = jnp.repeat(jnp.arange(rows), GRID_W).astype(F32)
    col = jnp.tile(jnp.arange(GRID_W), rows).astype(F32)
    n_freq = HEAD_DIM // 4
    inv = ROPE_THETA ** (-jnp.arange(n_freq, dtype=F32) / n_freq)
    ang = jnp.concatenate([row[:, None] * inv, col[:, None] * inv], axis=-1)
    return jnp.cos(ang), jnp.sin(ang)


def apply_rope(t, cos, sin):
    t1, t2 = jnp.split(t, 2, axis=-1)
    c = cos[None, :, None, :].astype(t.dtype)
    s = sin[None, :, None, :].astype(t.dtype)
    return jnp.concatenate([t1 * c - t2 * s, t1 * s + t2 * c], axis=-1)


def attend_full(q, k, v, sink=None):
    Bsz, Lq = q.shape[:2]
    qg = q.reshape(Bsz, Lq, ATT_KV_HEADS, GQA_REP, HEAD_DIM)
    s = jnp.einsum('bqgrd,bkgd->bgrqk', qg, k, preferred_element_type=F32) * ATT_SCALE
    if sink is not None:
        col = sink.astype(F32).reshape(ATT_KV_HEADS, GQA_REP)[None, :, :, None, None]
        s = jnp.concatenate([s, jnp.broadcast_to(col, s.shape[:-1] + (1,))], axis=-1)
    p = jax.nn.softmax(s, axis=-1)
    if sink is not None:
        p = p[..., :-1]
    o = jnp.einsum('bgrqk,bkgd->bqgrd', p.astype(v.dtype), v)
    return o.reshape(Bsz, Lq, ATT_WIDTH)


def attend_blocked(q, k, v, kc, vc):
    Bsz, L = q.shape[:2]
    nb = L // Q_BLOCK
    keys = jnp.concatenate([kc, k], axis=1)
    vals = jnp.concatenate([vc, v], axis=1)
    qb = jnp.moveaxis(q.reshape(Bsz, nb, Q_BLOCK, ATT_HEADS, HEAD_DIM), 1, 0)
    o = lax.map(lambda qi: attend_full(qi, keys, vals), qb)
    return jnp.moveaxis(o, 0, 1).reshape(Bsz, L, ATT_WIDTH)


def attend_window_sink(q, k, v, kc, vc, sink):
    Bsz, L = q.shape[:2]
    nb = L // Q_BLOCK
    Q = Q_BLOCK
    qb = q.reshape(Bsz, nb, Q, ATT_KV_HEADS, GQA_REP, HEAD_DIM)

    def band(t):
        tp = jnp.pad(t, ((0, 0), (Q, Q), (0, 0), (0, 0))).reshape(Bsz, nb + 2, Q, ATT_KV_HEADS, HEAD_DIM)
        return jnp.concatenate([tp[:, :-2], tp[:, 1:-1], tp[:, 2:]], axis=2)

    kb, vb = band(k), band(v)
    qpos = jnp.arange(L).reshape(nb, Q)
    kpos = (jnp.arange(nb)[:, None] - 1) * Q + jnp.arange(3 * Q)[None, :]
    valid = ((jnp.abs(qpos[:, :, None] - kpos[:, None, :]) <= WINDOW)
             & (kpos >= 0)[:, None, :] & (kpos < L)[:, None, :])
    s_loc = jnp.einsum('bnqgrd,bnkgd->bngrqk', qb, kb, preferred_element_type=F32) * ATT_SCALE
    s_loc = jnp.where(valid[None, :, None, None], s_loc, -jnp.inf)
    s_ctx = jnp.einsum('bnqgrd,bkgd->bngrqk', qb, kc, preferred_element_type=F32) * ATT_SCALE
    col = sink.astype(F32).reshape(ATT_KV_HEADS, GQA_REP)[None, None, :, :, None, None]
    s_sink = jnp.broadcast_to(col, s_loc.shape[:-1] + (1,))
    p = jax.nn.softmax(jnp.concatenate([s_loc, s_ctx, s_sink], axis=-1), axis=-1).astype(v.dtype)
    n_ctx = kc.shape[1]
    o = (jnp.einsum('bngrqk,bnkgd->bnqgrd', p[..., :3 * Q], vb)
         + jnp.einsum('bngrqk,bkgd->bnqgrd', p[..., 3 * Q:3 * Q + n_ctx], vc))
    return o.reshape(Bsz, L, ATT_WIDTH)


def ssd_scan(xs, dt, a_neg, bm, cm, h0):
    Bsz, L, H, P = xs.shape
    G, N = bm.shape[2], bm.shape[3]
    R = H // G
    Q = SSD_CHUNK
    nc = L // Q
    x = xs.astype(F32).reshape(Bsz, nc, Q, G, R, P)
    dt = dt.astype(F32).reshape(Bsz, nc, Q, G, R)
    bc = bm.astype(F32).reshape(Bsz, nc, Q, G, N)
    cc = cm.astype(F32).reshape(Bsz, nc, Q, G, N)
    acs = jnp.cumsum(dt * a_neg.reshape(G, R), axis=2)
    tri = jnp.tril(jnp.ones((Q, Q), dtype=bool))
    seg = acs[:, :, :, None] - acs[:, :, None, :]
    decay = jnp.exp(jnp.where(tri[:, :, None, None], seg, -jnp.inf))
    cb = jnp.einsum('bcign,bcjgn->bcijg', cc, bc)
    w = cb[..., None] * decay * dt[:, :, None]
    y_diag = jnp.einsum('bcijgr,bcjgrp->bcigrp', w, x)
    to_end = jnp.exp(acs[:, :, -1:] - acs) * dt
    states = jnp.einsum('bcjgn,bcjgr,bcjgrp->bcgrpn', bc, to_end, x)
    chunk_decay = jnp.exp(acs[:, :, -1])

    def step(h, inp):
        s_c, d_c = inp
        return d_c[..., None, None] * h + s_c, h

    h_last, h_prev = lax.scan(step, h0.astype(F32).reshape(Bsz, G, R, P, N),
                              (jnp.moveaxis(states, 1, 0), jnp.moveaxis(chunk_decay, 1, 0)))
    h_prev = jnp.moveaxis(h_prev, 0, 1)
    y_off = jnp.einsum('bcign,bcgrpn,bcigr->bcigrp', cc, h_prev, jnp.exp(acs))
    y = (y_diag + y_off).reshape(Bsz, L, H, P)
    return y, h_last.reshape(Bsz, H, P, N)


def flip_seq(t):
    return jnp.flip(t, axis=1)


def bidir_ssd(xs, dt, bm, cm, a_neg, h0f, h0b):
    yf, hf = ssd_scan(xs, dt[:, :, 0], a_neg[0], bm, cm, h0f)
    yb, hb = ssd_scan(flip_seq(xs), flip_seq(dt[:, :, 1]), a_neg[1], flip_seq(bm), flip_seq(cm), h0b)
    return yf + flip_seq(yb), hf, hb


def linear_scan(a, b, h0):
    def combine(left, right):
        return left[0] * right[0], right[0] * left[1] + right[1]
    a_cum, b_cum = lax.associative_scan(combine, (a, b), axis=1)
    h = a_cum * h0[:, None] + b_cum
    return h, h[:, -1]


def ssd_attn_mixer(hl, hc, cos, sin, w_in, conv_w, conv_b, dt_bias, a_log, d_skip, norm_g, q_g, k_g, ctx_out):
    a_neg = -jnp.exp(a_log.astype(F32))

    def project(h, rope):
        Bsz, L, _ = h.shape
        z, xbc, dt, q, k, v = jnp.split(h @ w_in, AB_SPLITS, axis=-1)
        xbc = jax.nn.silu(dwconv_centred(xbc, conv_w, conv_b))
        xs, bm, cm = jnp.split(xbc, [SSD_WIDTH, SSD_WIDTH + SSD_GROUPS * SSD_STATE], axis=-1)
        xs = xs.reshape(Bsz, L, SSD_HEADS, SSD_HEAD_DIM)
        bm = bm.reshape(Bsz, L, SSD_GROUPS, SSD_STATE)
        cm = cm.reshape(Bsz, L, SSD_GROUPS, SSD_STATE)
        dt = jax.nn.softplus(dt.reshape(Bsz, L, 2, SSD_HEADS).astype(F32) + dt_bias.astype(F32))
        q = rmsnorm(q.reshape(Bsz, L, ATT_HEADS, HEAD_DIM), q_g)
        k = rmsnorm(k.reshape(Bsz, L, ATT_KV_HEADS, HEAD_DIM), k_g)
        v = v.reshape(Bsz, L, ATT_KV_HEADS, HEAD_DIM)
        if rope:
            q, k = apply_rope(q, cos, sin), apply_rope(k, cos, sin)
        return z, xs, bm, cm, dt, q, k, v

    def ssd_out(y, xs, z):
        Bsz, L = xs.shape[:2]
        y = (y + d_skip.astype(F32)[:, None] * xs.astype(F32)).reshape(Bsz, L, SSD_WIDTH).astype(z.dtype)
        return rmsnorm(y * jax.nn.silu(z), norm_g)

    zc, xsc, bc, cc, dtc, qc, kc, vc = project(hc, False)
    zl, xsl, bl, cl, dtl, ql, kl, vl = project(hl, True)
    h0 = jnp.zeros((hl.shape[0], SSD_HEADS, SSD_HEAD_DIM, SSD_STATE), F32)
    yc_s, hf, hb = bidir_ssd(xsc, dtc, bc, cc, a_neg, h0, h0)
    yl_s, _, _ = bidir_ssd(xsl, dtl, bl, cl, a_neg, hf, hb)
    y_lat = jnp.concatenate([ssd_out(yl_s, xsl, zl), attend_blocked(ql, kl, vl, kc, vc)], axis=-1)
    y_ctx = None
    if ctx_out:
        y_ctx = jnp.concatenate([ssd_out(yc_s, xsc, zc), attend_full(qc, kc, vc)], axis=-1)
    return y_lat, y_ctx


def lru_swa_mixer(hl, hc, cos, sin, w_in, conv_w, conv_b, w_a, b_a, w_x, b_x, lam, sink, ctx_out):
    log_base = -LRU_C * jax.nn.softplus(-lam.astype(F32))

    def project(h, rope):
        Bsz, L, _ = h.shape
        gate, xr, q, k, v = jnp.split(h @ w_in, CD_SPLITS, axis=-1)
        xr = dwconv_centred(xr, conv_w, conv_b)
        q = q.reshape(Bsz, L, ATT_HEADS, HEAD_DIM)
        k = k.reshape(Bsz, L, ATT_KV_HEADS, HEAD_DIM)
        v = v.reshape(Bsz, L, ATT_KV_HEADS, HEAD_DIM)
        if rope:
            q, k = apply_rope(q, cos, sin), apply_rope(k, cos, sin)
        return gate, xr, q, k, v

    def bidir_lru(xr, h0f, h0b):
        Bsz, L, _ = xr.shape
        xf = xr.astype(F32)
        xb = xf.reshape(Bsz, L, LRU_BLOCKS, LRU_BLOCK_DIM)
        r = jax.nn.sigmoid(jnp.einsum('blhi,dhij->dblhj', xb, w_a.astype(F32)).reshape(2, Bsz, L, LRU_WIDTH)
                           + b_a.astype(F32)[:, None, None])
        ig = jax.nn.sigmoid(jnp.einsum('blhi,dhij->dblhj', xb, w_x.astype(F32)).reshape(2, Bsz, L, LRU_WIDTH)
                            + b_x.astype(F32)[:, None, None])
        log_a = r * log_base[:, None, None]
        a = jnp.exp(log_a)
        b = jnp.sqrt(-jnp.expm1(2.0 * log_a)) * (ig * xf)
        hf, lf = linear_scan(a[0], b[0], h0f)
        hb, lb = linear_scan(flip_seq(a[1]), flip_seq(b[1]), h0b)
        return hf + flip_seq(hb), lf, lb

    gc, xrc, qc, kc, vc = project(hc, False)
    gl, xrl, ql, kl, vl = project(hl, True)
    h0 = jnp.zeros((hl.shape[0], LRU_WIDTH), F32)
    yc_r, hf, hb = bidir_lru(xrc, h0, h0)
    yl_r, _, _ = bidir_lru(xrl, hf, hb)
    y_lat = jnp.concatenate([jax.nn.gelu(gl) * yl_r.astype(gl.dtype),
                             attend_window_sink(ql, kl, vl, kc, vc, sink)], axis=-1)
    y_ctx = None
    if ctx_out:
        y_ctx = jnp.concatenate([jax.nn.gelu(gc) * yc_r.astype(gc.dtype),
                                 attend_full(qc, kc, vc, sink)], axis=-1)
    return y_lat, y_ctx


def hier_moe(h, w_grp, b_grp, w_rt, b_rt, w1, w3, w2):
    Bsz, T, _ = h.shape
    gl = jnp.einsum('btd,dg->btg', h, w_grp, preferred_element_type=F32) + b_grp.astype(F32)
    pg = jax.nn.softmax(gl, axis=-1)
    g_idx = jnp.argmax(gl, axis=-1)
    p_grp = jnp.take_along_axis(pg, g_idx[..., None], axis=-1)
    el = (jnp.einsum('btd,de->bte', h, w_rt, preferred_element_type=F32) + b_rt.astype(F32))
    el = el.reshape(Bsz, T, MOE_GROUPS, EXPERTS_PER_GROUP)
    el_g = jnp.take_along_axis(el, g_idx[..., None, None], axis=2)[:, :, 0]
    top_v, top_i = lax.top_k(el_g, TOP_K)
    w_top = jax.nn.softmax(top_v, axis=-1) * p_grp
    eid = g_idx[..., None] * EXPERTS_PER_GROUP + top_i
    gates = jnp.sum(jax.nn.one_hot(eid, N_EXPERTS, dtype=F32) * w_top[..., None], axis=2)
    a = jnp.einsum('btd,edf->btef', h, w1)
    u = jnp.einsum('btd,edf->btef', h, w3)
    hid = jax.nn.silu(a) * u * gates[..., None].astype(h.dtype)
    return jnp.einsum('btef,efd->btd', hid, w2)


def setup_inputs(seed: int = 0) -> dict:
    key = jax.random.key(seed)
    ks = list(jax.random.split(key, 40))
    cnt = [0]

    def nk():
        cnt[0] += 1
        return ks[cnt[0] - 1]

    def nrm(shape, scale):
        return scale * jax.random.normal(nk(), shape, F32)

    def gain(shape):
        return 1.0 + nrm(shape, 0.02)

    D = D_MODEL
    dt0 = jnp.exp(jax.random.uniform(nk(), (N_EVEN, 2, SSD_HEADS), F32, math.log(1e-3), math.log(1e-1)))
    ssd_dt_bias = dt0 + jnp.log(-jnp.expm1(-dt0))
    ssd_a_log = jnp.log(jax.random.uniform(nk(), (N_EVEN, 2, SSD_HEADS), F32, 1.0, 16.0))
    a0 = jax.random.uniform(nk(), (N_ODD, 2, LRU_WIDTH), F32, 0.9, 0.999)
    sg = a0 ** (1.0 / LRU_C)
    lru_lam = jnp.log(sg) - jnp.log1p(-sg)
    return {
        'x': nrm((BATCH, SEQ, D), 1.0),
        'c': nrm((BATCH, D), 1.0),
        'ctx': nrm((BATCH, CTX_LEN, D), 1.0),
        'c_ctx': nrm((D,), 1.0),
        'w_mod': nrm((DEPTH, D, 6 * D), 0.5 * D ** -0.5),
        'b_mod': nrm((DEPTH, 6 * D), 0.01),
        'g_mix': gain((DEPTH, D)),
        'g_ffn': gain((DEPTH, D)),
        'moe_w_grp': nrm((DEPTH, D, MOE_GROUPS), D ** -0.5),
        'moe_b_grp': nrm((DEPTH, MOE_GROUPS), 0.01),
        'moe_w_rt': nrm((DEPTH, D, N_EXPERTS), D ** -0.5),
        'moe_b_rt': nrm((DEPTH, N_EXPERTS), 0.01),
        'moe_w1': nrm((DEPTH, N_EXPERTS, D, D_EXPERT), D ** -0.5),
        'moe_w3': nrm((DEPTH, N_EXPERTS, D, D_EXPERT), D ** -0.5),
        'moe_w2': nrm((DEPTH, N_EXPERTS, D_EXPERT, D), D_EXPERT ** -0.5),
        'ab_w_in': nrm((N_EVEN, D, AB_IN), D ** -0.5),
        'ab_w_out': nrm((N_EVEN, MIX_WIDTH, D), MIX_WIDTH ** -0.5),
        'ssd_conv_w': nrm((N_EVEN, CONV_W, SSD_CONV_CH), CONV_W ** -0.5),
        'ssd_conv_b': nrm((N_EVEN, SSD_CONV_CH), 0.01),
        'ssd_dt_bias': ssd_dt_bias,
        'ssd_a_log': ssd_a_log,
        'ssd_d': gain((N_EVEN, SSD_HEADS)),
        'ssd_norm_g': gain((N_EVEN, SSD_WIDTH)),
        'att_q_g': gain((N_EVEN, HEAD_DIM)),
        'att_k_g': gain((N_EVEN, HEAD_DIM)),
        'cd_w_in': nrm((N_ODD, D, CD_IN), D ** -0.5),
        'cd_w_out': nrm((N_ODD, MIX_WIDTH, D), MIX_WIDTH ** -0.5),
        'lru_conv_w': nrm((N_ODD, CONV_W, LRU_WIDTH), CONV_W ** -0.5),
        'lru_conv_b': nrm((N_ODD, LRU_WIDTH), 0.01),
        'lru_w_a': nrm((N_ODD, 2, LRU_BLOCKS, LRU_BLOCK_DIM, LRU_BLOCK_DIM), LRU_BLOCK_DIM ** -0.5),
        'lru_b_a': nrm((N_ODD, 2, LRU_WIDTH), 0.01),
        'lru_w_x': nrm((N_ODD, 2, LRU_BLOCKS, LRU_BLOCK_DIM, LRU_BLOCK_DIM), LRU_BLOCK_DIM ** -0.5),
        'lru_b_x': nrm((N_ODD, 2, LRU_WIDTH), 0.01),
        'lru_lam': lru_lam,
        'swa_sink': nrm((N_ODD, ATT_HEADS), 0.5),
        'g_final': gain((D,)),
    }


def reference(x, c, ctx, c_ctx, w_mod, b_mod, g_mix, g_ffn, moe_w_grp, moe_b_grp, moe_w_rt, moe_b_rt,
              moe_w1, moe_w3, moe_w2, ab_w_in, ab_w_out, ssd_conv_w, ssd_conv_b, ssd_dt_bias, ssd_a_log,
              ssd_d, ssd_norm_g, att_q_g, att_k_g, cd_w_in, cd_w_out, lru_conv_w, lru_conv_b, lru_w_a,
              lru_b_a, lru_w_x, lru_b_x, lru_lam, swa_sink, g_final):
    L = x.shape[1]
    cos, sin = axial_rope(L)
    c_act = jax.nn.silu(c)[:, None, :]
    cc_act = jax.nn.silu(c_ctx)[None, None, :]
    xc = ctx
    for i in range(DEPTH):
        last = i == DEPTH - 1
        j = i // 2
        sh1, sc1, g1, sh2, sc2, g2 = jnp.split(c_act @ w_mod[i] + b_mod[i], 6, axis=-1)
        ch1, cs1, cg1, ch2, cs2, cg2 = jnp.split(cc_act @ w_mod[i] + b_mod[i], 6, axis=-1)
        hl = rmsnorm(x, g_mix[i]) * (1.0 + sc1) + sh1
        hc = rmsnorm(xc, g_mix[i]) * (1.0 + cs1) + ch1
        if i % 2 == 0:
            yl, yc = ssd_attn_mixer(hl, hc, cos, sin, ab_w_in[j], ssd_conv_w[j], ssd_conv_b[j], ssd_dt_bias[j],
                                    ssd_a_log[j], ssd_d[j], ssd_norm_g[j], att_q_g[j], att_k_g[j], not last)
            w_out = ab_w_out[j]
        else:
            yl, yc = lru_swa_mixer(hl, hc, cos, sin, cd_w_in[j], lru_conv_w[j], lru_conv_b[j], lru_w_a[j],
                                   lru_b_a[j], lru_w_x[j], lru_b_x[j], lru_lam[j], swa_sink[j], not last)
            w_out = cd_w_out[j]
        moe = (moe_w_grp[i], moe_b_grp[i], moe_w_rt[i], moe_b_rt[i], moe_w1[i], moe_w3[i], moe_w2[i])
        x = x + g1 * (yl @ w_out)
        x = x + g2 * hier_moe(rmsnorm(x, g_ffn[i]) * (1.0 + sc2) + sh2, *moe)
        if not last:
            xc = xc + cg1 * (yc @ w_out)
            xc = xc + cg2 * hier_moe(rmsnorm(xc, g_ffn[i]) * (1.0 + cs2) + ch2, *moe)
    return rmsnorm(x, g_final)
```

```python
from contextlib import ExitStack
import numpy as np
import concourse.bass as bass
import concourse.mybir as mybir
from concourse.bass_utils import run_bass_kernel_spmd

F32 = mybir.dt.float32
BF16 = mybir.dt.bfloat16
ALU = mybir.AluOpType
AF = mybir.ActivationFunctionType
AX = mybir.AxisListType

D = 1024
EPS = 1e-6
ATT_SCALE = 128 ** -0.5
GRID_W = 64
BIG = 1.0e9


class _Res:
    __slots__ = ("lw", "rd")

    def __init__(self):
        self.lw = None
        self.rd = {}


class _TL:
    def __init__(self, name, sem, unit):
        self.name = name
        self.sem = sem
        self.unit = unit
        self.cnt = 0
        self.vc = {}
        self.snaps = {}


class AutoSync:
    def __init__(self, nc, n_dma_sems=8):
        self.nc = nc
        self.res = {}
        self.tl = {}
        self._stack = []
        for name in ("pe", "dve", "act", "pool", "sp"):
            cm = nc.semaphore("s_" + name)
            sem = cm.__enter__()
            self._stack.append(cm)
            self.tl[name] = _TL(name, sem, 1)
        self.eng = {"pe": nc.tensor, "dve": nc.vector, "act": nc.scalar, "pool": nc.gpsimd, "sp": nc.sync}
        self.dq = {}
        for q in ("sp", "act", "pool"):
            lst = []
            for i in range(n_dma_sems):
                cm = nc.semaphore("d_%s%d" % (q, i))
                sem = cm.__enter__()
                self._stack.append(cm)
                t = _TL("d_%s%d" % (q, i), sem, 16)
                self.tl[t.name] = t
                lst.append(t)
            self.dq[q] = [lst, 0]
        self.n_wait = 0
        self.n_inst = 0

    def close(self):
        for cm in reversed(self._stack):
            cm.__exit__(None, None, None)

    def _key(self, ap):
        t = ap.tensor
        if type(t).__name__.startswith("DRam"):
            return None
        return t.name

    def _r(self, key):
        r = self.res.get(key)
        if r is None:
            r = self.res[key] = _Res()
        return r

    def _need(self, reads, writes):
        deps = {}

        def add(t, v):
            if deps.get(t, 0) < v:
                deps[t] = v

        for k in reads:
            lw = self._r(k).lw
            if lw:
                add(*lw)
        for k in writes:
            r = self._r(k)
            if r.lw:
                add(*r.lw)
            for t, v in r.rd.items():
                add(t, v)
        return deps

    def _wait(self, E, deps):
        T = self.tl[E]
        for t, v in deps.items():
            if E == "pe" and t == "pe":
                continue
            if T.vc.get(t, 0) >= v:
                continue
            tl = self.tl[t]
            self.eng[E].wait_ge(tl.sem, v * tl.unit)
            self.n_wait += 1
            snap = tl.snaps.get(v)
            if snap:
                for a, b in snap.items():
                    if T.vc.get(a, 0) < b:
                        T.vc[a] = b
            if T.vc.get(t, 0) < v:
                T.vc[t] = v

    def _mark(self, ev, reads, writes):
        t, v = ev
        for k in reads:
            r = self._r(k)
            if r.rd.get(t, 0) < v:
                r.rd[t] = v
        for k in writes:
            r = self._r(k)
            r.lw = ev
            r.rd = {}

    def _classify(self, args, kwargs):
        reads, writes = [], []
        first = True
        for a in args:
            if isinstance(a, bass.AP):
                k = self._key(a)
                if k is not None:
                    (writes if first else reads).append(k)
            first = False
        for n, a in kwargs.items():
            if isinstance(a, bass.AP):
                k = self._key(a)
                if k is not None:
                    (writes if n in ("out", "accum_out") else reads).append(k)
        return reads, writes

    def op(self, E, name, *args, **kwargs):
        reads, writes = self._classify(args, kwargs)
        self._wait(E, self._need(reads, writes))
        inst = getattr(self.eng[E], name)(*args, **kwargs)
        T = self.tl[E]
        T.cnt += 1
        inst.then_inc(T.sem, 1)
        T.snaps[T.cnt] = dict(T.vc)
        self._mark((E, T.cnt), reads, writes)
        self.n_inst += 1
        return inst

    def dma(self, Q, out, in_, **kwargs):
        reads = [k for k in [self._key(in_)] if k is not None]
        writes = [k for k in [self._key(out)] if k is not None]
        self._wait(Q, self._need(reads, writes))
        lst, idx = self.dq[Q]
        tl = lst[idx % len(lst)]
        self.dq[Q][1] = idx + 1
        if tl.cnt > 0:
            self._wait(Q, {tl.name: tl.cnt})
        inst = self.eng[Q].dma_start(out=out, in_=in_, **kwargs)
        tl.cnt += 1
        inst.then_inc(tl.sem, 16)
        tl.snaps[tl.cnt] = dict(self.tl[Q].vc)
        self._mark((tl.name, tl.cnt), reads, writes)
        self.n_inst += 1
        return inst

    def barrier(self):
        now = {t: tl.cnt for t, tl in self.tl.items() if tl.cnt > 0}
        for E in ("sp", "act", "pool", "dve", "pe"):
            self._wait(E, {t: v for t, v in now.items() if t != E})
            if E != "pe" and now.get(E, 0) > 0:
                self._wait(E, {E: now[E]})


class Scope:
    _n = 0

    def __init__(self, nc, tag):
        self.nc = nc
        self.es = ExitStack()
        Scope._n += 1
        self.tag = "%s%d_" % (tag, Scope._n)

    def sb(self, name, shape, dt):
        return self.es.enter_context(self.nc.sbuf_tensor(self.tag + name, list(shape), dt))

    def ps(self, name, shape, dt=F32):
        return self.es.enter_context(self.nc.psum_tensor(self.tag + name, list(shape), dt))

    def close(self):
        self.es.close()


class Rot:
    def __init__(self, items):
        self.items = items
        self.i = 0

    def next(self):
        x = self.items[self.i % len(self.items)]
        self.i += 1
        return x


def rev(ap2, n):
    return bass.AP(ap2.tensor, ap2.offset + (n - 1), [[ap2.ap[0][0], ap2.ap[0][1]], [-1, n]])


def bc(ap, axis, shape):
    return ap.unsqueeze(axis).to_broadcast(list(shape))


CV_SSD_CW = 0
CV_SSD_CB = 50
CV_QG = 60
CV_KG = 61
CV_QGS = 62
CV_KGS = 63
CV_LRU_CW = 64
CV_LRU_CB = 104
CV_BA = 112
CV_BX = 128
CV_LAM = 144
CV_N = 160
RV_GMIX = 0
RV_GFFN = 2048
RV_GFIN = 4096
RV_NORMG = 5120
RV_SSD_D = 6144
RV_DTB = 6160
RV_ALOG = 6192
RV_BROUTE = 6224
RV_SINK = 6264
RV_N = 6272


def build(NL, NCT, dbg=False):
    nc = bass.Bass("TRN2", target_bir_lowering=False)
    NT = NL + NCT
    T = NT * 128
    L = NL * 128
    CTXN = NCT * 128
    W = T + 8
    COL_CTX = 2
    COL_LAT = CTXN + 6

    def din(name, shape, dt=F32):
        return nc.dram_tensor(name, list(shape), dt, kind="ExternalInput").ap()

    def dsc(name, shape, dt):
        return nc.dram_tensor(name, list(shape), dt, kind="ExternalOutput" if dbg else "Internal").ap()

    xin = din("xin", [T, D])
    cv16 = din("cv16", [16, 128])
    w_mod = din("w_mod", [2, D, 6 * D])
    b_mod = din("b_mod", [2, 6 * D])
    rowv = din("rowv", [1, RV_N])
    colv = din("colv", [CV_N, 128])
    cmask = din("cmask", [8, 128, 128])
    cos2 = din("cos2", [128, L])
    sin2 = din("sin2", [128, L])
    w_in = [din("ab_w_in", [D, 3872]), din("cd_w_in", [D, 3584])]
    w_sw = [din("ab_w_sw", [D, 1280]), din("cd_w_sw", [D, 1280])]
    w_out = [din("ab_w_out", [2048, D]), din("cd_w_out", [2048, D])]
    lru_bd = din("lru_bd", [2, 2, 8, 128, 128])
    moe_wr = din("moe_wr", [2, D, 20])
    moe_w1 = din("moe_w1", [2, 16, D, 256])
    moe_w3 = din("moe_w3", [2, 16, D, 256])
    moe_w2 = din("moe_w2", [2, 16, 256, D])
    rflag = din("rflag", [1, 8])
    out = nc.dram_tensor("out", [NL // 4 * 128, D], F32, kind="ExternalOutput").ap()

    XRES = dsc("XRES", [T, D], F32)
    MODV = dsc("MODV", [2, 2, 6 * D], F32)
    XBC = dsc("XBC", [1280, W], F32)
    XS = dsc("XS", [T, D], BF16)
    ZT = dsc("ZT", [T, D], BF16)
    BTOK = dsc("BTOK", [T, 128], BF16)
    BT = dsc("BT", [2, 64, T], BF16)
    CTs = dsc("CT", [2, 64, T], BF16)
    QT = dsc("QT", [8, 128, T], BF16)
    KT = dsc("KT", [2, 128, T + 128], BF16)
    VT = dsc("VT", [T + 128, 256], BF16)
    NLQ = NL // 4
    LQ = NLQ * 128
    QTq = dsc("QTq", [8, 128, LQ], BF16)
    KTq = dsc("KTq", [2, 128, LQ + 256], BF16)
    VTq = dsc("VTq", [LQ + 256, 256], BF16)
    YTL = dsc("YTL", [16, 128, LQ], BF16)
    Xq = dsc("Xq", [LQ, D], F32)
    XQ1 = dsc("XQ1", [LQ, D], F32)
    HTFq = dsc("HTFq", [8, 128, LQ], BF16)
    YF = dsc("YF", [T, D], F32)
    YT = dsc("YT", [16, 128, T], BF16)
    HTF = dsc("HTF", [8, 128, T], BF16)
    GATE = dsc("GATE", [8, 128, T], BF16)
    W1B = dsc("W1B", [2, 16, 128, 2048], BF16)
    W3B = dsc("W3B", [2, 16, 128, 2048], BF16)

    S = AutoSync(nc)
    op = S.op
    dma = S.dma

    G = Scope(nc, "g")
    ident_f = G.sb("identf", [128, 128], F32)
    ident_b = G.sb("identb", [128, 128], BF16)
    ones_b = G.sb("onesb", [128, 128], BF16)
    ones_f = G.sb("onesf", [128, 128], F32)
    masks = G.sb("masks", [128, 8, 128], F32)
    colT = G.sb("colT", [128, CV_N], F32)
    gates = G.sb("gates", [128, NT, 16], F32)

    def cvc(r):
        return colT[:, r:r + 1]

    MK_LF, MK_UF, MK_MF, MK_LB, MK_UB, MK_MB, MK_WP, MK_WN = range(8)

    blocks = [(0, NCT, True)]
    t0 = NCT
    while t0 < NT:
        n = min(4, NT - t0)
        blocks.append((t0, n, False))
        t0 += n

    def phase_consts():
        sc = Scope(nc, "c")
        cst = sc.sb("cst", [128, 2, 128], F32)
        ptr = sc.ps("ptr", [128, 256], F32)
        dma("sp", masks[:], cmask.rearrange("m p f -> p m f"))
        op("dve", "tensor_tensor", out=ident_f[:], in0=masks[:, MK_UF, :], in1=masks[:, MK_UB, :], op=ALU.mult)
        op("dve", "tensor_copy", out=ident_b[:], in_=ident_f[:])
        op("pool", "memset", ones_b[:], 1.0)
        op("pool", "memset", ones_f[:], 1.0)
        dma("sp", cst[:, 0, :], colv[0:128, :])
        dma("sp", cst[0:CV_N - 128, 1, :], colv[128:CV_N, :])
        op("pe", "transpose", ptr[:, 0:128], cst[:, 0, :], ident_f[:])
        op("pe", "transpose", ptr[:, 128:128 + (CV_N - 128)], cst[0:CV_N - 128, 1, :], ident_f[0:CV_N - 128, 0:CV_N - 128])
        op("dve", "tensor_copy", out=colT[:], in_=ptr[:, 0:CV_N])
        zpad = sc.sb("zpad", [128, 256], BF16)
        op("pool", "memset", zpad[:], 0.0)
        for g in range(2):
            dma("act", KT[g][:, T:T + 128], zpad[:, 0:128])
        dma("act", VT[T:T + 128, :], zpad[:])
        S.barrier()
        sc.close()

    def phase_mod(l):
        sc = Scope(nc, "m")
        c16 = sc.sb("c16", [16, 128], F32)
        cT = sc.sb("cT", [128, 16], F32)
        pc = sc.ps("pc", [128, 16], F32)
        wm = Rot([sc.sb("wm%d" % i, [128, 8, 512], F32) for i in range(2)])
        pm = Rot([sc.ps("pm%d" % i, [2, 512], F32) for i in range(2)])
        modrow = sc.sb("modrow", [2, 6 * D], F32)
        bm = sc.sb("bm", [2, 6 * D], F32)
        gm = sc.sb("gm", [2, 2, D], F32)
        res = sc.sb("res", [2, 6 * D], F32)
        dma("sp", c16[:], cv16)
        op("act", "activation", out=c16[:], in_=c16[:], func=AF.Silu)
        op("pe", "transpose", pc[:], c16[:], ident_f[0:16, 0:16])
        op("dve", "tensor_copy", out=cT[:], in_=pc[:])
        dma("sp", bm[:], b_mod[l:l + 1, :].to_broadcast([2, 6 * D]))
        dma("sp", gm[:, 0, :], rowv[:, RV_GMIX + l * D:RV_GMIX + (l + 1) * D].to_broadcast([2, D]))
        dma("sp", gm[:, 1, :], rowv[:, RV_GFFN + l * D:RV_GFFN + (l + 1) * D].to_broadcast([2, D]))
        for nb in range(12):
            w = wm.next()
            dma("sp", w[:], w_mod[l, :, nb * 512:(nb + 1) * 512].rearrange("(c p) n -> p c n", p=128))
            p = pm.next()
            for c in range(8):
                op("pe", "matmul", p[:], cT[:, 2 * c:2 * c + 2], w[:, c, :], start=(c == 0), stop=(c == 7))
            op("dve", "tensor_tensor", out=modrow[:, nb * 512:(nb + 1) * 512], in0=p[:], in1=bm[:, nb * 512:(nb + 1) * 512], op=ALU.add)
        for half in range(2):
            b = half * 3 * D
            op("dve", "scalar_tensor_tensor", out=res[:, b:b + D], in0=modrow[:, b + D:b + 2 * D], scalar=1.0,
               in1=gm[:, half, :], op0=ALU.add, op1=ALU.mult)
            op("dve", "tensor_copy", out=res[:, b + D:b + 2 * D], in_=modrow[:, b:b + D])
            op("dve", "tensor_copy", out=res[:, b + 2 * D:b + 3 * D], in_=modrow[:, b + 2 * D:b + 3 * D])
        dma("act", MODV[l], res[:])
        S.barrier()
        sc.close()

    def load_mod(sc, l, idx, name):
        tl = []
        for s in range(2):
            t = sc.sb("%s%d" % (name, s), [128, D], F32)
            dma("sp", t[:], MODV[l, s:s + 1, idx * D:(idx + 1) * D].to_broadcast([128, D]))
            tl.append(t)
        return tl

    def load_row(sc, off, n, name):
        t = sc.sb(name, [128, n], F32)
        dma("sp", t[:], rowv[:, off:off + n].to_broadcast([128, n]))
        return t

    def rstd_from_ss(sc_tiles, ss, n):
        t1, t2 = sc_tiles
        op("dve", "tensor_scalar", out=t1[:], in0=ss, scalar1=1.0 / n, scalar2=EPS, op0=ALU.mult, op1=ALU.add)
        op("act", "activation", out=t1[:], in_=t1[:], func=AF.Sqrt)
        op("dve", "reciprocal", out=t2[:], in_=t1[:])
        return t2

    def phase_wconv():
        sc = Scope(nc, "w")
        st = Rot([sc.sb("st%d" % i, [128, 2048], F32) for i in range(3)])
        sb_ = Rot([sc.sb("sb%d" % i, [128, 2048], BF16) for i in range(3)])
        i = 0
        for l in range(2):
            for e in range(16):
                for src, dst in ((moe_w1, W1B), (moe_w3, W3B)):
                    a = st.next()
                    b = sb_.next()
                    dma("sp", a[:].rearrange("p (c f) -> p c f", c=8), src[l, e].rearrange("(c p) f -> p c f", p=128))
                    op(("pool", "dve", "act")[i % 3], "tensor_copy" if i % 3 != 2 else "copy", out=b[:], in_=a[:])
                    dma("act", dst[l, e], b[:])
                    i += 1
        S.barrier()
        sc.close()

    def phase_inproj(l, GL):
        sc = Scope(nc, "a")
        NW = 3872 if l == 0 else 3584
        Wb = sc.sb("Wb", [128, 8, NW + 1280], BF16)
        for c in range(8):
            dma("pool", Wb[:, c, 0:NW], w_in[l][c * 128:(c + 1) * 128, :])
            dma("pool", Wb[:, c, NW:NW + 1280], w_sw[l][c * 128:(c + 1) * 128, :])
        gsc = load_mod(sc, l, 0, "gsc")
        sh = load_mod(sc, l, 1, "sh")
        xt = Rot([sc.sb("xt%d" % i, [128, D], F32) for i in range(2)])
        junk = sc.sb("junk", [128, D], BF16)
        ss = sc.sb("ss", [128, 1], F32)
        r1 = sc.sb("r1", [128, 1], F32)
        r2 = sc.sb("r2", [128, 1], F32)
        tmp = sc.sb("tmp", [128, D], F32)
        htok = Rot([sc.sb("htok%d" % i, [128, D], BF16) for i in range(2)])
        hTs = [sc.sb("hT%d" % i, [128, 8, 512], BF16) for i in range(2)]
        ptp = sc.ps("ptp", [128, 8, 128], BF16)
        pf = [sc.ps("pf%d" % i, [128, 512], F32) for i in range(4)]
        pss = sc.ps("pss", [128, 512], F32)
        pz = sc.ps("pz", [128, 1024], F32)
        pdv = pss
        stg = Rot([sc.sb("stg%d" % i, [128, 512], F32) for i in range(2)])
        stb = Rot([sc.sb("stb%d" % i, [128, 512], BF16) for i in range(2)])
        zst = Rot([sc.sb("zst%d" % i, [128, D], BF16) for i in range(2)])
        vst = Rot([sc.sb("vst%d" % i, [128, 256], BF16) for i in range(2)])
        sq = sc.sb("sq", [128, 512], BF16)
        rs1 = sc.sb("rs1", [128, 512], F32)
        rs2 = sc.sb("rs2", [128, 512], F32)
        qn = sc.sb("qn", [128, 512], F32)
        qs = sc.sb("qs", [128, 512], F32)
        cosb = sc.sb("cosb", [128, 512], F32)
        sinb = sc.sb("sinb", [128, 512], F32)
        if l == 0:
            dtb = load_row(sc, RV_DTB, 32, "dtb")
            alog = load_row(sc, RV_ALOG, 32, "alog")
            aneg = sc.sb("aneg", [128, 32], F32)
            op("act", "activation", out=aneg[:], in_=alog[:], func=AF.Exp)
            op("dve", "tensor_scalar", out=aneg[:], in0=aneg[:], scalar1=-1.0, scalar2=None, op0=ALU.mult)
            d1 = sc.sb("d1", [128, 32], F32)
            d2 = sc.sb("d2", [128, 32], F32)
            d3 = sc.sb("d3", [128, 32], F32)
        if l == 0:
            zc = sc.sb("zc", [128, 4], F32)
            op("pool", "memset", zc[:], 0.0)
            for r0 in range(0, 1280, 128):
                dma("act", XBC[r0:r0 + 128, 0:2], zc[:, 0:2])
                dma("act", XBC[r0:r0 + 128, CTXN + 2:CTXN + 6], zc[:, 0:4])
                dma("act", XBC[r0:r0 + 128, W - 2:W], zc[:, 0:2])
        xsrc = xin if l == 0 else XRES
        if l == 0:
            fm = [("raw", 1024 + 128 * i, i) for i in range(10)]
            qcol, kcol, vcol = 2336, 3360, 3616
        else:
            fm = [("gelu", 128 * i, i) for i in range(8)] + [("raw", 1024 + 128 * i, i) for i in range(8)]
            qcol, kcol, vcol = 2048, 3072, 3328
        def prologue(bi):
            tile0, n, is_ctx = blocks[bi]
            hT = hTs[bi % 2]
            s = 1 if is_ctx else 0
            for j in range(n):
                x = xt.next()
                dma("sp", x[:], xsrc[(tile0 + j) * 128:(tile0 + j + 1) * 128, :])
                op("act", "activation", out=junk[:], in_=x[:], func=AF.Square, accum_out=ss[:])
                rstd = rstd_from_ss((r1, r2), ss[:], D)
                op("dve", "scalar_tensor_tensor", out=tmp[:], in0=x[:], scalar=rstd[:, 0:1], in1=gsc[s][:], op0=ALU.mult, op1=ALU.mult)
                h = htok.next()
                op("pool", "tensor_tensor", out=h[:], in0=tmp[:], in1=sh[s][:], op=ALU.add)
                for c in range(8):
                    op("pe", "transpose", ptp[:, c, :], h[:, c * 128:(c + 1) * 128], ident_b[:])
                op("act", "copy", out=hT[:, :, j * 128:(j + 1) * 128], in_=ptp[:])

        def body(bi):
            tile0, n, is_ctx = blocks[bi]
            hT = hTs[bi % 2]
            NB = n * 128
            s = 1 if is_ctx else 0
            tok0 = tile0 * 128
            if not is_ctx:
                lt0 = tok0 - CTXN
                dma("sp", cosb[:, 0:NB], cos2[:, lt0:lt0 + NB])
                dma("sp", sinb[:, 0:NB], sin2[:, lt0:lt0 + NB])
            colbase = (COL_CTX + tok0) if is_ctx else (COL_LAT + tok0 - CTXN)

            def mm_fm(p, col):
                for c in range(8):
                    op("pe", "matmul", p[:, 0:NB], Wb[:, c, col:col + 128], hT[:, c, 0:NB], start=(c == 0), stop=(c == 7))

            for (kind, col, idx) in fm:
                p = pf[idx % 4]
                mm_fm(p, col)
                if kind == "raw":
                    st = stg.next()
                    op("act", "copy", out=st[:, 0:NB], in_=p[:, 0:NB])
                    dma("act", XBC[idx * 128:(idx + 1) * 128, colbase:colbase + NB], st[:, 0:NB])
                else:
                    st = stb.next()
                    op("act", "activation", out=st[:, 0:NB], in_=p[:, 0:NB], func=AF.Gelu)
                    dma("act", GATE[idx, :, tok0:tok0 + NB], st[:, 0:NB])
            for hh in range(10):
                isq = hh < 8
                if l == 1 and is_ctx and isq:
                    continue
                col = (qcol + hh * 128) if isq else (kcol + (hh - 8) * 128)
                cols = NW + hh * 128
                pq, pw = (pf[0], pf[1]) if hh % 2 == 0 else (pf[2], pf[3])
                mm_fm(pq, col)
                mm_fm(pw, cols)
                if l == 0:
                    op("act", "activation", out=sq[:, 0:NB], in_=pq[:, 0:NB], func=AF.Square)
                    op("pe", "matmul", pss[:, 0:NB], ones_b[:], sq[:, 0:NB], start=True, stop=True)
                    op("act", "activation", out=rs1[:, 0:NB], in_=pss[:, 0:NB], func=AF.Sqrt, scale=1.0 / 128, bias=EPS)
                    op("dve", "reciprocal", out=rs2[:, 0:NB], in_=rs1[:, 0:NB])
                    g_ = cvc(CV_QG if isq else CV_KG)
                    gs_ = cvc(CV_QGS if isq else CV_KGS)
                    op("dve", "scalar_tensor_tensor", out=qn[:, 0:NB], in0=pq[:, 0:NB], scalar=g_, in1=rs2[:, 0:NB], op0=ALU.mult, op1=ALU.mult)
                    op("dve", "scalar_tensor_tensor", out=qs[:, 0:NB], in0=pw[:, 0:NB], scalar=gs_, in1=rs2[:, 0:NB], op0=ALU.mult, op1=ALU.mult)
                    a_n, a_s = qn, qs
                    e1, e2 = "pool", "pool"
                else:
                    a_n, a_s = pq, pw
                    e1, e2 = "dve", "dve"
                st = stb.next()
                if is_ctx:
                    op("act", "copy", out=st[:, 0:NB], in_=a_n[:, 0:NB])
                else:
                    o1, o2 = (qn, qs) if l == 0 else (rs1, rs2)
                    op(e1, "tensor_tensor", out=o1[:, 0:NB], in0=a_n[:, 0:NB], in1=cosb[:, 0:NB], op=ALU.mult)
                    op(e2, "tensor_tensor", out=o2[:, 0:NB], in0=a_s[:, 0:NB], in1=sinb[:, 0:NB], op=ALU.mult)
                    op("dve" if l == 0 else "pool", "tensor_tensor", out=st[:, 0:NB], in0=o1[:, 0:NB], in1=o2[:, 0:NB], op=ALU.add)
                dst = QT[hh] if isq else KT[hh - 8]
                dma("act", dst[:, tok0:tok0 + NB], st[:, 0:NB])
            for j in range(n):
                ti = tile0 + j
                if l == 0:
                    for half in range(2):
                        for c in range(8):
                            op("pe", "matmul", pz[:, half * 512:(half + 1) * 512], hT[:, c, j * 128:(j + 1) * 128],
                               Wb[:, c, half * 512:(half + 1) * 512], start=(c == 0), stop=(c == 7))
                    z = zst.next()
                    op("act", "activation", out=z[:], in_=pz[:], func=AF.Silu)
                    dma("act", ZT[ti * 128:(ti + 1) * 128, :], z[:])
                    for c in range(8):
                        op("pe", "matmul", pdv[:, 0:32], hT[:, c, j * 128:(j + 1) * 128], Wb[:, c, 2304:2336], start=(c == 0), stop=(c == 7))
                    op("dve", "tensor_tensor", out=d1[:], in0=pdv[:, 0:32], in1=dtb[:], op=ALU.add)
                    op("dve", "scalar_tensor_tensor", out=d2[:], in0=d1[:], scalar=-1.0, in1=d1[:], op0=ALU.mult, op1=ALU.max)
                    op("act", "activation", out=d2[:], in_=d2[:], func=AF.Exp, scale=-1.0)
                    op("act", "activation", out=d3[:], in_=d2[:], func=AF.Ln, bias=1.0)
                    op("dve", "scalar_tensor_tensor", out=GL["dtall"][:, ti, :], in0=d1[:], scalar=0.0, in1=d3[:], op0=ALU.max, op1=ALU.add)
                    op("dve", "tensor_tensor", out=GL["dAall"][:, ti, :], in0=GL["dtall"][:, ti, :], in1=aneg[:], op=ALU.mult)
                for c in range(8):
                    op("pe", "matmul", pdv[:, 256:512], hT[:, c, j * 128:(j + 1) * 128], Wb[:, c, vcol:vcol + 256], start=(c == 0), stop=(c == 7))
                v = vst.next()
                op("act", "copy", out=v[:], in_=pdv[:, 256:512])
                dma("act", VT[ti * 128:(ti + 1) * 128, :], v[:])
        prologue(0)
        for bi in range(len(blocks)):
            if bi + 1 < len(blocks):
                prologue(bi + 1)
            body(bi)
        S.barrier()
        sc.close()

    def conv_spans(maxn):
        sp = []
        for c0 in range(0, CTXN, maxn):
            n = min(maxn, CTXN - c0)
            sp.append((COL_CTX + c0, n, c0))
        for c0 in range(0, L, maxn):
            n = min(maxn, L - c0)
            sp.append((COL_LAT + c0, n, CTXN + c0))
        return sp

    def conv5(raw, acc, n, wrow0, stride, cc, brow):
        op("dve", "tensor_scalar", out=acc[:, 0:n], in0=raw[:, 0:n], scalar1=cvc(wrow0 + cc), scalar2=cvc(brow + cc), op0=ALU.mult, op1=ALU.add)
        for k in range(1, 5):
            op("dve", "scalar_tensor_tensor", out=acc[:, 0:n], in0=raw[:, k:k + n], scalar=cvc(wrow0 + k * stride + cc), in1=acc[:, 0:n], op0=ALU.mult, op1=ALU.add)

    def phase_conv0():
        sc = Scope(nc, "v")
        raw = Rot([sc.sb("raw%d" % i, [128, 1028], F32) for i in range(2)])
        acc = Rot([sc.sb("acc%d" % i, [128, 1024], F32) for i in range(2)])
        act = Rot([sc.sb("act%d" % i, [128, 1024], BF16) for i in range(2)])
        ptp = Rot([sc.ps("ptp%d" % i, [128, 4, 128], BF16) for i in range(2)])
        xst = Rot([sc.sb("xst%d" % i, [128, 4, 128], BF16) for i in range(3)])
        for cc in range(10):
            for (c0, n, tok0) in conv_spans(1024):
                r = raw.next()
                dma("sp", r[:, 0:n + 4], XBC[cc * 128:(cc + 1) * 128, c0 - 2:c0 + n + 2])
                a = acc.next()
                conv5(r, a, n, CV_SSD_CW, 10, cc, CV_SSD_CB)
                b = act.next()
                op("act", "activation", out=b[:, 0:n], in_=a[:, 0:n], func=AF.Silu)
                if cc >= 8:
                    dst = BT if cc == 8 else CTs
                    dma("act", dst[0, :, tok0:tok0 + n], b[0:64, 0:n])
                    dma("act", dst[1, :, tok0:tok0 + n], b[64:128, 0:n])
                if cc <= 8:
                    for j0 in range(0, n // 128, 4):
                        nj = min(4, n // 128 - j0)
                        p = ptp.next()
                        for j in range(nj):
                            op("pe", "transpose", p[:, j, :], b[:, (j0 + j) * 128:(j0 + j + 1) * 128], ident_b[:])
                        x = xst.next()
                        if (j0 // 4) % 2:
                            op("act", "copy", out=x[:, 0:nj, :], in_=p[:, 0:nj, :])
                        else:
                            op("dve", "tensor_copy", out=x[:, 0:nj, :], in_=p[:, 0:nj, :])
                        r0 = tok0 + j0 * 128
                        if cc < 8:
                            dma("act", XS[r0:r0 + nj * 128, cc * 128:(cc + 1) * 128].rearrange("(j p) c -> p j c", p=128), x[:, 0:nj, :])
                        else:
                            dma("act", BTOK[r0:r0 + nj * 128, :].rearrange("(j p) c -> p j c", p=128), x[:, 0:nj, :])
        S.barrier()
        sc.close()

    def phase_ssd(GL):
        sc = Scope(nc, "s")
        dtall, dAall = GL["dtall"], GL["dAall"]
        dsk = load_row(sc, RV_SSD_D, 16, "dsk")
        normg = load_row(sc, RV_NORMG, D, "normg")
        Hf = sc.sb("Hf", [64, 2, 512], F32)
        Hb = sc.sb("Hb", [64, 2, 512], BF16)
        tmpH = sc.sb("tmpH", [64, 2, 512], F32)
        xs_ = Rot([sc.sb("xs%d" % i, [128, D], BF16) for i in range(3)])
        btok_ = Rot([sc.sb("btok%d" % i, [128, 128], BF16) for i in range(3)])
        bt_ = Rot([sc.sb("bt%d" % i, [64, 2, 128], BF16) for i in range(3)])
        ct_ = Rot([sc.sb("ct%d" % i, [64, 2, 128], BF16) for i in range(3)])
        psm_ = Rot([sc.ps("psm", [128, 512], F32)])
        psm = psm_.items[0]
        pcb = psm[:, 0:256].rearrange("p (g i) -> p g i", g=2)
        pac = psm[:, 256:288]
        ptb = sc.ps("ptb", [128, 8, 128], BF16)
        pseg = sc.ps("pseg", [128, 1024], F32)
        pyd = sc.ps("pyd", [128, 1024], F32)
        pch = sc.ps("pch", [128, 1024], F32)
        cbm_ = Rot([sc.sb("cbm%d" % i, [128, 2, 128], F32) for i in range(3)])
        Z_ = Rot([sc.sb("Z%d" % i, [128, 16, 128], F32) for i in range(2)])
        e__ = Rot([sc.sb("e%d" % i, [128, 16, 128], F32) for i in range(3)])
        wT_ = Rot([sc.sb("wT%d" % i, [128, 16, 128], BF16) for i in range(2)])
        sm_ = Rot([sc.sb("sm%d" % i, [128, 96], F32) for i in range(3)])
        y1_ = Rot([sc.sb("y1%d" % i, [128, D], F32) for i in range(2)])
        y2 = Rot([sc.sb("y2%d" % i, [128, D], F32) for i in range(2)])
        xw_ = Rot([sc.sb("xw%d" % i, [128, D], BF16) for i in range(3)])
        yf_ = Rot([sc.sb("yf%d" % i, [128, D], F32) for i in range(2)])
        zs_ = Rot([sc.sb("zs%d" % i, [128, D], BF16) for i in range(2)])
        ss = sc.sb("ss", [128, 1], F32)
        r1 = sc.sb("r1", [128, 1], F32)
        r2 = sc.sb("r2", [128, 1], F32)
        junk = sc.sb("junk", [128, D], BF16)
        ytok = sc.sb("ytok", [128, D], BF16)
        yst = Rot([sc.sb("yst%d" % i, [128, 8, 128], BF16) for i in range(2)])

        Ssb_ = Rot([sc.sb("Ssb%d" % i, [64, 2, 512], F32) for i in range(2)])

        def partA1(ti, d):
            mL, mU, mM = (MK_LF, MK_UF, MK_MF) if d == 0 else (MK_LB, MK_UB, MK_MB)
            r0 = ti * 128
            xs = xs_.next()
            dma("sp", xs[:], XS[r0:r0 + 128, :])
            btok = btok_.next()
            dma("sp", btok[:], BTOK[r0:r0 + 128, :])
            bt = bt_.next()
            dma("sp", bt[:], BT[:, :, r0:r0 + 128].rearrange("g n t -> n g t"))
            ct = ct_.next()
            dma("sp", ct[:], CTs[:, :, r0:r0 + 128].rearrange("g n t -> n g t"))
            dA = dAall[:, ti, d * 16:(d + 1) * 16]
            dt = dtall[:, ti, d * 16:(d + 1) * 16]
            cbm, Z, e_, sm, xw = cbm_.next(), Z_.next(), e__.next(), sm_.next(), xw_.next()
            acs, eacs, ecd, te = sm[:, 0:32], sm[:, 32:48], sm[:, 48:64], sm[:, 64:80]
            for g in range(2):
                op("pe", "matmul", pcb[:, g, :], bt[:, g, :], ct[:, g, :], start=True, stop=True)
            op("pe", "matmul", pac[:, 0:16], masks[:, mU, :], dA, start=True, stop=True)
            op("pe", "matmul", pac[:, 16:32], ones_f[:], dA, start=True, stop=True)
            op("dve", "tensor_tensor", out=cbm[:], in0=pcb, in1=bc(masks[:, mM, :], 1, [128, 2, 128]), op=ALU.mult)
            op("dve", "tensor_copy", out=acs, in_=pac)
            op("pool", "tensor_tensor", out=Z[:], in0=bc(masks[:, mU, :], 1, [128, 16, 128]), in1=bc(dA, 2, [128, 16, 128]), op=ALU.mult)
            Zf = Z[:].rearrange("p h i -> p (h i)")
            ef = e_[:].rearrange("p h i -> p (h i)")
            for hf_ in range(2):
                for q in range(2):
                    c0 = hf_ * 1024 + q * 512
                    op("pe", "matmul", pseg[:, q * 512:(q + 1) * 512], masks[:, mL, :], Zf[:, c0:c0 + 512], start=True, stop=True)
                op("act", "activation", out=ef[:, hf_ * 1024:(hf_ + 1) * 1024], in_=pseg[:], func=AF.Exp)
            op("act", "activation", out=eacs, in_=acs[:, 0:16], func=AF.Exp)
            op("act", "activation", out=ecd, in_=acs[:, 16:32], func=AF.Exp)
            op("dve", "tensor_tensor", out=te, in0=acs[:, 16:32], in1=acs[:, 0:16], op=ALU.subtract)
            op("act", "activation", out=te, in_=te, func=AF.Exp)
            op("dve", "tensor_tensor", out=te, in0=te, in1=dt, op=ALU.mult)
            op("pool", "tensor_tensor", out=xw[:].rearrange("p (h q) -> p h q", h=16), in0=xs[:].rearrange("p (h q) -> p h q", h=16),
               in1=bc(te, 2, [128, 16, 64]), op=ALU.mult)
            return dict(ti=ti, xs=xs, ct=ct, btok=btok, sm=sm, cbm=cbm, e=e_, xw=xw, dt=dt)

        def partA2(st):
            xs, btok, cbm, e_, xw, dt = st["xs"], st["btok"], st["cbm"], st["e"], st["xw"], st["dt"]
            wT, y1, Ssb = wT_.next(), y1_.next(), Ssb_.next()
            op("dve", "tensor_tensor", out=e_[:], in0=e_[:], in1=bc(dt, 2, [128, 16, 128]), op=ALU.mult)
            op("dve", "tensor_tensor", out=wT[:].rearrange("p (g h) i -> p g h i", g=2), in0=e_[:].rearrange("p (g h) i -> p g h i", g=2),
               in1=bc(cbm[:], 2, [128, 2, 8, 128]), op=ALU.mult)
            for h in range(16):
                op("pe", "matmul", pyd[:, h * 64:(h + 1) * 64], wT[:, h, :], xs[:, h * 64:(h + 1) * 64], start=True, stop=True)
            op("act", "copy", out=y1[:], in_=pyd[:])
            pS = pyd[0:64, 0:1024].rearrange("p (g n) -> p g n", g=2)
            for g in range(2):
                op("pe", "matmul", pS[:, g, :], btok[:, g * 64:(g + 1) * 64], xw[:, g * 512:(g + 1) * 512], start=True, stop=True)
            op("act", "copy", out=Ssb[:], in_=pS)
            st["y1"] = y1
            st["Ssb"] = Ssb
            return st

        def partB(st):
            ct, sm, y1, Ssb = st["ct"], st["sm"], st["y1"], st["Ssb"]
            eacs = sm[:, 32:48]
            for g in range(2):
                op("pe", "matmul", pch[:, g * 512:(g + 1) * 512], ct[:, g, :], Hb[:, g, :], start=True, stop=True)
            op("dve", "tensor_tensor", out=tmpH[:].rearrange("p g (h q) -> p (g h) q", h=8), in0=Hf[:].rearrange("p g (h q) -> p (g h) q", h=8),
               in1=bc(sm[0:64, 48:64], 2, [64, 16, 64]), op=ALU.mult)
            op("dve", "tensor_tensor", out=Hf[:], in0=tmpH[:], in1=Ssb[:], op=ALU.add)
            op("act", "copy", out=Hb[:], in_=Hf[:])
            y = y2.next()
            op("dve", "tensor_tensor", out=y[:].rearrange("p (h q) -> p h q", h=16), in0=pch[:].rearrange("p (h q) -> p h q", h=16),
               in1=bc(eacs, 2, [128, 16, 64]), op=ALU.mult)
            op("pool", "tensor_tensor", out=y[:], in0=y[:], in1=y1[:], op=ALU.add)
            return y

        def zero_state():
            op("pool", "memset", Hf[:], 0.0)
            op("pool", "memset", Hb[:], 0.0)

        def fin_fwd(st, y):
            ti = st["ti"]
            dma("act", YF[ti * 128:(ti + 1) * 128, :], y[:])

        def fin_bwd(st, y):
            ti, xs = st["ti"], st["xs"]
            r0 = ti * 128
            yf = yf_.next()
            dma("sp", yf[:], YF[r0:r0 + 128, :])
            zs = zs_.next()
            dma("sp", zs[:], ZT[r0:r0 + 128, :])
            op("pool", "tensor_tensor", out=y[:], in0=y[:], in1=yf[:], op=ALU.add)
            op("dve", "tensor_tensor", out=yf[:].rearrange("p (h q) -> p h q", h=16), in0=xs[:].rearrange("p (h q) -> p h q", h=16),
               in1=bc(dsk[:], 2, [128, 16, 64]), op=ALU.mult)
            op("pool", "tensor_tensor", out=y[:], in0=y[:], in1=yf[:], op=ALU.add)
            op("dve", "tensor_tensor", out=y[:], in0=y[:], in1=zs[:], op=ALU.mult)
            op("act", "activation", out=junk[:], in_=y[:], func=AF.Square, accum_out=ss[:])
            rstd = rstd_from_ss((r1, r2), ss[:], D)
            op("dve", "scalar_tensor_tensor", out=ytok[:], in0=y[:], scalar=rstd[:, 0:1], in1=normg[:], op0=ALU.mult, op1=ALU.mult)
            yT = yst.next()
            for c in range(8):
                op("pe", "transpose", ptb[:, c, :], ytok[:, c * 128:(c + 1) * 128], ident_b[:])
            op("act", "copy", out=yT[:], in_=ptb[:])
            dma("act", YT[0:8, :, r0:r0 + 128].rearrange("c p t -> p c t"), yT[:])

        def run(order, d, fin):
            zero_state()
            n = len(order)
            s1 = {}
            s2 = {}
            s1[0] = partA1(order[0], d)
            if n > 1:
                s1[1] = partA1(order[1], d)
            s2[0] = partA2(s1.pop(0))
            for k in range(n):
                st = s2.pop(k)
                y = partB(st)
                fin(st, y)
                if k + 1 < n:
                    s2[k + 1] = partA2(s1.pop(k + 1))
                if k + 2 < n:
                    s1[k + 2] = partA1(order[k + 2], d)

        run(list(range(NT)), 0, fin_fwd)
        S.barrier()
        run(list(range(NCT - 1, -1, -1)) + list(range(NT - 1, NCT - 1, -1)), 1, fin_bwd)
        S.barrier()
        sc.close()

    def phase_attn(l):
        sc = Scope(nc, "t")
        KTs = sc.sb("KTs", [128, 2, T], BF16)
        Vs = sc.sb("Vs", [128, NT, 256], BF16)
        for g in range(2):
            dma("sp", KTs[:, g, :], KT[g][:, 0:T])
        dma("sp", Vs[:], VT[0:T, :].rearrange("(n p) c -> p n c", p=128))
        q_ = Rot([sc.sb("q%d" % i, [128, 4, 128], BF16) for i in range(3)])
        ps_s = Rot([sc.ps("pss%d" % i, [128, 512], F32) for i in range(3)])
        pT_ = Rot([sc.sb("pT%d" % i, [128, 512], BF16) for i in range(4)])
        po_ = Rot([sc.ps("po%d" % i, [128, 512], F32) for i in range(2)])
        pm_ = Rot([sc.ps("pm%d" % i, [128, 512], F32) for i in range(2)])
        ac_ = Rot([sc.sb("ac%d" % i, [128, 512], F32) for i in range(2)])
        rs_ = Rot([sc.sb("rs%d" % i, [128, 512], F32) for i in range(2)])
        o_ = Rot([sc.sb("o%d" % i, [128, 4, 128], BF16) for i in range(2)])
        if l == 1:
            sk = load_row(sc, RV_SINK, 8, "sk")
            op("act", "activation", out=sk[:], in_=sk[:], func=AF.Exp)
        groups = []
        for ti in range(NT):
            is_ctx = ti < NCT
            if l == 1 and is_ctx:
                continue
            if l == 0:
                keys = [(k, None) for k in (range(NCT) if is_ctx else range(NT))]
            else:
                keys = [(k, None) for k in range(NCT)]
                if ti - 1 >= NCT:
                    keys.append((ti - 1, MK_WP))
                keys.append((ti, None))
                if ti + 1 < NT:
                    keys.append((ti + 1, MK_WN))
            for g in range(2):
                groups.append((ti, g, keys))
        items = []
        for gi, (ti, g, keys) in enumerate(groups):
            for i, (kt, mk) in enumerate(keys):
                items.append((gi, i, kt, mk, i == 0, i == len(keys) - 1))
        qbuf = {}

        def load_q(gi):
            if gi < len(groups) and gi not in qbuf:
                ti, g, _ = groups[gi]
                q = q_.next()
                dma("sp", q[:], QT[g * 4:(g + 1) * 4, :, ti * 128:(ti + 1) * 128].rearrange("h d t -> d h t"))
                qbuf[gi] = q

        def issue_S(n):
            gi, i, kt, mk, first, last = items[n]
            load_q(gi)
            if first:
                load_q(gi + 1)
            g = groups[gi][1]
            p = ps_s.next()
            op("pe", "matmul", p[:], KTs[:, g, kt * 128:(kt + 1) * 128], qbuf[gi][:].rearrange("d h t -> d (h t)"), start=True, stop=True)
            return p

        LOOK = 2
        pend = [issue_S(n) for n in range(min(LOOK, len(items)))]
        acc = {}
        for n, (gi, i, kt, mk, first, last) in enumerate(items):
            if n + LOOK < len(items):
                pend.append(issue_S(n + LOOK))
            p = pend.pop(0)
            ti, g, keys = groups[gi]
            if first:
                acc[gi] = (po_.next(), pm_.next(), ac_.next())
            po, pm, ac = acc[gi]
            pT = pT_.next()
            op("act", "activation", out=pT[:], in_=p[:], func=AF.Exp, scale=ATT_SCALE)
            if mk is not None:
                op("pool", "tensor_tensor", out=pT[:].rearrange("k (h t) -> k h t", h=4), in0=pT[:].rearrange("k (h t) -> k h t", h=4),
                   in1=bc(masks[:, mk, :], 1, [128, 4, 128]), op=ALU.mult)
            op("pe", "matmul", po[:], Vs[:, kt, g * 128:(g + 1) * 128], pT[:], start=first, stop=last)
            nk = len(keys)
            use_dve = (nk >= 4) and (i % 2 == 1)
            if use_dve:
                if i == 1:
                    op("dve", "tensor_copy", out=ac[:], in_=pT[:])
                else:
                    op("dve", "tensor_tensor", out=ac[:], in0=ac[:], in1=pT[:], op=ALU.add)
            else:
                op("pe", "matmul", pm[:], ones_b[:], pT[:], start=first, stop=(last and nk < 4))
            if last:
                if nk >= 4:
                    op("pe", "matmul", pm[:], ones_f[:], ac[:], start=False, stop=True)
                rs = rs_.next()
                if l == 1:
                    op("dve", "tensor_tensor", out=rs[:].rearrange("d (h t) -> d h t", h=4), in0=pm[:].rearrange("d (h t) -> d h t", h=4),
                       in1=bc(sk[:, g * 4:(g + 1) * 4], 2, [128, 4, 128]), op=ALU.add)
                    op("dve", "reciprocal", out=rs[:], in_=rs[:])
                else:
                    op("dve", "reciprocal", out=rs[:], in_=pm[:])
                o = o_.next()
                op("dve", "tensor_tensor", out=o[:].rearrange("d h t -> d (h t)"), in0=po[:], in1=rs[:], op=ALU.mult)
                dma("act", YT[8 + g * 4:8 + (g + 1) * 4, :, ti * 128:(ti + 1) * 128].rearrange("h d t -> d h t"), o[:])
                del acc[gi]
                qbuf.pop(gi, None)
        S.barrier()
        sc.close()

    def phase_select():
        sc = Scope(nc, "q")
        fl = sc.sb("fl", [128, 8], F32)
        dma("sp", fl[:], rflag.to_broadcast([128, 8]))
        zt = sc.sb("zt", [128, 256], BF16)
        cb_ = Rot([sc.sb("cb%d" % i, [128, LQ + 256], BF16) for i in range(3)])
        ab_ = Rot([sc.sb("ab%d" % i, [128, LQ + 256], BF16) for i in range(2)])
        cf_ = Rot([sc.sb("cf%d" % i, [128, D], F32) for i in range(3)])
        af_ = Rot([sc.sb("af%d" % i, [128, D], F32) for i in range(2)])
        cnt = [0]

        def sel(dst, cands, n, f32=False):
            acc = (af_ if f32 else ab_).next()
            e = "dve"
            cnt[0] += 1
            for j, c in enumerate(cands):
                t = (cf_ if f32 else cb_).next()
                dma("sp", t[:, 0:n], c)
                if j == 0:
                    op(e, "tensor_scalar", out=acc[:, 0:n], in0=t[:, 0:n], scalar1=fl[:, 0:1], scalar2=None, op0=ALU.mult)
                else:
                    op(e, "scalar_tensor_tensor", out=acc[:, 0:n], in0=t[:, 0:n], scalar=fl[:, j:j + 1], in1=acc[:, 0:n], op0=ALU.mult, op1=ALU.add)
            dma("act", dst, acc[:, 0:n])

        for h in range(8):
            sel(QTq[h], [QT[h][:, CTXN + j * LQ:CTXN + (j + 1) * LQ] for j in range(4)], LQ)
            sel(YTL[h], [YT[h][:, CTXN + j * LQ:CTXN + (j + 1) * LQ] for j in range(4)], LQ)
        for g in range(2):
            sel(KTq[g], [KT[g][:, CTXN + j * LQ - 128:CTXN + (j + 1) * LQ + 128] for j in range(4)], LQ + 256)
        for tl in range(NLQ + 2):
            sel(VTq[tl * 128:(tl + 1) * 128, :], [VT[CTXN + j * LQ - 128 + tl * 128:CTXN + j * LQ + tl * 128, :] for j in range(4)], 256)
        for tl in range(NLQ):
            sel(Xq[tl * 128:(tl + 1) * 128, :], [XRES[CTXN + j * LQ + tl * 128:CTXN + j * LQ + (tl + 1) * 128, :] for j in range(4)], D, f32=True)
        S.barrier()
        sc.close()

    def phase_attn_local():
        sc = Scope(nc, "u")
        fl = sc.sb("fl", [128, 8], F32)
        dma("sp", fl[:], rflag.to_broadcast([128, 8]))
        NE = NLQ + 2
        Kc = sc.sb("Kc", [128, 2, CTXN], BF16)
        Vc = sc.sb("Vc", [128, NCT, 256], BF16)
        Kq = sc.sb("Kq", [128, 2, NE * 128], BF16)
        Vq = sc.sb("Vq", [128, NE, 256], BF16)
        for g in range(2):
            dma("sp", Kc[:, g, :], KT[g][:, 0:CTXN])
            dma("sp", Kq[:, g, :], KTq[g])
        dma("sp", Vc[:], VT[0:CTXN, :].rearrange("(n p) c -> p n c", p=128))
        dma("sp", Vq[:], VTq.rearrange("(n p) c -> p n c", p=128))
        mfirst = sc.sb("mfirst", [128, 128], F32)
        mlast = sc.sb("mlast", [128, 128], F32)
        op("dve", "tensor_scalar", out=mfirst[:], in0=masks[:, MK_WP, :], scalar1=fl[:, 4:5], scalar2=None, op0=ALU.mult)
        op("dve", "tensor_scalar", out=mlast[:], in0=masks[:, MK_WN, :], scalar1=fl[:, 5:6], scalar2=None, op0=ALU.mult)
        q_ = Rot([sc.sb("q%d" % i, [128, 4, 128], BF16) for i in range(3)])
        ps_s = Rot([sc.ps("pss%d" % i, [128, 512], F32) for i in range(3)])
        pT_ = Rot([sc.sb("pT%d" % i, [128, 512], BF16) for i in range(4)])
        po_ = Rot([sc.ps("po%d" % i, [128, 512], F32) for i in range(2)])
        pm_ = Rot([sc.ps("pm%d" % i, [128, 512], F32) for i in range(2)])
        rs_ = Rot([sc.sb("rs%d" % i, [128, 512], F32) for i in range(2)])
        o_ = Rot([sc.sb("o%d" % i, [128, 4, 128], BF16) for i in range(2)])
        sk = load_row(sc, RV_SINK, 8, "sk")
        op("act", "activation", out=sk[:], in_=sk[:], func=AF.Exp)
        groups = []
        for j in range(NLQ):
            keys = [("c", k, None) for k in range(NCT)]
            keys.append(("q", j, mfirst[:] if j == 0 else masks[:, MK_WP, :]))
            keys.append(("q", j + 1, None))
            keys.append(("q", j + 2, mlast[:] if j == NLQ - 1 else masks[:, MK_WN, :]))
            for g in range(2):
                groups.append((j, g, keys))
        items = []
        for gi, (j, g, keys) in enumerate(groups):
            for i, (src, kt, mk) in enumerate(keys):
                items.append((gi, i, src, kt, mk, i == 0, i == len(keys) - 1))
        qbuf = {}

        def load_q(gi):
            if gi < len(groups) and gi not in qbuf:
                j, g, _ = groups[gi]
                q = q_.next()
                dma("sp", q[:], QTq[g * 4:(g + 1) * 4, :, j * 128:(j + 1) * 128].rearrange("h d t -> d h t"))
                qbuf[gi] = q

        def issue_S(n):
            gi, i, src, kt, mk, first, last = items[n]
            load_q(gi)
            if first:
                load_q(gi + 1)
            g = groups[gi][1]
            kk = Kc if src == "c" else Kq
            p = ps_s.next()
            op("pe", "matmul", p[:], kk[:, g, kt * 128:(kt + 1) * 128], qbuf[gi][:].rearrange("d h t -> d (h t)"), start=True, stop=True)
            return p

        LOOK = 2
        pend = [issue_S(n) for n in range(min(LOOK, len(items)))]
        acc = {}
        for n, (gi, i, src, kt, mk, first, last) in enumerate(items):
            if n + LOOK < len(items):
                pend.append(issue_S(n + LOOK))
            p = pend.pop(0)
            j, g, keys = groups[gi]
            if first:
                acc[gi] = (po_.next(), pm_.next())
            po, pm = acc[gi]
            pT = pT_.next()
            op("act", "activation", out=pT[:], in_=p[:], func=AF.Exp, scale=ATT_SCALE)
            if mk is not None:
                op("pool", "tensor_tensor", out=pT[:].rearrange("k (h t) -> k h t", h=4), in0=pT[:].rearrange("k (h t) -> k h t", h=4),
                   in1=bc(mk, 1, [128, 4, 128]), op=ALU.mult)
            vv = Vc if src == "c" else Vq
            op("pe", "matmul", po[:], vv[:, kt, g * 128:(g + 1) * 128], pT[:], start=first, stop=last)
            op("pe", "matmul", pm[:], ones_b[:], pT[:], start=first, stop=last)
            if last:
                rs = rs_.next()
                op("dve", "tensor_tensor", out=rs[:].rearrange("d (h t) -> d h t", h=4), in0=pm[:].rearrange("d (h t) -> d h t", h=4),
                   in1=bc(sk[:, g * 4:(g + 1) * 4], 2, [128, 4, 128]), op=ALU.add)
                op("dve", "reciprocal", out=rs[:], in_=rs[:])
                o = o_.next()
                op("dve", "tensor_tensor", out=o[:].rearrange("d h t -> d (h t)"), in0=po[:], in1=rs[:], op=ALU.mult)
                dma("act", YTL[8 + g * 4:8 + (g + 1) * 4, :, j * 128:(j + 1) * 128].rearrange("h d t -> d h t"), o[:])
                del acc[gi]
                qbuf.pop(gi, None)
        S.barrier()
        sc.close()

    def phase_oproj(l):
        sc = Scope(nc, "o")
        Wo = sc.sb("Wo", [128, 16, D], BF16)
        for c in range(16):
            dma("pool", Wo[:, c, :], w_out[l][c * 128:(c + 1) * 128, :])
        g1 = load_mod(sc, l, 2, "g1")
        gsc2 = load_mod(sc, l, 3, "gsc2")
        sh2 = load_mod(sc, l, 4, "sh2")
        wr = sc.sb("wr", [128, 8, 20], F32)
        dma("sp", wr[:], moe_wr[l].rearrange("(c p) n -> p c n", p=128))
        brow = load_row(sc, RV_BROUTE + l * 20, 20, "brow")
        y_ = Rot([sc.sb("y%d" % i, [128, 16, 128], BF16) for i in range(2)])
        x_ = Rot([sc.sb("x%d" % i, [128, D], F32) for i in range(2)])
        po = sc.ps("po", [128, 1024], F32)
        tmp = sc.sb("tmp", [128, D], F32)
        xn_ = Rot([sc.sb("xn%d" % i, [128, D], F32) for i in range(2)])
        junk = sc.sb("junk", [128, D], BF16)
        ss = sc.sb("ss", [128, 1], F32)
        r1 = sc.sb("r1", [128, 1], F32)
        r2 = sc.sb("r2", [128, 1], F32)
        h32 = sc.sb("h32", [128, D], F32)
        pt32 = sc.ps("pt32", [128, 8, 128], F32)
        hT32 = sc.sb("hT32", [128, 8, 128], F32)
        hTb = Rot([sc.sb("hTb%d" % i, [128, 8, 128], BF16) for i in range(2)])
        plog = sc.ps("plog", [128, 32], F32)
        lg = sc.sb("lg", [128, 20], F32)
        sm = sc.sb("sm", [128, 16], F32)
        oh = sc.sb("oh", [128, 4], F32)
        eg = sc.sb("eg", [128, 4], F32)
        pen = sc.sb("pen", [128, 4], F32)
        elm = sc.sb("elm", [128, 16], F32)
        mk1 = sc.sb("mk1", [128, 16], F32)
        el2 = sc.sb("el2", [128, 16], F32)
        mk2 = sc.sb("mk2", [128, 16], F32)
        local = (l == 1)
        if not local:
            xsrc, ysrc, xdst, hdst = xin, YT, XRES, HTF
            tiles = list(range(NT))
            nctx = NCT
        else:
            xsrc, ysrc, xdst, hdst = Xq, YTL, XQ1, HTFq
            tiles = list(range(NLQ))
            nctx = 0
        t_lo = 0
        NTP = len(tiles)
        LG = sc.sb("LG", [128, NT, 20], F32)
        po2 = [po, sc.ps("po_b", [128, 1024], F32)]
        pend = {}

        def mm(k):
            ti = tiles[k]
            r0 = ti * 128
            y = y_.next()
            dma("sp", y[:], ysrc[:, :, r0:r0 + 128].rearrange("c p t -> p c t"))
            x = x_.next()
            dma("sp", x[:], xsrc[r0:r0 + 128, :])
            p = po2[k % 2]
            for half in range(2):
                for c in range(16):
                    op("pe", "matmul", p[:, half * 512:(half + 1) * 512], y[:, c, :], Wo[:, c, half * 512:(half + 1) * 512], start=(c == 0), stop=(c == 15))
            pend[k] = (p, x)

        mm(0)
        for k, ti in enumerate(tiles):
            if k + 1 < len(tiles):
                mm(k + 1)
            p, x = pend.pop(k)
            s = 1 if ti < nctx else 0
            r0 = ti * 128
            op("dve", "tensor_tensor", out=tmp[:], in0=p[:], in1=g1[s][:], op=ALU.mult)
            xn = xn_.next()
            op("pool", "tensor_tensor", out=xn[:], in0=tmp[:], in1=x[:], op=ALU.add)
            dma("act", xdst[r0:r0 + 128, :], xn[:])
            op("act", "activation", out=junk[:], in_=xn[:], func=AF.Square, accum_out=ss[:])
            rstd = rstd_from_ss((r1, r2), ss[:], D)
            op("dve", "scalar_tensor_tensor", out=tmp[:], in0=xn[:], scalar=rstd[:, 0:1], in1=gsc2[s][:], op0=ALU.mult, op1=ALU.mult)
            op("dve", "tensor_tensor", out=h32[:], in0=tmp[:], in1=sh2[s][:], op=ALU.add)
            for c in range(8):
                op("pe", "transpose", pt32[:, c, :], h32[:, c * 128:(c + 1) * 128], ident_f[:])
            op("act", "copy", out=hT32[:], in_=pt32[:])
            hb = hTb.next()
            op("pool", "tensor_copy", out=hb[:], in_=hT32[:])
            dma("act", hdst[:, :, r0:r0 + 128].rearrange("c p t -> p c t"), hb[:])
            for c in range(8):
                op("pe", "matmul", plog[:, 0:20], hT32[:, c, :], wr[:, c, :], start=(c == 0), stop=(c == 7))
            op("dve", "tensor_tensor", out=LG[:, ti, :], in0=plog[:, 0:20], in1=brow[:], op=ALU.add)
        R = sc.sb("R", [128, 10, NT], F32)
        gmax, gsum, pgrp, m1, m2, dm, ed, w1p, w2p = [R[:, i, 0:NTP] for i in range(9)]
        OH = sc.sb("OH", [128, NT, 4], F32)
        ELM = sc.sb("ELM", [128, NT, 16], F32)
        MK1 = sc.sb("MK1", [128, NT, 16], F32)
        EL2 = sc.sb("EL2", [128, NT, 16], F32)
        MK2 = sc.sb("MK2", [128, NT, 16], F32)
        GLv = LG[:, 0:NTP, 0:4]
        ELv = LG[:, 0:NTP, 4:20]
        oh = OH[:, 0:NTP, :]
        elm, mk1, el2, mk2 = [t[:, 0:NTP, :] for t in (ELM, MK1, EL2, MK2)]
        op("dve", "reduce_max", out=gmax, in_=GLv, axis=AX.X)
        op("dve", "tensor_tensor", out=oh, in0=GLv, in1=bc(gmax, 2, [128, NTP, 4]), op=ALU.is_ge)
        op("dve", "tensor_tensor", out=elm[:, :, 0:4], in0=GLv, in1=bc(gmax, 2, [128, NTP, 4]), op=ALU.subtract)
        op("act", "activation", out=elm[:, :, 0:4], in_=elm[:, :, 0:4], func=AF.Exp)
        op("dve", "reduce_sum", out=gsum, in_=elm[:, :, 0:4], axis=AX.X)
        op("dve", "reciprocal", out=pgrp, in_=gsum)
        op("dve", "tensor_scalar", out=oh, in0=oh, scalar1=1.0, scalar2=BIG, op0=ALU.subtract, op1=ALU.mult)
        op("dve", "tensor_tensor", out=elm.rearrange("p t (g k) -> p t g k", g=4), in0=ELv.rearrange("p t (g k) -> p t g k", g=4),
           in1=bc(oh, 3, [128, NTP, 4, 4]), op=ALU.add)
        op("dve", "reduce_max", out=m1, in_=elm, axis=AX.X)
        op("dve", "tensor_tensor", out=mk1, in0=elm, in1=bc(m1, 2, [128, NTP, 16]), op=ALU.is_ge)
        op("dve", "scalar_tensor_tensor", out=el2, in0=mk1, scalar=-BIG, in1=elm, op0=ALU.mult, op1=ALU.add)
        op("dve", "reduce_max", out=m2, in_=el2, axis=AX.X)
        op("dve", "tensor_tensor", out=mk2, in0=el2, in1=bc(m2, 2, [128, NTP, 16]), op=ALU.is_ge)
        op("dve", "tensor_tensor", out=dm, in0=m2, in1=m1, op=ALU.subtract)
        op("act", "activation", out=ed, in_=dm, func=AF.Exp)
        op("dve", "tensor_scalar", out=w1p, in0=ed, scalar1=1.0, scalar2=None, op0=ALU.add)
        op("dve", "reciprocal", out=w1p, in_=w1p)
        op("dve", "tensor_tensor", out=w1p, in0=w1p, in1=pgrp, op=ALU.mult)
        op("dve", "tensor_tensor", out=w2p, in0=w1p, in1=ed, op=ALU.mult)
        op("dve", "tensor_tensor", out=mk1, in0=mk1, in1=bc(w1p, 2, [128, NTP, 16]), op=ALU.mult)
        op("dve", "tensor_tensor", out=mk2, in0=mk2, in1=bc(w2p, 2, [128, NTP, 16]), op=ALU.mult)
        op("dve", "tensor_tensor", out=gates[:, 0:NTP, :], in0=mk1, in1=mk2, op=ALU.add)
        S.barrier()
        sc.close()

    def phase_moe(l):
        sc = Scope(nc, "e")
        last = (l == 1)
        W2b = sc.sb("W2b", [128, 32, D], BF16)
        for e in range(16):
            dma("pool", W2b[:, 2 * e:2 * e + 2, :], moe_w2[l, e].rearrange("(fc p) d -> p fc d", p=128))
        g2 = load_mod(sc, l, 5, "g2")
        if last:
            gfin = load_row(sc, RV_GFIN, D, "gfin")
        hT_ = Rot([sc.sb("hT%d" % i, [128, 8, 512], BF16) for i in range(1)])
        gB_ = Rot([sc.sb("gateB%d" % i, [128, 4, 512], BF16) for i in range(2)])
        hid = sc.sb("hid", [128, 32, 512], BF16)
        w1_ = Rot([sc.sb("w1%d" % i, [128, 8, 256], BF16) for i in range(2)])
        w3_ = Rot([sc.sb("w3%d" % i, [128, 8, 256], BF16) for i in range(2)])
        pa_ = Rot([sc.ps("pa%d" % i, [128, 512], F32) for i in range(2)])
        pu_ = Rot([sc.ps("pu%d" % i, [128, 512], F32) for i in range(2)])
        po_ = Rot([sc.ps("po%d" % i, [128, 512], F32) for i in range(2)])
        sg_ = Rot([sc.sb("sg%d" % i, [128, 512], BF16) for i in range(2)])
        t1_ = Rot([sc.sb("t1%d" % i, [128, 512], BF16) for i in range(2)])
        x_ = Rot([sc.sb("x%d" % i, [128, D], F32) for i in range(1)])
        xn_ = Rot([sc.sb("xn%d" % i, [128, D], F32) for i in range(1)])
        tmp = sc.sb("tmp", [128, 512], F32)
        junk = sc.sb("junk", [128, D], BF16)
        ss = sc.sb("ss", [128, 1], F32)
        r1 = sc.sb("r1", [128, 1], F32)
        r2 = sc.sb("r2", [128, 1], F32)
        if not last:
            blks, hsrc, xsrc2 = blocks, HTF, XRES
        else:
            blks, hsrc, xsrc2 = [(t0_, min(4, NLQ - t0_), False) for t0_ in range(0, NLQ, 4)], HTFq, XQ1
        for (tile0, n, is_ctx) in blks:
            NB = n * 128
            s = 1 if is_ctx else 0
            tok0 = tile0 * 128
            hT = hT_.next()
            dma("sp", hT[:, :, 0:NB], hsrc[:, :, tok0:tok0 + NB].rearrange("c p t -> p c t"))
            for e in range(16):
                if e % 4 == 0:
                    gateB = gB_.next()
                    for j in range(n):
                        p = po_.next()
                        for k in range(4):
                            op("pe", "matmul", p[:, k * 128:(k + 1) * 128], gates[:, tile0 + j, e + k:e + k + 1].to_broadcast([128, 128]),
                               ident_f[:], start=True, stop=True)
                        op("act", "copy", out=gateB[:, :, j * 128:(j + 1) * 128], in_=p[:].rearrange("p (k t) -> p k t", k=4))
                w1 = w1_.next()
                w3 = w3_.next()
                dma("sp", w1[:].rearrange("p c f -> p (c f)"), W1B[l, e])
                dma("sp", w3[:].rearrange("p c f -> p (c f)"), W3B[l, e])
                for fc in range(2):
                    pa = pa_.next()
                    pu = pu_.next()
                    for c in range(8):
                        op("pe", "matmul", pa[:, 0:NB], w1[:, c, fc * 128:(fc + 1) * 128], hT[:, c, 0:NB], start=(c == 0), stop=(c == 7))
                    for c in range(8):
                        op("pe", "matmul", pu[:, 0:NB], w3[:, c, fc * 128:(fc + 1) * 128], hT[:, c, 0:NB], start=(c == 0), stop=(c == 7))
                    sg = sg_.next()
                    op("act", "activation", out=sg[:, 0:NB], in_=pa[:, 0:NB], func=AF.Silu)
                    t1 = t1_.next()
                    op("dve", "tensor_tensor", out=t1[:, 0:NB], in0=pu[:, 0:NB], in1=gateB[:, e % 4, 0:NB], op=ALU.mult)
                    op("pool", "tensor_tensor", out=hid[:, 2 * e + fc, 0:NB], in0=sg[:, 0:NB], in1=t1[:, 0:NB], op=ALU.mult)
            for j in range(n):
                ti = tile0 + j
                r0 = ti * 128
                x = x_.next()
                dma("sp", x[:], xsrc2[r0:r0 + 128, :])
                xn = xn_.next()
                for half in range(2):
                    po = po_.next()
                    for k in range(32):
                        op("pe", "matmul", po[:], hid[:, k, j * 128:(j + 1) * 128], W2b[:, k, half * 512:(half + 1) * 512], start=(k == 0), stop=(k == 31))
                    op("dve", "tensor_tensor", out=tmp[:], in0=po[:], in1=g2[s][:, half * 512:(half + 1) * 512], op=ALU.mult)
                    op("pool", "tensor_tensor", out=xn[:, half * 512:(half + 1) * 512], in0=tmp[:], in1=x[:, half * 512:(half + 1) * 512], op=ALU.add)
                if not last:
                    dma("act", XRES[r0:r0 + 128, :], xn[:])
                else:
                    op("act", "activation", out=junk[:], in_=xn[:], func=AF.Square, accum_out=ss[:])
                    rstd = rstd_from_ss((r1, r2), ss[:], D)
                    op("dve", "scalar_tensor_tensor", out=x[:], in0=xn[:], scalar=rstd[:, 0:1], in1=gfin[:], op0=ALU.mult, op1=ALU.mult)
                    dma("act", out[ti * 128:(ti + 1) * 128, :], x[:])
        S.barrier()
        sc.close()

    def phase_lru():
        sc = Scope(nc, "r")
        xc = sc.sb("xc", [128, T], F32)
        xcb = sc.sb("xcb", [128, T], BF16)
        hf = sc.sb("hf", [128, T], F32)
        hb = sc.sb("hb", [128, T], F32)
        raw = Rot([sc.sb("raw%d" % i, [128, 1028], F32) for i in range(1)])
        wa = sc.sb("wa", [128, 2, 2, 128], BF16)
        pr = sc.ps("pr", [128, 2048], F32)
        pi = sc.ps("pi", [128, 2048], F32)
        rb = sc.sb("rb", [128, 2048], F32)
        ib = sc.sb("ib", [128, 2048], F32)
        sb2 = sc.sb("sb2", [128, 2048], F32)
        gt = Rot([sc.sb("gt%d" % i, [128, 1024], BF16) for i in range(2)])
        yo = Rot([sc.sb("yo%d" % i, [128, 1024], BF16) for i in range(2)])
        lb = sc.sb("lb", [128, 16], F32)
        lb2 = sc.sb("lb2", [128, 16], F32)
        op("act", "activation", out=lb[:], in_=colT[:, CV_LAM:CV_LAM + 16], func=AF.Exp, scale=-1.0)
        op("act", "activation", out=lb[:], in_=lb[:], func=AF.Ln, bias=1.0)
        op("dve", "tensor_scalar", out=lb2[:], in0=lb[:], scalar1=-16.0, scalar2=None, op0=ALU.mult)
        op("dve", "tensor_scalar", out=lb[:], in0=lb[:], scalar1=-8.0, scalar2=None, op0=ALU.mult)
        spans = []
        for c0 in range(0, CTXN, 2048):
            spans.append((c0, min(2048, CTXN - c0)))
        for c0 in range(0, L, 2048):
            spans.append((CTXN + c0, min(2048, L - c0)))
        nctx_sp = len([s_ for s_ in spans if s_[0] < CTXN])
        for cc in range(8):
            for (c0, n, tok0) in conv_spans(1024):
                r = raw.next()
                dma("sp", r[:, 0:n + 4], XBC[cc * 128:(cc + 1) * 128, c0 - 2:c0 + n + 2])
                op("dve", "tensor_scalar", out=xc[:, tok0:tok0 + n], in0=r[:, 0:n], scalar1=cvc(CV_LRU_CW + cc), scalar2=cvc(CV_LRU_CB + cc), op0=ALU.mult, op1=ALU.add)
                for k in range(1, 5):
                    op("dve", "scalar_tensor_tensor", out=xc[:, tok0:tok0 + n], in0=r[:, k:k + n], scalar=cvc(CV_LRU_CW + k * 8 + cc),
                       in1=xc[:, tok0:tok0 + n], op0=ALU.mult, op1=ALU.add)
            op("pool", "tensor_copy", out=xcb[:], in_=xc[:])
            for ax in range(2):
                for d in range(2):
                    dma("pool", wa[:, ax, d, :], lru_bd[ax, d, cc])
            for d in range(2):
                if d == 0:
                    order = spans
                else:
                    order = spans[:nctx_sp][::-1] + spans[nctx_sp:][::-1]
                hh = hf if d == 0 else hb
                for si, (s0, n) in enumerate(order):
                    for q0 in range(0, n, 512):
                        nq = min(512, n - q0)
                        op("pe", "matmul", pr[:, q0:q0 + nq], wa[:, 0, d, :], xcb[:, s0 + q0:s0 + q0 + nq], start=True, stop=True)
                        op("pe", "matmul", pi[:, q0:q0 + nq], wa[:, 1, d, :], xcb[:, s0 + q0:s0 + q0 + nq], start=True, stop=True)
                    op("act", "activation", out=rb[:, 0:n], in_=pr[:, 0:n], func=AF.Sigmoid, bias=cvc(CV_BA + d * 8 + cc))
                    op("act", "activation", out=ib[:, 0:n], in_=pi[:, 0:n], func=AF.Sigmoid, bias=cvc(CV_BX + d * 8 + cc))
                    op("act", "activation", out=sb2[:, 0:n], in_=rb[:, 0:n], func=AF.Exp, scale=lb2[:, d * 8 + cc:d * 8 + cc + 1])
                    op("act", "activation", out=rb[:, 0:n], in_=rb[:, 0:n], func=AF.Exp, scale=lb[:, d * 8 + cc:d * 8 + cc + 1])
                    op("act", "activation", out=sb2[:, 0:n], in_=sb2[:, 0:n], func=AF.Sqrt, scale=-1.0, bias=1.0)
                    op("dve", "tensor_tensor", out=ib[:, 0:n], in0=ib[:, 0:n], in1=sb2[:, 0:n], op=ALU.mult)
                    op("dve", "tensor_tensor", out=ib[:, 0:n], in0=ib[:, 0:n], in1=xc[:, s0:s0 + n], op=ALU.mult)
                    if d == 0:
                        init = 0.0 if s0 == 0 else hf[:, s0 - 1:s0]
                        op("dve", "tensor_tensor_scan", out=hf[:, s0:s0 + n], data0=rb[:, 0:n], data1=ib[:, 0:n], initial=init, op0=ALU.mult, op1=ALU.add)
                    else:
                        if s0 + n == CTXN:
                            init = 0.0
                        elif s0 + n == T:
                            init = hb[:, 0:1]
                        else:
                            init = hb[:, s0 + n:s0 + n + 1]
                        op("dve", "tensor_tensor_scan", out=rev(hb[:, s0:s0 + n], n), data0=rev(rb[:, 0:n], n), data1=rev(ib[:, 0:n], n),
                           initial=init, op0=ALU.mult, op1=ALU.add)
            for (s0, n) in [(CTXN + c0, min(1024, L - c0)) for c0 in range(0, L, 1024)]:
                g = gt.next()
                dma("sp", g[:, 0:n], GATE[cc, :, s0:s0 + n])
                op("pool", "tensor_tensor", out=hf[:, s0:s0 + n], in0=hf[:, s0:s0 + n], in1=hb[:, s0:s0 + n], op=ALU.add)
                y = yo.next()
                op("dve", "tensor_tensor", out=y[:, 0:n], in0=hf[:, s0:s0 + n], in1=g[:, 0:n], op=ALU.mult)
                dma("act", YT[cc, :, s0:s0 + n], y[:, 0:n])
        S.barrier()
        sc.close()

    phase_consts()
    phase_wconv()
    phase_mod(0)
    G0 = Scope(nc, "l0")
    GL = {"dtall": G0.sb("dtall", [128, NT, 32], F32), "dAall": G0.sb("dAall", [128, NT, 32], F32)}
    phase_inproj(0, GL)
    phase_conv0()
    phase_ssd(GL)
    G0.close()
    phase_attn(0)
    phase_oproj(0)
    phase_moe(0)
    phase_mod(1)
    phase_inproj(1, None)
    phase_lru()
    phase_select()
    phase_attn_local()
    phase_oproj(1)
    phase_moe(1)
    S.barrier()
    G.close()
    S.close()
    return nc


def _host_consts(L):
    t = np.arange(128)
    tt, ii = np.meshgrid(t, t, indexing="ij")
    m = np.zeros((8, 128, 128), np.float32)
    m[0] = tt > ii
    m[1] = tt <= ii
    m[2] = ii >= tt
    m[3] = tt < ii
    m[4] = tt >= ii
    m[5] = tt >= ii
    m[6] = tt >= ii
    m[7] = tt <= ii
    rows = L // GRID_W
    row = np.repeat(np.arange(rows), GRID_W).astype(np.float32)
    col = np.tile(np.arange(GRID_W), rows).astype(np.float32)
    n_freq = 32
    inv = (10000.0 ** (-np.arange(n_freq, dtype=np.float32) / n_freq)).astype(np.float32)
    ang = np.concatenate([row[:, None] * inv, col[:, None] * inv], axis=-1).astype(np.float32)
    cos = np.cos(ang).astype(np.float32).T
    sin = np.sin(ang).astype(np.float32).T
    cos2 = np.concatenate([cos, cos], axis=0)
    sin2 = np.concatenate([-sin, sin], axis=0)
    return m, np.ascontiguousarray(cos2), np.ascontiguousarray(sin2)


def _swap_heads(w, nheads):
    w = w.reshape(w.shape[0], nheads, 2, 64)
    return np.ascontiguousarray(w[:, :, ::-1, :]).reshape(w.shape[0], nheads * 128)


def make_inputs(inp, b, NL, NCT):
    f = lambda a: np.ascontiguousarray(np.asarray(a, dtype=np.float32))
    L = NL * 128
    m = {}
    m["xin"] = f(np.concatenate([inp["ctx"][b], inp["x"][b]], axis=0))
    c16 = np.zeros((16, 128), np.float32)
    c16[0::2] = f(inp["c"][b]).reshape(8, 128)
    c16[1::2] = f(inp["c_ctx"]).reshape(8, 128)
    m["cv16"] = c16
    m["w_mod"] = f(inp["w_mod"])
    m["b_mod"] = f(inp["b_mod"])
    rv = np.zeros((1, RV_N), np.float32)
    rv[0, RV_GMIX:RV_GMIX + 2048] = f(inp["g_mix"]).reshape(-1)
    rv[0, RV_GFFN:RV_GFFN + 2048] = f(inp["g_ffn"]).reshape(-1)
    rv[0, RV_GFIN:RV_GFIN + 1024] = f(inp["g_final"])
    rv[0, RV_NORMG:RV_NORMG + 1024] = f(inp["ssd_norm_g"][0])
    rv[0, RV_SSD_D:RV_SSD_D + 16] = f(inp["ssd_d"][0])
    rv[0, RV_DTB:RV_DTB + 32] = f(inp["ssd_dt_bias"][0]).reshape(-1)
    rv[0, RV_ALOG:RV_ALOG + 32] = f(inp["ssd_a_log"][0]).reshape(-1)
    for l in range(2):
        rv[0, RV_BROUTE + l * 20:RV_BROUTE + l * 20 + 4] = f(inp["moe_b_grp"][l])
        rv[0, RV_BROUTE + l * 20 + 4:RV_BROUTE + l * 20 + 20] = f(inp["moe_b_rt"][l])
    rv[0, RV_SINK:RV_SINK + 8] = f(inp["swa_sink"][0])
    m["rowv"] = rv
    cvv = np.zeros((CV_N, 128), np.float32)
    cvv[CV_SSD_CW:CV_SSD_CW + 50] = f(inp["ssd_conv_w"][0]).reshape(5, 10, 128).reshape(50, 128)
    cvv[CV_SSD_CB:CV_SSD_CB + 10] = f(inp["ssd_conv_b"][0]).reshape(10, 128)
    qg = f(inp["att_q_g"][0])
    kg = f(inp["att_k_g"][0])
    cvv[CV_QG] = qg
    cvv[CV_KG] = kg
    cvv[CV_QGS] = np.concatenate([qg[64:], qg[:64]])
    cvv[CV_KGS] = np.concatenate([kg[64:], kg[:64]])
    cvv[CV_LRU_CW:CV_LRU_CW + 40] = f(inp["lru_conv_w"][0]).reshape(5, 8, 128).reshape(40, 128)
    cvv[CV_LRU_CB:CV_LRU_CB + 8] = f(inp["lru_conv_b"][0]).reshape(8, 128)
    cvv[CV_BA:CV_BA + 16] = f(inp["lru_b_a"][0]).reshape(16, 128)
    cvv[CV_BX:CV_BX + 16] = f(inp["lru_b_x"][0]).reshape(16, 128)
    cvv[CV_LAM:CV_LAM + 16] = f(inp["lru_lam"][0]).reshape(16, 128)
    m["colv"] = cvv
    cm, cos2, sin2 = _host_consts(L)
    m["cmask"] = cm
    m["cos2"] = cos2
    m["sin2"] = sin2
    ab = f(inp["ab_w_in"][0])
    cd = f(inp["cd_w_in"][0])
    m["ab_w_in"] = ab
    m["cd_w_in"] = cd
    m["ab_w_sw"] = np.concatenate([_swap_heads(ab[:, 2336:3360], 8), _swap_heads(ab[:, 3360:3616], 2)], axis=1)
    m["cd_w_sw"] = np.concatenate([_swap_heads(cd[:, 2048:3072], 8), _swap_heads(cd[:, 3072:3328], 2)], axis=1)
    m["ab_w_out"] = f(inp["ab_w_out"][0])
    m["cd_w_out"] = f(inp["cd_w_out"][0])
    bd = np.zeros((2, 2, 8, 128, 128), np.float32)
    for ax, key in enumerate(("lru_w_a", "lru_w_x")):
        w = f(inp[key][0])
        for d in range(2):
            for cc in range(8):
                bd[ax, d, cc, 0:64, 0:64] = w[d, 2 * cc]
                bd[ax, d, cc, 64:128, 64:128] = w[d, 2 * cc + 1]
    m["lru_bd"] = bd
    m["moe_wr"] = np.ascontiguousarray(np.concatenate([f(inp["moe_w_grp"]), f(inp["moe_w_rt"])], axis=-1))
    m["moe_w1"] = f(inp["moe_w1"])
    m["moe_w3"] = f(inp["moe_w3"])
    m["moe_w2"] = f(inp["moe_w2"])
    return m


def rank_flags(r):
    fl = np.zeros((1, 8), np.float32)
    fl[0, r] = 1.0
    fl[0, 4] = 1.0 if r > 0 else 0.0
    fl[0, 5] = 1.0 if r < 3 else 0.0
    return fl


_NC_CACHE = {}


def kernel(**inputs):
    B, L, _ = inputs["x"].shape
    NL = L // 128
    NCT = inputs["ctx"].shape[1] // 128
    key = (NL, NCT)
    if key not in _NC_CACHE:
        _NC_CACHE[key] = build(NL, NCT)
    nc = _NC_CACHE[key]
    maps = [make_inputs(inputs, b, NL, NCT) for b in range(B)]
    in_maps = []
    for i in range(8):
        m = dict(maps[(i // 4) % B])
        m["rflag"] = rank_flags(i % 4)
        in_maps.append(m)
    res = run_bass_kernel_spmd(nc, in_maps, core_ids=list(range(8)))
    LQ = L // 4
    full = np.zeros((B, L, D), np.float32)
    for i in range(8):
        b, r = (i // 4) % B, i % 4
        if i // 4 < B:
            full[b, r * LQ:(r + 1) * LQ] = np.asarray(res.results[i]["out"], dtype=np.float32)
    return full
```

```python
from contextlib import ExitStack
import numpy as np
import concourse.bass as bass
import concourse.mybir as mybir
from concourse.bass_utils import run_bass_kernel_spmd

F32 = mybir.dt.float32
BF16 = mybir.dt.bfloat16
ALU = mybir.AluOpType
AF = mybir.ActivationFunctionType
AX = mybir.AxisListType

D = 1024
EPS = 1e-6
ATT_SCALE = 128 ** -0.5
GRID_W = 64
BIG = 1.0e9


class _Res:
    __slots__ = ("lw", "rd")

    def __init__(self):
        self.lw = None
        self.rd = {}


class _TL:
    def __init__(self, name, sem, unit):
        self.name = name
        self.sem = sem
        self.unit = unit
        self.cnt = 0
        self.vc = {}
        self.snaps = {}


class AutoSync:
    def __init__(self, nc, n_dma_sems=8):
        self.nc = nc
        self.res = {}
        self.tl = {}
        self._stack = []
        for name in ("pe", "dve", "act", "pool", "sp"):
            cm = nc.semaphore("s_" + name)
            sem = cm.__enter__()
            self._stack.append(cm)
            self.tl[name] = _TL(name, sem, 1)
        self.eng = {"pe": nc.tensor, "dve": nc.vector, "act": nc.scalar, "pool": nc.gpsimd, "sp": nc.sync}
        self.dq = {}
        for q in ("sp", "act", "pool"):
            lst = []
            for i in range(n_dma_sems):
                cm = nc.semaphore("d_%s%d" % (q, i))
                sem = cm.__enter__()
                self._stack.append(cm)
                t = _TL("d_%s%d" % (q, i), sem, 16)
                self.tl[t.name] = t
                lst.append(t)
            self.dq[q] = [lst, 0]
        self.n_wait = 0
        self.n_inst = 0

    def close(self):
        for cm in reversed(self._stack):
            cm.__exit__(None, None, None)

    def _key(self, ap):
        t = ap.tensor
        if type(t).__name__.startswith("DRam"):
            return None
        return t.name

    def _r(self, key):
        r = self.res.get(key)
        if r is None:
            r = self.res[key] = _Res()
        return r

    def _need(self, reads, writes):
        deps = {}

        def add(t, v):
            if deps.get(t, 0) < v:
                deps[t] = v

        for k in reads:
            lw = self._r(k).lw
            if lw:
                add(*lw)
        for k in writes:
            r = self._r(k)
            if r.lw:
                add(*r.lw)
            for t, v in r.rd.items():
                add(t, v)
        return deps

    def _wait(self, E, deps):
        T = self.tl[E]
        for t, v in deps.items():
            if E == "pe" and t == "pe":
                continue
            if T.vc.get(t, 0) >= v:
                continue
            tl = self.tl[t]
            self.eng[E].wait_ge(tl.sem, v * tl.unit)
            self.n_wait += 1
            snap = tl.snaps.get(v)
            if snap:
                for a, b in snap.items():
                    if T.vc.get(a, 0) < b:
                        T.vc[a] = b
            if T.vc.get(t, 0) < v:
                T.vc[t] = v

    def _mark(self, ev, reads, writes):
        t, v = ev
        for k in reads:
            r = self._r(k)
            if r.rd.get(t, 0) < v:
                r.rd[t] = v
        for k in writes:
            r = self._r(k)
            r.lw = ev
            r.rd = {}

    def _classify(self, args, kwargs):
        reads, writes = [], []
        first = True
        for a in args:
            if isinstance(a, bass.AP):
                k = self._key(a)
                if k is not None:
                    (writes if first else reads).append(k)
            first = False
        for n, a in kwargs.items():
            if isinstance(a, bass.AP):
                k = self._key(a)
                if k is not None:
                    (writes if n in ("out", "accum_out") else reads).append(k)
        return reads, writes

    def op(self, E, name, *args, **kwargs):
        reads, writes = self._classify(args, kwargs)
        self._wait(E, self._need(reads, writes))
        inst = getattr(self.eng[E], name)(*args, **kwargs)
        T = self.tl[E]
        T.cnt += 1
        inst.then_inc(T.sem, 1)
        T.snaps[T.cnt] = dict(T.vc)
        self._mark((E, T.cnt), reads, writes)
        self.n_inst += 1
        return inst

    def dma(self, Q, out, in_, **kwargs):
        reads = [k for k in [self._key(in_)] if k is not None]
        writes = [k for k in [self._key(out)] if k is not None]
        self._wait(Q, self._need(reads, writes))
        lst, idx = self.dq[Q]
        tl = lst[idx % len(lst)]
        self.dq[Q][1] = idx + 1
        if tl.cnt > 0:
            self._wait(Q, {tl.name: tl.cnt})
        inst = self.eng[Q].dma_start(out=out, in_=in_, **kwargs)
        tl.cnt += 1
        inst.then_inc(tl.sem, 16)
        tl.snaps[tl.cnt] = dict(self.tl[Q].vc)
        self._mark((tl.name, tl.cnt), reads, writes)
        self.n_inst += 1
        return inst

    def barrier(self):
        now = {t: tl.cnt for t, tl in self.tl.items() if tl.cnt > 0}
        for E in ("sp", "act", "pool", "dve", "pe"):
            self._wait(E, {t: v for t, v in now.items() if t != E})
            if E != "pe" and now.get(E, 0) > 0:
                self._wait(E, {E: now[E]})


class Scope:
    _n = 0

    def __init__(self, nc, tag):
        self.nc = nc
        self.es = ExitStack()
        Scope._n += 1
        self.tag = "%s%d_" % (tag, Scope._n)

    def sb(self, name, shape, dt):
        return self.es.enter_context(self.nc.sbuf_tensor(self.tag + name, list(shape), dt))

    def ps(self, name, shape, dt=F32):
        return self.es.enter_context(self.nc.psum_tensor(self.tag + name, list(shape), dt))

    def close(self):
        self.es.close()


class Rot:
    def __init__(self, items):
        self.items = items
        self.i = 0

    def next(self):
        x = self.items[self.i % len(self.items)]
        self.i += 1
        return x


def rev(ap2, n):
    return bass.AP(ap2.tensor, ap2.offset + (n - 1), [[ap2.ap[0][0], ap2.ap[0][1]], [-1, n]])


def bc(ap, axis, shape):
    return ap.unsqueeze(axis).to_broadcast(list(shape))


CV_SSD_CW = 0
CV_SSD_CB = 50
CV_QG = 60
CV_KG = 61
CV_QGS = 62
CV_KGS = 63
CV_LRU_CW = 64
CV_LRU_CB = 104
CV_BA = 112
CV_BX = 128
CV_LAM = 144
CV_N = 160
RV_GMIX = 0
RV_GFFN = 2048
RV_GFIN = 4096
RV_NORMG = 5120
RV_SSD_D = 6144
RV_DTB = 6160
RV_ALOG = 6192
RV_BROUTE = 6224
RV_SINK = 6264
RV_N = 6272


def build(NL, NCT, dbg=False):
    nc = bass.Bass("TRN2", target_bir_lowering=False)
    NT = NL + NCT
    T = NT * 128
    L = NL * 128
    CTXN = NCT * 128
    W = T + 8
    COL_CTX = 2
    COL_LAT = CTXN + 6

    def din(name, shape, dt=F32):
        return nc.dram_tensor(name, list(shape), dt, kind="ExternalInput").ap()

    def dsc(name, shape, dt):
        return nc.dram_tensor(name, list(shape), dt, kind="ExternalOutput" if dbg else "Internal").ap()

    xin = din("xin", [T, D])
    cv16 = din("cv16", [16, 128])
    w_mod = din("w_mod", [2, D, 6 * D])
    b_mod = din("b_mod", [2, 6 * D])
    rowv = din("rowv", [1, RV_N])
    colv = din("colv", [CV_N, 128])
    cmask = din("cmask", [8, 128, 128])
    cos2 = din("cos2", [128, L])
    sin2 = din("sin2", [128, L])
    w_in = [din("ab_w_in", [D, 3872]), din("cd_w_in", [D, 3584])]
    w_sw = [din("ab_w_sw", [D, 1280]), din("cd_w_sw", [D, 1280])]
    w_out = [din("ab_w_out", [2048, D]), din("cd_w_out", [2048, D])]
    lru_bd = din("lru_bd", [2, 2, 8, 128, 128])
    moe_wr = din("moe_wr", [2, D, 20])
    moe_w1 = din("moe_w1", [2, 16, D, 256])
    moe_w3 = din("moe_w3", [2, 16, D, 256])
    moe_w2 = din("moe_w2", [2, 16, 256, D])
    rflag = din("rflag", [1, 8])
    out = nc.dram_tensor("out", [NL // 4 * 128, D], F32, kind="ExternalOutput").ap()

    XRES = dsc("XRES", [T, D], F32)
    MODV = dsc("MODV", [2, 2, 6 * D], F32)
    XBC = dsc("XBC", [1280, W], F32)
    XS = dsc("XS", [T, D], BF16)
    ZT = dsc("ZT", [T, D], BF16)
    BTOK = dsc("BTOK", [T, 128], BF16)
    BT = dsc("BT", [2, 64, T], BF16)
    CTs = dsc("CT", [2, 64, T], BF16)
    QT = dsc("QT", [8, 128, T], BF16)
    KT = dsc("KT", [2, 128, T + 128], BF16)
    VT = dsc("VT", [T + 128, 256], BF16)
    NLQ = NL // 4
    LQ = NLQ * 128
    QTq = dsc("QTq", [8, 128, LQ], BF16)
    KTq = dsc("KTq", [2, 128, LQ + 256], BF16)
    VTq = dsc("VTq", [LQ + 256, 256], BF16)
    YTL = dsc("YTL", [16, 128, LQ], BF16)
    Xq = dsc("Xq", [LQ, D], F32)
    XQ1 = dsc("XQ1", [LQ, D], F32)
    HTFq = dsc("HTFq", [8, 128, LQ], BF16)
    YF = dsc("YF", [T, D], F32)
    YT = dsc("YT", [16, 128, T], BF16)
    HTF = dsc("HTF", [8, 128, T], BF16)
    GATE = dsc("GATE", [8, 128, T], BF16)
    W1B = dsc("W1B", [2, 16, 128, 2048], BF16)
    W3B = dsc("W3B", [2, 16, 128, 2048], BF16)

    S = AutoSync(nc)
    op = S.op
    dma = S.dma

    G = Scope(nc, "g")
    ident_f = G.sb("identf", [128, 128], F32)
    ident_b = G.sb("identb", [128, 128], BF16)
    ones_b = G.sb("onesb", [128, 128], BF16)
    ones_f = G.sb("onesf", [128, 128], F32)
    masks = G.sb("masks", [128, 8, 128], F32)
    colT = G.sb("colT", [128, CV_N], F32)
    gates = G.sb("gates", [128, NT, 16], F32)

    def cvc(r):
        return colT[:, r:r + 1]

    MK_LF, MK_UF, MK_MF, MK_LB, MK_UB, MK_MB, MK_WP, MK_WN = range(8)

    blocks = [(0, NCT, True)]
    t0 = NCT
    while t0 < NT:
        n = min(4, NT - t0)
        blocks.append((t0, n, False))
        t0 += n

    def phase_consts():
        sc = Scope(nc, "c")
        cst = sc.sb("cst", [128, 2, 128], F32)
        ptr = sc.ps("ptr", [128, 256], F32)
        dma("sp", masks[:], cmask.rearrange("m p f -> p m f"))
        op("dve", "tensor_tensor", out=ident_f[:], in0=masks[:, MK_UF, :], in1=masks[:, MK_UB, :], op=ALU.mult)
        op("dve", "tensor_copy", out=ident_b[:], in_=ident_f[:])
        op("pool", "memset", ones_b[:], 1.0)
        op("pool", "memset", ones_f[:], 1.0)
        dma("sp", cst[:, 0, :], colv[0:128, :])
        dma("sp", cst[0:CV_N - 128, 1, :], colv[128:CV_N, :])
        op("pe", "transpose", ptr[:, 0:128], cst[:, 0, :], ident_f[:])
        op("pe", "transpose", ptr[:, 128:128 + (CV_N - 128)], cst[0:CV_N - 128, 1, :], ident_f[0:CV_N - 128, 0:CV_N - 128])
        op("dve", "tensor_copy", out=colT[:], in_=ptr[:, 0:CV_N])
        zpad = sc.sb("zpad", [128, 256], BF16)
        op("pool", "memset", zpad[:], 0.0)
        for g in range(2):
            dma("act", KT[g][:, T:T + 128], zpad[:, 0:128])
        dma("act", VT[T:T + 128, :], zpad[:])
        S.barrier()
        sc.close()

    def phase_mod(l):
        sc = Scope(nc, "m")
        c16 = sc.sb("c16", [16, 128], F32)
        cT = sc.sb("cT", [128, 16], F32)
        pc = sc.ps("pc", [128, 16], F32)
        wm = Rot([sc.sb("wm%d" % i, [128, 8, 512], F32) for i in range(2)])
        pm = Rot([sc.ps("pm%d" % i, [2, 512], F32) for i in range(2)])
        modrow = sc.sb("modrow", [2, 6 * D], F32)
        bm = sc.sb("bm", [2, 6 * D], F32)
        gm = sc.sb("gm", [2, 2, D], F32)
        res = sc.sb("res", [2, 6 * D], F32)
        dma("sp", c16[:], cv16)
        op("act", "activation", out=c16[:], in_=c16[:], func=AF.Silu)
        op("pe", "transpose", pc[:], c16[:], ident_f[0:16, 0:16])
        op("dve", "tensor_copy", out=cT[:], in_=pc[:])
        dma("sp", bm[:], b_mod[l:l + 1, :].to_broadcast([2, 6 * D]))
        dma("sp", gm[:, 0, :], rowv[:, RV_GMIX + l * D:RV_GMIX + (l + 1) * D].to_broadcast([2, D]))
        dma("sp", gm[:, 1, :], rowv[:, RV_GFFN + l * D:RV_GFFN + (l + 1) * D].to_broadcast([2, D]))
        for nb in range(12):
            w = wm.next()
            dma("sp", w[:], w_mod[l, :, nb * 512:(nb + 1) * 512].rearrange("(c p) n -> p c n", p=128))
            p = pm.next()
            for c in range(8):
                op("pe", "matmul", p[:], cT[:, 2 * c:2 * c + 2], w[:, c, :], start=(c == 0), stop=(c == 7))
            op("dve", "tensor_tensor", out=modrow[:, nb * 512:(nb + 1) * 512], in0=p[:], in1=bm[:, nb * 512:(nb + 1) * 512], op=ALU.add)
        for half in range(2):
            b = half * 3 * D
            op("dve", "scalar_tensor_tensor", out=res[:, b:b + D], in0=modrow[:, b + D:b + 2 * D], scalar=1.0,
               in1=gm[:, half, :], op0=ALU.add, op1=ALU.mult)
            op("dve", "tensor_copy", out=res[:, b + D:b + 2 * D], in_=modrow[:, b:b + D])
            op("dve", "tensor_copy", out=res[:, b + 2 * D:b + 3 * D], in_=modrow[:, b + 2 * D:b + 3 * D])
        dma("act", MODV[l], res[:])
        S.barrier()
        sc.close()

    def load_mod(sc, l, idx, name):
        tl = []
        for s in range(2):
            t = sc.sb("%s%d" % (name, s), [128, D], F32)
            dma("sp", t[:], MODV[l, s:s + 1, idx * D:(idx + 1) * D].to_broadcast([128, D]))
            tl.append(t)
        return tl

    def load_row(sc, off, n, name):
        t = sc.sb(name, [128, n], F32)
        dma("sp", t[:], rowv[:, off:off + n].to_broadcast([128, n]))
        return t

    def rstd_from_ss(sc_tiles, ss, n):
        t1, t2 = sc_tiles
        op("dve", "tensor_scalar", out=t1[:], in0=ss, scalar1=1.0 / n, scalar2=EPS, op0=ALU.mult, op1=ALU.add)
        op("act", "activation", out=t1[:], in_=t1[:], func=AF.Sqrt)
        op("dve", "reciprocal", out=t2[:], in_=t1[:])
        return t2

    def phase_wconv():
        sc = Scope(nc, "w")
        st = Rot([sc.sb("st%d" % i, [128, 2048], F32) for i in range(3)])
        sb_ = Rot([sc.sb("sb%d" % i, [128, 2048], BF16) for i in range(3)])
        i = 0
        for l in range(2):
            for e in range(16):
                for src, dst in ((moe_w1, W1B), (moe_w3, W3B)):
                    a = st.next()
                    b = sb_.next()
                    dma("sp", a[:].rearrange("p (c f) -> p c f", c=8), src[l, e].rearrange("(c p) f -> p c f", p=128))
                    op(("pool", "dve", "act")[i % 3], "tensor_copy" if i % 3 != 2 else "copy", out=b[:], in_=a[:])
                    dma("act", dst[l, e], b[:])
                    i += 1
        S.barrier()
        sc.close()

    def phase_inproj(l, GL):
        sc = Scope(nc, "a")
        NW = 3872 if l == 0 else 3584
        Wb = sc.sb("Wb", [128, 8, NW + 1280], BF16)
        for c in range(8):
            dma("pool", Wb[:, c, 0:NW], w_in[l][c * 128:(c + 1) * 128, :])
            dma("pool", Wb[:, c, NW:NW + 1280], w_sw[l][c * 128:(c + 1) * 128, :])
        gsc = load_mod(sc, l, 0, "gsc")
        sh = load_mod(sc, l, 1, "sh")
        xt = Rot([sc.sb("xt%d" % i, [128, D], F32) for i in range(2)])
        junk = sc.sb("junk", [128, D], BF16)
        ss = sc.sb("ss", [128, 1], F32)
        r1 = sc.sb("r1", [128, 1], F32)
        r2 = sc.sb("r2", [128, 1], F32)
        tmp = sc.sb("tmp", [128, D], F32)
        htok = Rot([sc.sb("htok%d" % i, [128, D], BF16) for i in range(2)])
        hTs = [sc.sb("hT%d" % i, [128, 8, 512], BF16) for i in range(2)]
        ptp = sc.ps("ptp", [128, 8, 128], BF16)
        pf = [sc.ps("pf%d" % i, [128, 512], F32) for i in range(4)]
        pss = sc.ps("pss", [128, 512], F32)
        pz = sc.ps("pz", [128, 1024], F32)
        pdv = pss
        stg = Rot([sc.sb("stg%d" % i, [128, 512], F32) for i in range(2)])
        stb = Rot([sc.sb("stb%d" % i, [128, 512], BF16) for i in range(2)])
        zst = Rot([sc.sb("zst%d" % i, [128, D], BF16) for i in range(2)])
        vst = Rot([sc.sb("vst%d" % i, [128, 256], BF16) for i in range(2)])
        sq = sc.sb("sq", [128, 512], BF16)
        rs1 = sc.sb("rs1", [128, 512], F32)
        rs2 = sc.sb("rs2", [128, 512], F32)
        qn = sc.sb("qn", [128, 512], F32)
        qs = sc.sb("qs", [128, 512], F32)
        cosb = sc.sb("cosb", [128, 512], F32)
        sinb = sc.sb("sinb", [128, 512], F32)
        if l == 0:
            dtb = load_row(sc, RV_DTB, 32, "dtb")
            alog = load_row(sc, RV_ALOG, 32, "alog")
            aneg = sc.sb("aneg", [128, 32], F32)
            op("act", "activation", out=aneg[:], in_=alog[:], func=AF.Exp)
            op("dve", "tensor_scalar", out=aneg[:], in0=aneg[:], scalar1=-1.0, scalar2=None, op0=ALU.mult)
            d1 = sc.sb("d1", [128, 32], F32)
            d2 = sc.sb("d2", [128, 32], F32)
            d3 = sc.sb("d3", [128, 32], F32)
        if l == 0:
            zc = sc.sb("zc", [128, 4], F32)
            op("pool", "memset", zc[:], 0.0)
            for r0 in range(0, 1280, 128):
                dma("act", XBC[r0:r0 + 128, 0:2], zc[:, 0:2])
                dma("act", XBC[r0:r0 + 128, CTXN + 2:CTXN + 6], zc[:, 0:4])
                dma("act", XBC[r0:r0 + 128, W - 2:W], zc[:, 0:2])
        xsrc = xin if l == 0 else XRES
        if l == 0:
            fm = [("raw", 1024 + 128 * i, i) for i in range(10)]
            qcol, kcol, vcol = 2336, 3360, 3616
        else:
            fm = [("gelu", 128 * i, i) for i in range(8)] + [("raw", 1024 + 128 * i, i) for i in range(8)]
            qcol, kcol, vcol = 2048, 3072, 3328
        def prologue(bi):
            tile0, n, is_ctx = blocks[bi]
            hT = hTs[bi % 2]
            s = 1 if is_ctx else 0
            for j in range(n):
                x = xt.next()
                dma("sp", x[:], xsrc[(tile0 + j) * 128:(tile0 + j + 1) * 128, :])
                op("act", "activation", out=junk[:], in_=x[:], func=AF.Square, accum_out=ss[:])
                rstd = rstd_from_ss((r1, r2), ss[:], D)
                op("dve", "scalar_tensor_tensor", out=tmp[:], in0=x[:], scalar=rstd[:, 0:1], in1=gsc[s][:], op0=ALU.mult, op1=ALU.mult)
                h = htok.next()
                op("pool", "tensor_tensor", out=h[:], in0=tmp[:], in1=sh[s][:], op=ALU.add)
                for c in range(8):
                    op("pe", "transpose", ptp[:, c, :], h[:, c * 128:(c + 1) * 128], ident_b[:])
                op("act", "copy", out=hT[:, :, j * 128:(j + 1) * 128], in_=ptp[:])

        def body(bi):
            tile0, n, is_ctx = blocks[bi]
            hT = hTs[bi % 2]
            NB = n * 128
            s = 1 if is_ctx else 0
            tok0 = tile0 * 128
            if not is_ctx:
                lt0 = tok0 - CTXN
                dma("sp", cosb[:, 0:NB], cos2[:, lt0:lt0 + NB])
                dma("sp", sinb[:, 0:NB], sin2[:, lt0:lt0 + NB])
            colbase = (COL_CTX + tok0) if is_ctx else (COL_LAT + tok0 - CTXN)

            def mm_fm(p, col):
                for c in range(8):
                    op("pe", "matmul", p[:, 0:NB], Wb[:, c, col:col + 128], hT[:, c, 0:NB], start=(c == 0), stop=(c == 7))

            for (kind, col, idx) in fm:
                p = pf[idx % 4]
                mm_fm(p, col)
                if kind == "raw":
                    st = stg.next()
                    op("act", "copy", out=st[:, 0:NB], in_=p[:, 0:NB])
                    dma("act", XBC[idx * 128:(idx + 1) * 128, colbase:colbase + NB], st[:, 0:NB])
                else:
                    st = stb.next()
                    op("act", "activation", out=st[:, 0:NB], in_=p[:, 0:NB], func=AF.Gelu)
                    dma("act", GATE[idx, :, tok0:tok0 + NB], st[:, 0:NB])
            for hh in range(10):
                isq = hh < 8
                if l == 1 and is_ctx and isq:
                    continue
                col = (qcol + hh * 128) if isq else (kcol + (hh - 8) * 128)
                cols = NW + hh * 128
                pq, pw = (pf[0], pf[1]) if hh % 2 == 0 else (pf[2], pf[3])
                mm_fm(pq, col)
                mm_fm(pw, cols)
                if l == 0:
                    op("act", "activation", out=sq[:, 0:NB], in_=pq[:, 0:NB], func=AF.Square)
                    op("pe", "matmul", pss[:, 0:NB], ones_b[:], sq[:, 0:NB], start=True, stop=True)
                    op("act", "activation", out=rs1[:, 0:NB], in_=pss[:, 0:NB], func=AF.Sqrt, scale=1.0 / 128, bias=EPS)
                    op("dve", "reciprocal", out=rs2[:, 0:NB], in_=rs1[:, 0:NB])
                    g_ = cvc(CV_QG if isq else CV_KG)
                    gs_ = cvc(CV_QGS if isq else CV_KGS)
                    op("dve", "scalar_tensor_tensor", out=qn[:, 0:NB], in0=pq[:, 0:NB], scalar=g_, in1=rs2[:, 0:NB], op0=ALU.mult, op1=ALU.mult)
                    op("dve", "scalar_tensor_tensor", out=qs[:, 0:NB], in0=pw[:, 0:NB], scalar=gs_, in1=rs2[:, 0:NB], op0=ALU.mult, op1=ALU.mult)
                    a_n, a_s = qn, qs
                    e1, e2 = "pool", "pool"
                else:
                    a_n, a_s = pq, pw
                    e1, e2 = "dve", "dve"
                st = stb.next()
                if is_ctx:
                    op("act", "copy", out=st[:, 0:NB], in_=a_n[:, 0:NB])
                else:
                    o1, o2 = (qn, qs) if l == 0 else (rs1, rs2)
                    op(e1, "tensor_tensor", out=o1[:, 0:NB], in0=a_n[:, 0:NB], in1=cosb[:, 0:NB], op=ALU.mult)
                    op(e2, "tensor_tensor", out=o2[:, 0:NB], in0=a_s[:, 0:NB], in1=sinb[:, 0:NB], op=ALU.mult)
                    op("dve" if l == 0 else "pool", "tensor_tensor", out=st[:, 0:NB], in0=o1[:, 0:NB], in1=o2[:, 0:NB], op=ALU.add)
                dst = QT[hh] if isq else KT[hh - 8]
                dma("act", dst[:, tok0:tok0 + NB], st[:, 0:NB])
            for j in range(n):
                ti = tile0 + j
                if l == 0:
                    for half in range(2):
                        for c in range(8):
                            op("pe", "matmul", pz[:, half * 512:(half + 1) * 512], hT[:, c, j * 128:(j + 1) * 128],
                               Wb[:, c, half * 512:(half + 1) * 512], start=(c == 0), stop=(c == 7))
                    z = zst.next()
                    op("act", "activation", out=z[:], in_=pz[:], func=AF.Silu)
                    dma("act", ZT[ti * 128:(ti + 1) * 128, :], z[:])
                    for c in range(8):
                        op("pe", "matmul", pdv[:, 0:32], hT[:, c, j * 128:(j + 1) * 128], Wb[:, c, 2304:2336], start=(c == 0), stop=(c == 7))
                    op("dve", "tensor_tensor", out=d1[:], in0=pdv[:, 0:32], in1=dtb[:], op=ALU.add)
                    op("dve", "scalar_tensor_tensor", out=d2[:], in0=d1[:], scalar=-1.0, in1=d1[:], op0=ALU.mult, op1=ALU.max)
                    op("act", "activation", out=d2[:], in_=d2[:], func=AF.Exp, scale=-1.0)
                    op("act", "activation", out=d3[:], in_=d2[:], func=AF.Ln, bias=1.0)
                    op("dve", "scalar_tensor_tensor", out=GL["dtall"][:, ti, :], in0=d1[:], scalar=0.0, in1=d3[:], op0=ALU.max, op1=ALU.add)
                    op("dve", "tensor_tensor", out=GL["dAall"][:, ti, :], in0=GL["dtall"][:, ti, :], in1=aneg[:], op=ALU.mult)
                for c in range(8):
                    op("pe", "matmul", pdv[:, 256:512], hT[:, c, j * 128:(j + 1) * 128], Wb[:, c, vcol:vcol + 256], start=(c == 0), stop=(c == 7))
                v = vst.next()
                op("act", "copy", out=v[:], in_=pdv[:, 256:512])
                dma("act", VT[ti * 128:(ti + 1) * 128, :], v[:])
        prologue(0)
        for bi in range(len(blocks)):
            if bi + 1 < len(blocks):
                prologue(bi + 1)
            body(bi)
        S.barrier()
        sc.close()

    def conv_spans(maxn):
        sp = []
        for c0 in range(0, CTXN, maxn):
            n = min(maxn, CTXN - c0)
            sp.append((COL_CTX + c0, n, c0))
        for c0 in range(0, L, maxn):
            n = min(maxn, L - c0)
            sp.append((COL_LAT + c0, n, CTXN + c0))
        return sp

    def conv5(raw, acc, n, wrow0, stride, cc, brow):
        op("dve", "tensor_scalar", out=acc[:, 0:n], in0=raw[:, 0:n], scalar1=cvc(wrow0 + cc), scalar2=cvc(brow + cc), op0=ALU.mult, op1=ALU.add)
        for k in range(1, 5):
            op("dve", "scalar_tensor_tensor", out=acc[:, 0:n], in0=raw[:, k:k + n], scalar=cvc(wrow0 + k * stride + cc), in1=acc[:, 0:n], op0=ALU.mult, op1=ALU.add)

    def phase_conv0():
        sc = Scope(nc, "v")
        raw = Rot([sc.sb("raw%d" % i, [128, 1028], F32) for i in range(2)])
        acc = Rot([sc.sb("acc%d" % i, [128, 1024], F32) for i in range(2)])
        act = Rot([sc.sb("act%d" % i, [128, 1024], BF16) for i in range(2)])
        ptp = Rot([sc.ps("ptp%d" % i, [128, 4, 128], BF16) for i in range(2)])
        xst = Rot([sc.sb("xst%d" % i, [128, 4, 128], BF16) for i in range(3)])
        for cc in range(10):
            for (c0, n, tok0) in conv_spans(1024):
                r = raw.next()
                dma("sp", r[:, 0:n + 4], XBC[cc * 128:(cc + 1) * 128, c0 - 2:c0 + n + 2])
                a = acc.next()
                conv5(r, a, n, CV_SSD_CW, 10, cc, CV_SSD_CB)
                b = act.next()
                op("act", "activation", out=b[:, 0:n], in_=a[:, 0:n], func=AF.Silu)
                if cc >= 8:
                    dst = BT if cc == 8 else CTs
                    dma("act", dst[0, :, tok0:tok0 + n], b[0:64, 0:n])
                    dma("act", dst[1, :, tok0:tok0 + n], b[64:128, 0:n])
                if cc <= 8:
                    for j0 in range(0, n // 128, 4):
                        nj = min(4, n // 128 - j0)
                        p = ptp.next()
                        for j in range(nj):
                            op("pe", "transpose", p[:, j, :], b[:, (j0 + j) * 128:(j0 + j + 1) * 128], ident_b[:])
                        x = xst.next()
                        if (j0 // 4) % 2:
                            op("act", "copy", out=x[:, 0:nj, :], in_=p[:, 0:nj, :])
                        else:
                            op("dve", "tensor_copy", out=x[:, 0:nj, :], in_=p[:, 0:nj, :])
                        r0 = tok0 + j0 * 128
                        if cc < 8:
                            dma("act", XS[r0:r0 + nj * 128, cc * 128:(cc + 1) * 128].rearrange("(j p) c -> p j c", p=128), x[:, 0:nj, :])
                        else:
                            dma("act", BTOK[r0:r0 + nj * 128, :].rearrange("(j p) c -> p j c", p=128), x[:, 0:nj, :])
        S.barrier()
        sc.close()

    def phase_ssd(GL):
        sc = Scope(nc, "s")
        dtall, dAall = GL["dtall"], GL["dAall"]
        dsk = load_row(sc, RV_SSD_D, 16, "dsk")
        normg = load_row(sc, RV_NORMG, D, "normg")
        Hf = sc.sb("Hf", [64, 2, 512], F32)
        Hb = sc.sb("Hb", [64, 2, 512], BF16)
        tmpH = sc.sb("tmpH", [64, 2, 512], F32)
        xs_ = Rot([sc.sb("xs%d" % i, [128, D], BF16) for i in range(3)])
        btok_ = Rot([sc.sb("btok%d" % i, [128, 128], BF16) for i in range(3)])
        bt_ = Rot([sc.sb("bt%d" % i, [64, 2, 128], BF16) for i in range(3)])
        ct_ = Rot([sc.sb("ct%d" % i, [64, 2, 128], BF16) for i in range(3)])
        psm_ = Rot([sc.ps("psm", [128, 512], F32)])
        psm = psm_.items[0]
        pcb = psm[:, 0:256].rearrange("p (g i) -> p g i", g=2)
        pac = psm[:, 256:288]
        ptb = sc.ps("ptb", [128, 8, 128], BF16)
        pseg = sc.ps("pseg", [128, 1024], F32)
        pyd = sc.ps("pyd", [128, 1024], F32)
        pch = sc.ps("pch", [128, 1024], F32)
        cbm_ = Rot([sc.sb("cbm%d" % i, [128, 2, 128], F32) for i in range(3)])
        Z_ = Rot([sc.sb("Z%d" % i, [128, 16, 128], F32) for i in range(2)])
        e__ = Rot([sc.sb("e%d" % i, [128, 16, 128], F32) for i in range(3)])
        wT_ = Rot([sc.sb("wT%d" % i, [128, 16, 128], BF16) for i in range(2)])
        sm_ = Rot([tuple(sc.sb("sm%s%d" % (nm, i), [128, w], F32) for nm, w in (("a", 32), ("e", 16), ("c", 16), ("t", 16))) for i in range(3)])
        y1_ = Rot([sc.sb("y1%d" % i, [128, D], F32) for i in range(2)])
        y2 = Rot([sc.sb("y2%d" % i, [128, D], F32) for i in range(2)])
        xw_ = Rot([sc.sb("xw%d" % i, [128, D], BF16) for i in range(3)])
        yf_ = Rot([sc.sb("yf%d" % i, [128, D], F32) for i in range(2)])
        zs_ = Rot([sc.sb("zs%d" % i, [128, D], BF16) for i in range(2)])
        ss = sc.sb("ss", [128, 1], F32)
        r1 = sc.sb("r1", [128, 1], F32)
        r2 = sc.sb("r2", [128, 1], F32)
        junk = sc.sb("junk", [128, D], BF16)
        ytok = sc.sb("ytok", [128, D], BF16)
        yst = Rot([sc.sb("yst%d" % i, [128, 8, 128], BF16) for i in range(2)])

        Ssb_ = Rot([sc.sb("Ssb%d" % i, [64, 2, 512], F32) for i in range(2)])

        def partA1(ti, d):
            mL, mU, mM = (MK_LF, MK_UF, MK_MF) if d == 0 else (MK_LB, MK_UB, MK_MB)
            r0 = ti * 128
            xs = xs_.next()
            dma("sp", xs[:], XS[r0:r0 + 128, :])
            btok = btok_.next()
            dma("sp", btok[:], BTOK[r0:r0 + 128, :])
            bt = bt_.next()
            dma("sp", bt[:], BT[:, :, r0:r0 + 128].rearrange("g n t -> n g t"))
            ct = ct_.next()
            dma("sp", ct[:], CTs[:, :, r0:r0 + 128].rearrange("g n t -> n g t"))
            dA = dAall[:, ti, d * 16:(d + 1) * 16]
            dt = dtall[:, ti, d * 16:(d + 1) * 16]
            cbm, Z, e_, sm, xw = cbm_.next(), Z_.next(), e__.next(), sm_.next(), xw_.next()
            acs, eacs, ecd, te = sm[0][:], sm[1][:], sm[2][:], sm[3][:]
            op("pool", "tensor_tensor", out=Z[:], in0=bc(masks[:, mU, :], 1, [128, 16, 128]), in1=bc(dA, 2, [128, 16, 128]), op=ALU.mult)
            op("pe", "matmul", pac[:, 0:16], masks[:, mU, :], dA, start=True, stop=True)
            op("pe", "matmul", pac[:, 16:32], ones_f[:], dA, start=True, stop=True)
            for g in range(2):
                op("pe", "matmul", pcb[:, g, :], bt[:, g, :], ct[:, g, :], start=True, stop=True)
            op("dve", "tensor_copy", out=acs, in_=pac)
            op("dve", "tensor_tensor", out=te, in0=acs[:, 16:32], in1=acs[:, 0:16], op=ALU.subtract)
            op("act", "activation", out=te, in_=te, func=AF.Exp)
            op("act", "activation", out=eacs, in_=acs[:, 0:16], func=AF.Exp)
            op("act", "activation", out=ecd, in_=acs[:, 16:32], func=AF.Exp)
            op("dve", "tensor_tensor", out=te, in0=te, in1=dt, op=ALU.mult)
            op("dve", "tensor_tensor", out=cbm[:], in0=pcb, in1=bc(masks[:, mM, :], 1, [128, 2, 128]), op=ALU.mult)
            op("pool", "tensor_tensor", out=xw[:].rearrange("p (h q) -> p h q", h=16), in0=xs[:].rearrange("p (h q) -> p h q", h=16),
               in1=bc(te, 2, [128, 16, 64]), op=ALU.mult)
            Zf = Z[:].rearrange("p h i -> p (h i)")
            ef = e_[:].rearrange("p h i -> p (h i)")
            for hf_ in range(2):
                for q in range(2):
                    c0 = hf_ * 1024 + q * 512
                    op("pe", "matmul", pseg[:, q * 512:(q + 1) * 512], masks[:, mL, :], Zf[:, c0:c0 + 512], start=True, stop=True)
                op("act", "activation", out=ef[:, hf_ * 1024:(hf_ + 1) * 1024], in_=pseg[:], func=AF.Exp)
            return dict(ti=ti, xs=xs, ct=ct, btok=btok, sm=sm, cbm=cbm, e=e_, xw=xw, dt=dt)

        def partA2(st):
            xs, btok, cbm, e_, xw, dt = st["xs"], st["btok"], st["cbm"], st["e"], st["xw"], st["dt"]
            wT, y1, Ssb = wT_.next(), y1_.next(), Ssb_.next()
            op("dve", "tensor_tensor", out=e_[:], in0=e_[:], in1=bc(dt, 2, [128, 16, 128]), op=ALU.mult)
            op("dve", "tensor_tensor", out=wT[:].rearrange("p (g h) i -> p g h i", g=2), in0=e_[:].rearrange("p (g h) i -> p g h i", g=2),
               in1=bc(cbm[:], 2, [128, 2, 8, 128]), op=ALU.mult)
            for h in range(16):
                op("pe", "matmul", pyd[:, h * 64:(h + 1) * 64], wT[:, h, :], xs[:, h * 64:(h + 1) * 64], start=True, stop=True)
            op("act", "copy", out=y1[:], in_=pyd[:])
            pS = pyd[0:64, 0:1024].rearrange("p (g n) -> p g n", g=2)
            for g in range(2):
                op("pe", "matmul", pS[:, g, :], btok[:, g * 64:(g + 1) * 64], xw[:, g * 512:(g + 1) * 512], start=True, stop=True)
            op("act", "copy", out=Ssb[:], in_=pS)
            st["y1"] = y1
            st["Ssb"] = Ssb
            return st

        def partB(st):
            ct, sm, y1, Ssb = st["ct"], st["sm"], st["y1"], st["Ssb"]
            eacs = sm[1][:]
            for g in range(2):
                op("pe", "matmul", pch[:, g * 512:(g + 1) * 512], ct[:, g, :], Hb[:, g, :], start=True, stop=True)
            op("dve", "tensor_tensor", out=tmpH[:].rearrange("p g (h q) -> p (g h) q", h=8), in0=Hf[:].rearrange("p g (h q) -> p (g h) q", h=8),
               in1=bc(sm[2][0:64, :], 2, [64, 16, 64]), op=ALU.mult)
            op("dve", "tensor_tensor", out=Hf[:], in0=tmpH[:], in1=Ssb[:], op=ALU.add)
            op("act", "copy", out=Hb[:], in_=Hf[:])
            y = y2.next()
            op("dve", "tensor_tensor", out=y[:].rearrange("p (h q) -> p h q", h=16), in0=pch[:].rearrange("p (h q) -> p h q", h=16),
               in1=bc(eacs, 2, [128, 16, 64]), op=ALU.mult)
            op("pool", "tensor_tensor", out=y[:], in0=y[:], in1=y1[:], op=ALU.add)
            return y

        def zero_state():
            op("pool", "memset", Hf[:], 0.0)
            op("pool", "memset", Hb[:], 0.0)

        def fin_fwd(st, y):
            ti = st["ti"]
            dma("act", YF[ti * 128:(ti + 1) * 128, :], y[:])

        def fin_bwd(st, y):
            ti, xs = st["ti"], st["xs"]
            r0 = ti * 128
            yf = yf_.next()
            dma("sp", yf[:], YF[r0:r0 + 128, :])
            zs = zs_.next()
            dma("sp", zs[:], ZT[r0:r0 + 128, :])
            op("pool", "tensor_tensor", out=y[:], in0=y[:], in1=yf[:], op=ALU.add)
            op("dve", "tensor_tensor", out=yf[:].rearrange("p (h q) -> p h q", h=16), in0=xs[:].rearrange("p (h q) -> p h q", h=16),
               in1=bc(dsk[:], 2, [128, 16, 64]), op=ALU.mult)
            op("pool", "tensor_tensor", out=y[:], in0=y[:], in1=yf[:], op=ALU.add)
            op("dve", "tensor_tensor", out=y[:], in0=y[:], in1=zs[:], op=ALU.mult)
            op("act", "activation", out=junk[:], in_=y[:], func=AF.Square, accum_out=ss[:])
            rstd = rstd_from_ss((r1, r2), ss[:], D)
            op("dve", "scalar_tensor_tensor", out=ytok[:], in0=y[:], scalar=rstd[:, 0:1], in1=normg[:], op0=ALU.mult, op1=ALU.mult)
            yT = yst.next()
            for c in range(8):
                op("pe", "transpose", ptb[:, c, :], ytok[:, c * 128:(c + 1) * 128], ident_b[:])
            op("act", "copy", out=yT[:], in_=ptb[:])
            dma("act", YT[0:8, :, r0:r0 + 128].rearrange("c p t -> p c t"), yT[:])

        def run(order, d, fin):
            zero_state()
            n = len(order)
            s1 = {}
            s2 = {}
            s1[0] = partA1(order[0], d)
            if n > 1:
                s1[1] = partA1(order[1], d)
            s2[0] = partA2(s1.pop(0))
            for k in range(n):
                st = s2.pop(k)
                y = partB(st)
                fin(st, y)
                if k + 1 < n:
                    s2[k + 1] = partA2(s1.pop(k + 1))
                if k + 2 < n:
                    s1[k + 2] = partA1(order[k + 2], d)

        run(list(range(NT)), 0, fin_fwd)
        S.barrier()
        run(list(range(NCT - 1, -1, -1)) + list(range(NT - 1, NCT - 1, -1)), 1, fin_bwd)
        S.barrier()
        sc.close()

    def phase_attn(l):
        sc = Scope(nc, "t")
        KTs = sc.sb("KTs", [128, 2, T], BF16)
        Vs = sc.sb("Vs", [128, NT, 256], BF16)
        for g in range(2):
            dma("sp", KTs[:, g, :], KT[g][:, 0:T])
        dma("sp", Vs[:], VT[0:T, :].rearrange("(n p) c -> p n c", p=128))
        q_ = Rot([sc.sb("q%d" % i, [128, 4, 128], BF16) for i in range(3)])
        ps_s = Rot([sc.ps("pss%d" % i, [128, 512], F32) for i in range(3)])
        pT_ = Rot([sc.sb("pT%d" % i, [128, 512], BF16) for i in range(4)])
        po_ = Rot([sc.ps("po%d" % i, [128, 512], F32) for i in range(2)])
        pm_ = Rot([sc.ps("pm%d" % i, [128, 512], F32) for i in range(2)])
        ac_ = Rot([sc.sb("ac%d" % i, [128, 512], F32) for i in range(2)])
        rs_ = Rot([sc.sb("rs%d" % i, [128, 512], F32) for i in range(2)])
        o_ = Rot([sc.sb("o%d" % i, [128, 4, 128], BF16) for i in range(2)])
        if l == 1:
            sk = load_row(sc, RV_SINK, 8, "sk")
            op("act", "activation", out=sk[:], in_=sk[:], func=AF.Exp)
        groups = []
        for ti in range(NT):
            is_ctx = ti < NCT
            if l == 1 and is_ctx:
                continue
            if l == 0:
                keys = [(k, None) for k in (range(NCT) if is_ctx else range(NT))]
            else:
                keys = [(k, None) for k in range(NCT)]
                if ti - 1 >= NCT:
                    keys.append((ti - 1, MK_WP))
                keys.append((ti, None))
                if ti + 1 < NT:
                    keys.append((ti + 1, MK_WN))
            for g in range(2):
                groups.append((ti, g, keys))
        items = []
        for gi, (ti, g, keys) in enumerate(groups):
            for i, (kt, mk) in enumerate(keys):
                items.append((gi, i, kt, mk, i == 0, i == len(keys) - 1))
        qbuf = {}

        def load_q(gi):
            if gi < len(groups) and gi not in qbuf:
                ti, g, _ = groups[gi]
                q = q_.next()
                dma("sp", q[:], QT[g * 4:(g + 1) * 4, :, ti * 128:(ti + 1) * 128].rearrange("h d t -> d h t"))
                qbuf[gi] = q

        def issue_S(n):
            gi, i, kt, mk, first, last = items[n]
            load_q(gi)
            if first:
                load_q(gi + 1)
            g = groups[gi][1]
            p = ps_s.next()
            op("pe", "matmul", p[:], KTs[:, g, kt * 128:(kt + 1) * 128], qbuf[gi][:].rearrange("d h t -> d (h t)"), start=True, stop=True)
            return p

        LOOK = 2
        pend = [issue_S(n) for n in range(min(LOOK, len(items)))]
        acc = {}
        for n, (gi, i, kt, mk, first, last) in enumerate(items):
            if n + LOOK < len(items):
                pend.append(issue_S(n + LOOK))
            p = pend.pop(0)
            ti, g, keys = groups[gi]
            if first:
                acc[gi] = (po_.next(), pm_.next(), ac_.next())
            po, pm, ac = acc[gi]
            pT = pT_.next()
            op("act", "activation", out=pT[:], in_=p[:], func=AF.Exp, scale=ATT_SCALE)
            if mk is not None:
                op("pool", "tensor_tensor", out=pT[:].rearrange("k (h t) -> k h t", h=4), in0=pT[:].rearrange("k (h t) -> k h t", h=4),
                   in1=bc(masks[:, mk, :], 1, [128, 4, 128]), op=ALU.mult)
            op("pe", "matmul", po[:], Vs[:, kt, g * 128:(g + 1) * 128], pT[:], start=first, stop=last)
            nk = len(keys)
            use_dve = (nk >= 4) and (i % 2 == 1)
            if use_dve:
                if i == 1:
                    op("dve", "tensor_copy", out=ac[:], in_=pT[:])
                else:
                    op("dve", "tensor_tensor", out=ac[:], in0=ac[:], in1=pT[:], op=ALU.add)
            else:
                op("pe", "matmul", pm[:], ones_b[:], pT[:], start=first, stop=(last and nk < 4))
            if last:
                if nk >= 4:
                    op("pe", "matmul", pm[:], ones_f[:], ac[:], start=False, stop=True)
                rs = rs_.next()
                if l == 1:
                    op("dve", "tensor_tensor", out=rs[:].rearrange("d (h t) -> d h t", h=4), in0=pm[:].rearrange("d (h t) -> d h t", h=4),
                       in1=bc(sk[:, g * 4:(g + 1) * 4], 2, [128, 4, 128]), op=ALU.add)
                    op("dve", "reciprocal", out=rs[:], in_=rs[:])
                else:
                    op("dve", "reciprocal", out=rs[:], in_=pm[:])
                o = o_.next()
                op("dve", "tensor_tensor", out=o[:].rearrange("d h t -> d (h t)"), in0=po[:], in1=rs[:], op=ALU.mult)
                dma("act", YT[8 + g * 4:8 + (g + 1) * 4, :, ti * 128:(ti + 1) * 128].rearrange("h d t -> d h t"), o[:])
                del acc[gi]
                qbuf.pop(gi, None)
        S.barrier()
        sc.close()

    def phase_select():
        sc = Scope(nc, "q")
        fl = sc.sb("fl", [128, 8], F32)
        dma("sp", fl[:], rflag.to_broadcast([128, 8]))
        zt = sc.sb("zt", [128, 256], BF16)
        cb_ = Rot([sc.sb("cb%d" % i, [128, LQ + 256], BF16) for i in range(3)])
        ab_ = Rot([sc.sb("ab%d" % i, [128, LQ + 256], BF16) for i in range(2)])
        cf_ = Rot([sc.sb("cf%d" % i, [128, D], F32) for i in range(3)])
        af_ = Rot([sc.sb("af%d" % i, [128, D], F32) for i in range(2)])
        cnt = [0]

        def sel(dst, cands, n, f32=False):
            acc = (af_ if f32 else ab_).next()
            e = "dve"
            cnt[0] += 1
            for j, c in enumerate(cands):
                t = (cf_ if f32 else cb_).next()
                dma("sp", t[:, 0:n], c)
                if j == 0:
                    op(e, "tensor_scalar", out=acc[:, 0:n], in0=t[:, 0:n], scalar1=fl[:, 0:1], scalar2=None, op0=ALU.mult)
                else:
                    op(e, "scalar_tensor_tensor", out=acc[:, 0:n], in0=t[:, 0:n], scalar=fl[:, j:j + 1], in1=acc[:, 0:n], op0=ALU.mult, op1=ALU.add)
            dma("act", dst, acc[:, 0:n])

        for h in range(8):
            sel(QTq[h], [QT[h][:, CTXN + j * LQ:CTXN + (j + 1) * LQ] for j in range(4)], LQ)
            sel(YTL[h], [YT[h][:, CTXN + j * LQ:CTXN + (j + 1) * LQ] for j in range(4)], LQ)
        for g in range(2):
            sel(KTq[g], [KT[g][:, CTXN + j * LQ - 128:CTXN + (j + 1) * LQ + 128] for j in range(4)], LQ + 256)
        for tl in range(NLQ + 2):
            sel(VTq[tl * 128:(tl + 1) * 128, :], [VT[CTXN + j * LQ - 128 + tl * 128:CTXN + j * LQ + tl * 128, :] for j in range(4)], 256)
        for tl in range(NLQ):
            sel(Xq[tl * 128:(tl + 1) * 128, :], [XRES[CTXN + j * LQ + tl * 128:CTXN + j * LQ + (tl + 1) * 128, :] for j in range(4)], D, f32=True)
        S.barrier()
        sc.close()

    def phase_attn_local():
        sc = Scope(nc, "u")
        fl = sc.sb("fl", [128, 8], F32)
        dma("sp", fl[:], rflag.to_broadcast([128, 8]))
        NE = NLQ + 2
        Kc = sc.sb("Kc", [128, 2, CTXN], BF16)
        Vc = sc.sb("Vc", [128, NCT, 256], BF16)
        Kq = sc.sb("Kq", [128, 2, NE * 128], BF16)
        Vq = sc.sb("Vq", [128, NE, 256], BF16)
        for g in range(2):
            dma("sp", Kc[:, g, :], KT[g][:, 0:CTXN])
            dma("sp", Kq[:, g, :], KTq[g])
        dma("sp", Vc[:], VT[0:CTXN, :].rearrange("(n p) c -> p n c", p=128))
        dma("sp", Vq[:], VTq.rearrange("(n p) c -> p n c", p=128))
        mfirst = sc.sb("mfirst", [128, 128], F32)
        mlast = sc.sb("mlast", [128, 128], F32)
        op("dve", "tensor_scalar", out=mfirst[:], in0=masks[:, MK_WP, :], scalar1=fl[:, 4:5], scalar2=None, op0=ALU.mult)
        op("dve", "tensor_scalar", out=mlast[:], in0=masks[:, MK_WN, :], scalar1=fl[:, 5:6], scalar2=None, op0=ALU.mult)
        q_ = Rot([sc.sb("q%d" % i, [128, 4, 128], BF16) for i in range(3)])
        ps_s = Rot([sc.ps("pss%d" % i, [128, 512], F32) for i in range(3)])
        pT_ = Rot([sc.sb("pT%d" % i, [128, 512], BF16) for i in range(4)])
        po_ = Rot([sc.ps("po%d" % i, [128, 512], F32) for i in range(2)])
        pm_ = Rot([sc.ps("pm%d" % i, [128, 512], F32) for i in range(2)])
        rs_ = Rot([sc.sb("rs%d" % i, [128, 512], F32) for i in range(2)])
        o_ = Rot([sc.sb("o%d" % i, [128, 4, 128], BF16) for i in range(2)])
        sk = load_row(sc, RV_SINK, 8, "sk")
        op("act", "activation", out=sk[:], in_=sk[:], func=AF.Exp)
        groups = []
        for j in range(NLQ):
            keys = [("c", k, None) for k in range(NCT)]
            keys.append(("q", j, mfirst[:] if j == 0 else masks[:, MK_WP, :]))
            keys.append(("q", j + 1, None))
            keys.append(("q", j + 2, mlast[:] if j == NLQ - 1 else masks[:, MK_WN, :]))
            for g in range(2):
                groups.append((j, g, keys))
        items = []
        for gi, (j, g, keys) in enumerate(groups):
            for i, (src, kt, mk) in enumerate(keys):
                items.append((gi, i, src, kt, mk, i == 0, i == len(keys) - 1))
        qbuf = {}

        def load_q(gi):
            if gi < len(groups) and gi not in qbuf:
                j, g, _ = groups[gi]
                q = q_.next()
                dma("sp", q[:], QTq[g * 4:(g + 1) * 4, :, j * 128:(j + 1) * 128].rearrange("h d t -> d h t"))
                qbuf[gi] = q

        def issue_S(n):
            gi, i, src, kt, mk, first, last = items[n]
            load_q(gi)
            if first:
                load_q(gi + 1)
            g = groups[gi][1]
            kk = Kc if src == "c" else Kq
            p = ps_s.next()
            op("pe", "matmul", p[:], kk[:, g, kt * 128:(kt + 1) * 128], qbuf[gi][:].rearrange("d h t -> d (h t)"), start=True, stop=True)
            return p

        LOOK = 2
        pend = [issue_S(n) for n in range(min(LOOK, len(items)))]
        acc = {}
        for n, (gi, i, src, kt, mk, first, last) in enumerate(items):
            if n + LOOK < len(items):
                pend.append(issue_S(n + LOOK))
            p = pend.pop(0)
            j, g, keys = groups[gi]
            if first:
                acc[gi] = (po_.next(), pm_.next())
            po, pm = acc[gi]
            pT = pT_.next()
            op("act", "activation", out=pT[:], in_=p[:], func=AF.Exp, scale=ATT_SCALE)
            if mk is not None:
                op("pool", "tensor_tensor", out=pT[:].rearrange("k (h t) -> k h t", h=4), in0=pT[:].rearrange("k (h t) -> k h t", h=4),
                   in1=bc(mk, 1, [128, 4, 128]), op=ALU.mult)
            vv = Vc if src == "c" else Vq
            op("pe", "matmul", po[:], vv[:, kt, g * 128:(g + 1) * 128], pT[:], start=first, stop=last)
            op("pe", "matmul", pm[:], ones_b[:], pT[:], start=first, stop=last)
            if last:
                rs = rs_.next()
                op("dve", "tensor_tensor", out=rs[:].rearrange("d (h t) -> d h t", h=4), in0=pm[:].rearrange("d (h t) -> d h t", h=4),
                   in1=bc(sk[:, g * 4:(g + 1) * 4], 2, [128, 4, 128]), op=ALU.add)
                op("dve", "reciprocal", out=rs[:], in_=rs[:])
                o = o_.next()
                op("dve", "tensor_tensor", out=o[:].rearrange("d h t -> d (h t)"), in0=po[:], in1=rs[:], op=ALU.mult)
                dma("act", YTL[8 + g * 4:8 + (g + 1) * 4, :, j * 128:(j + 1) * 128].rearrange("h d t -> d h t"), o[:])
                del acc[gi]
                qbuf.pop(gi, None)
        S.barrier()
        sc.close()

    def phase_oproj(l):
        sc = Scope(nc, "o")
        Wo = sc.sb("Wo", [128, 16, D], BF16)
        for c in range(16):
            dma("pool", Wo[:, c, :], w_out[l][c * 128:(c + 1) * 128, :])
        g1 = load_mod(sc, l, 2, "g1")
        gsc2 = load_mod(sc, l, 3, "gsc2")
        sh2 = load_mod(sc, l, 4, "sh2")
        wr = sc.sb("wr", [128, 8, 20], F32)
        dma("sp", wr[:], moe_wr[l].rearrange("(c p) n -> p c n", p=128))
        brow = load_row(sc, RV_BROUTE + l * 20, 20, "brow")
        y_ = Rot([sc.sb("y%d" % i, [128, 16, 128], BF16) for i in range(2)])
        x_ = Rot([sc.sb("x%d" % i, [128, D], F32) for i in range(2)])
        po = sc.ps("po", [128, 1024], F32)
        tmp = sc.sb("tmp", [128, D], F32)
        xn_ = Rot([sc.sb("xn%d" % i, [128, D], F32) for i in range(2)])
        junk = sc.sb("junk", [128, D], BF16)
        ss = sc.sb("ss", [128, 1], F32)
        r1 = sc.sb("r1", [128, 1], F32)
        r2 = sc.sb("r2", [128, 1], F32)
        h32 = sc.sb("h32", [128, D], F32)
        pt32 = sc.ps("pt32", [128, 8, 128], F32)
        hT32 = sc.sb("hT32", [128, 8, 128], F32)
        hTb = Rot([sc.sb("hTb%d" % i, [128, 8, 128], BF16) for i in range(2)])
        plog = sc.ps("plog", [128, 32], F32)
        lg = sc.sb("lg", [128, 20], F32)
        sm = sc.sb("sm", [128, 16], F32)
        oh = sc.sb("oh", [128, 4], F32)
        eg = sc.sb("eg", [128, 4], F32)
        pen = sc.sb("pen", [128, 4], F32)
        elm = sc.sb("elm", [128, 16], F32)
        mk1 = sc.sb("mk1", [128, 16], F32)
        el2 = sc.sb("el2", [128, 16], F32)
        mk2 = sc.sb("mk2", [128, 16], F32)
        local = (l == 1)
        if not local:
            xsrc, ysrc, xdst, hdst = xin, YT, XRES, HTF
            tiles = list(range(NT))
            nctx = NCT
        else:
            xsrc, ysrc, xdst, hdst = Xq, YTL, XQ1, HTFq
            tiles = list(range(NLQ))
            nctx = 0
        t_lo = 0
        NTP = len(tiles)
        LG = sc.sb("LG", [128, NT, 20], F32)
        po2 = [po, sc.ps("po_b", [128, 1024], F32)]
        pend = {}

        def mm(k):
            ti = tiles[k]
            r0 = ti * 128
            y = y_.next()
            dma("sp", y[:], ysrc[:, :, r0:r0 + 128].rearrange("c p t -> p c t"))
            x = x_.next()
            dma("sp", x[:], xsrc[r0:r0 + 128, :])
            p = po2[k % 2]
            for half in range(2):
                for c in range(16):
                    op("pe", "matmul", p[:, half * 512:(half + 1) * 512], y[:, c, :], Wo[:, c, half * 512:(half + 1) * 512], start=(c == 0), stop=(c == 15))
            pend[k] = (p, x)

        mm(0)
        for k, ti in enumerate(tiles):
            if k + 1 < len(tiles):
                mm(k + 1)
            p, x = pend.pop(k)
            s = 1 if ti < nctx else 0
            r0 = ti * 128
            op("dve", "tensor_tensor", out=tmp[:], in0=p[:], in1=g1[s][:], op=ALU.mult)
            xn = xn_.next()
            op("pool", "tensor_tensor", out=xn[:], in0=tmp[:], in1=x[:], op=ALU.add)
            dma("act", xdst[r0:r0 + 128, :], xn[:])
            op("act", "activation", out=junk[:], in_=xn[:], func=AF.Square, accum_out=ss[:])
            rstd = rstd_from_ss((r1, r2), ss[:], D)
            op("dve", "scalar_tensor_tensor", out=tmp[:], in0=xn[:], scalar=rstd[:, 0:1], in1=gsc2[s][:], op0=ALU.mult, op1=ALU.mult)
            op("dve", "tensor_tensor", out=h32[:], in0=tmp[:], in1=sh2[s][:], op=ALU.add)
            for c in range(8):
                op("pe", "transpose", pt32[:, c, :], h32[:, c * 128:(c + 1) * 128], ident_f[:])
            op("act", "copy", out=hT32[:], in_=pt32[:])
            hb = hTb.next()
            op("pool", "tensor_copy", out=hb[:], in_=hT32[:])
            dma("act", hdst[:, :, r0:r0 + 128].rearrange("c p t -> p c t"), hb[:])
            for c in range(8):
                op("pe", "matmul", plog[:, 0:20], hT32[:, c, :], wr[:, c, :], start=(c == 0), stop=(c == 7))
            op("dve", "tensor_tensor", out=LG[:, ti, :], in0=plog[:, 0:20], in1=brow[:], op=ALU.add)
        R = sc.sb("R", [128, 10, NT], F32)
        gmax, gsum, pgrp, m1, m2, dm, ed, w1p, w2p = [R[:, i, 0:NTP] for i in range(9)]
        OH = sc.sb("OH", [128, NT, 4], F32)
        ELM = sc.sb("ELM", [128, NT, 16], F32)
        MK1 = sc.sb("MK1", [128, NT, 16], F32)
        EL2 = sc.sb("EL2", [128, NT, 16], F32)
        MK2 = sc.sb("MK2", [128, NT, 16], F32)
        GLv = LG[:, 0:NTP, 0:4]
        ELv = LG[:, 0:NTP, 4:20]
        oh = OH[:, 0:NTP, :]
        elm, mk1, el2, mk2 = [t[:, 0:NTP, :] for t in (ELM, MK1, EL2, MK2)]
        op("dve", "reduce_max", out=gmax, in_=GLv, axis=AX.X)
        op("dve", "tensor_tensor", out=oh, in0=GLv, in1=bc(gmax, 2, [128, NTP, 4]), op=ALU.is_ge)
        op("dve", "tensor_tensor", out=elm[:, :, 0:4], in0=GLv, in1=bc(gmax, 2, [128, NTP, 4]), op=ALU.subtract)
        op("act", "activation", out=elm[:, :, 0:4], in_=elm[:, :, 0:4], func=AF.Exp)
        op("dve", "reduce_sum", out=gsum, in_=elm[:, :, 0:4], axis=AX.X)
        op("dve", "reciprocal", out=pgrp, in_=gsum)
        op("dve", "tensor_scalar", out=oh, in0=oh, scalar1=1.0, scalar2=BIG, op0=ALU.subtract, op1=ALU.mult)
        op("dve", "tensor_tensor", out=elm.rearrange("p t (g k) -> p t g k", g=4), in0=ELv.rearrange("p t (g k) -> p t g k", g=4),
           in1=bc(oh, 3, [128, NTP, 4, 4]), op=ALU.add)
        op("dve", "reduce_max", out=m1, in_=elm, axis=AX.X)
        op("dve", "tensor_tensor", out=mk1, in0=elm, in1=bc(m1, 2, [128, NTP, 16]), op=ALU.is_ge)
        op("dve", "scalar_tensor_tensor", out=el2, in0=mk1, scalar=-BIG, in1=elm, op0=ALU.mult, op1=ALU.add)
        op("dve", "reduce_max", out=m2, in_=el2, axis=AX.X)
        op("dve", "tensor_tensor", out=mk2, in0=el2, in1=bc(m2, 2, [128, NTP, 16]), op=ALU.is_ge)
        op("dve", "tensor_tensor", out=dm, in0=m2, in1=m1, op=ALU.subtract)
        op("act", "activation", out=ed, in_=dm, func=AF.Exp)
        op("dve", "tensor_scalar", out=w1p, in0=ed, scalar1=1.0, scalar2=None, op0=ALU.add)
        op("dve", "reciprocal", out=w1p, in_=w1p)
        op("dve", "tensor_tensor", out=w1p, in0=w1p, in1=pgrp, op=ALU.mult)
        op("dve", "tensor_tensor", out=w2p, in0=w1p, in1=ed, op=ALU.mult)
        op("dve", "tensor_tensor", out=mk1, in0=mk1, in1=bc(w1p, 2, [128, NTP, 16]), op=ALU.mult)
        op("dve", "tensor_tensor", out=mk2, in0=mk2, in1=bc(w2p, 2, [128, NTP, 16]), op=ALU.mult)
        op("dve", "tensor_tensor", out=gates[:, 0:NTP, :], in0=mk1, in1=mk2, op=ALU.add)
        S.barrier()
        sc.close()

    def phase_moe(l):
        sc = Scope(nc, "e")
        last = (l == 1)
        W2b = sc.sb("W2b", [128, 32, D], BF16)
        for e in range(16):
            dma("pool", W2b[:, 2 * e:2 * e + 2, :], moe_w2[l, e].rearrange("(fc p) d -> p fc d", p=128))
        g2 = load_mod(sc, l, 5, "g2")
        if last:
            gfin = load_row(sc, RV_GFIN, D, "gfin")
        hT_ = Rot([sc.sb("hT%d" % i, [128, 8, 512], BF16) for i in range(1)])
        gB_ = Rot([sc.sb("gateB%d" % i, [128, 4, 512], BF16) for i in range(2)])
        hid = sc.sb("hid", [128, 32, 512], BF16)
        w1_ = Rot([sc.sb("w1%d" % i, [128, 8, 256], BF16) for i in range(2)])
        w3_ = Rot([sc.sb("w3%d" % i, [128, 8, 256], BF16) for i in range(2)])
        pa_ = Rot([sc.ps("pa%d" % i, [128, 512], F32) for i in range(2)])
        pu_ = Rot([sc.ps("pu%d" % i, [128, 512], F32) for i in range(2)])
        po_ = Rot([sc.ps("po%d" % i, [128, 512], F32) for i in range(2)])
        sg_ = Rot([sc.sb("sg%d" % i, [128, 512], BF16) for i in range(2)])
        t1_ = Rot([sc.sb("t1%d" % i, [128, 512], BF16) for i in range(2)])
        x_ = Rot([sc.sb("x%d" % i, [128, D], F32) for i in range(1)])
        xn_ = Rot([sc.sb("xn%d" % i, [128, D], F32) for i in range(1)])
        tmp = sc.sb("tmp", [128, 512], F32)
        junk = sc.sb("junk", [128, D], BF16)
        ss = sc.sb("ss", [128, 1], F32)
        r1 = sc.sb("r1", [128, 1], F32)
        r2 = sc.sb("r2", [128, 1], F32)
        if not last:
            blks, hsrc, xsrc2 = blocks, HTF, XRES
        else:
            blks, hsrc, xsrc2 = [(t0_, min(4, NLQ - t0_), False) for t0_ in range(0, NLQ, 4)], HTFq, XQ1
        for (tile0, n, is_ctx) in blks:
            NB = n * 128
            s = 1 if is_ctx else 0
            tok0 = tile0 * 128
            hT = hT_.next()
            dma("sp", hT[:, :, 0:NB], hsrc[:, :, tok0:tok0 + NB].rearrange("c p t -> p c t"))
            for e in range(16):
                if e % 4 == 0:
                    gateB = gB_.next()
                    for j in range(n):
                        p = po_.next()
                        for k in range(4):
                            op("pe", "matmul", p[:, k * 128:(k + 1) * 128], gates[:, tile0 + j, e + k:e + k + 1].to_broadcast([128, 128]),
                               ident_f[:], start=True, stop=True)
                        op("act", "copy", out=gateB[:, :, j * 128:(j + 1) * 128], in_=p[:].rearrange("p (k t) -> p k t", k=4))
                w1 = w1_.next()
                w3 = w3_.next()
                dma("sp", w1[:].rearrange("p c f -> p (c f)"), W1B[l, e])
                dma("sp", w3[:].rearrange("p c f -> p (c f)"), W3B[l, e])
                for fc in range(2):
                    pa = pa_.next()
                    pu = pu_.next()
                    for c in range(8):
                        op("pe", "matmul", pa[:, 0:NB], w1[:, c, fc * 128:(fc + 1) * 128], hT[:, c, 0:NB], start=(c == 0), stop=(c == 7))
                    for c in range(8):
                        op("pe", "matmul", pu[:, 0:NB], w3[:, c, fc * 128:(fc + 1) * 128], hT[:, c, 0:NB], start=(c == 0), stop=(c == 7))
                    sg = sg_.next()
                    op("act", "activation", out=sg[:, 0:NB], in_=pa[:, 0:NB], func=AF.Silu)
                    t1 = t1_.next()
                    op("dve", "tensor_tensor", out=t1[:, 0:NB], in0=pu[:, 0:NB], in1=gateB[:, e % 4, 0:NB], op=ALU.mult)
                    op("pool", "tensor_tensor", out=hid[:, 2 * e + fc, 0:NB], in0=sg[:, 0:NB], in1=t1[:, 0:NB], op=ALU.mult)
            for j in range(n):
                ti = tile0 + j
                r0 = ti * 128
                x = x_.next()
                dma("sp", x[:], xsrc2[r0:r0 + 128, :])
                xn = xn_.next()
                for half in range(2):
                    po = po_.next()
                    for k in range(32):
                        op("pe", "matmul", po[:], hid[:, k, j * 128:(j + 1) * 128], W2b[:, k, half * 512:(half + 1) * 512], start=(k == 0), stop=(k == 31))
                    op("dve", "tensor_tensor", out=tmp[:], in0=po[:], in1=g2[s][:, half * 512:(half + 1) * 512], op=ALU.mult)
                    op("pool", "tensor_tensor", out=xn[:, half * 512:(half + 1) * 512], in0=tmp[:], in1=x[:, half * 512:(half + 1) * 512], op=ALU.add)
                if not last:
                    dma("act", XRES[r0:r0 + 128, :], xn[:])
                else:
                    op("act", "activation", out=junk[:], in_=xn[:], func=AF.Square, accum_out=ss[:])
                    rstd = rstd_from_ss((r1, r2), ss[:], D)
                    op("dve", "scalar_tensor_tensor", out=x[:], in0=xn[:], scalar=rstd[:, 0:1], in1=gfin[:], op0=ALU.mult, op1=ALU.mult)
                    dma("act", out[ti * 128:(ti + 1) * 128, :], x[:])
        S.barrier()
        sc.close()

    def phase_lru():
        sc = Scope(nc, "r")
        xc = sc.sb("xc", [128, T], F32)
        xcb = sc.sb("xcb", [128, T], BF16)
        hf = sc.sb("hf", [128, T], F32)
        hb = sc.sb("hb", [128, T], F32)
        raw = Rot([sc.sb("raw%d" % i, [128, 1028], F32) for i in range(1)])
        wa = sc.sb("wa", [128, 2, 2, 128], BF16)
        pr = sc.ps("pr", [128, 2048], F32)
        pi = sc.ps("pi", [128, 2048], F32)
        rb = sc.sb("rb", [128, 2048], F32)
        ib = sc.sb("ib", [128, 2048], F32)
        sb2 = sc.sb("sb2", [128, 2048], F32)
        gt = Rot([sc.sb("gt%d" % i, [128, 1024], BF16) for i in range(2)])
        yo = Rot([sc.sb("yo%d" % i, [128, 1024], BF16) for i in range(2)])
        lb = sc.sb("lb", [128, 16], F32)
        lb2 = sc.sb("lb2", [128, 16], F32)
        op("act", "activation", out=lb[:], in_=colT[:, CV_LAM:CV_LAM + 16], func=AF.Exp, scale=-1.0)
        op("act", "activation", out=lb[:], in_=lb[:], func=AF.Ln, bias=1.0)
        op("dve", "tensor_scalar", out=lb2[:], in0=lb[:], scalar1=-16.0, scalar2=None, op0=ALU.mult)
        op("dve", "tensor_scalar", out=lb[:], in0=lb[:], scalar1=-8.0, scalar2=None, op0=ALU.mult)
        spans = []
        for c0 in range(0, CTXN, 2048):
            spans.append((c0, min(2048, CTXN - c0)))
        for c0 in range(0, L, 2048):
            spans.append((CTXN + c0, min(2048, L - c0)))
        nctx_sp = len([s_ for s_ in spans if s_[0] < CTXN])
        for cc in range(8):
            for (c0, n, tok0) in conv_spans(1024):
                r = raw.next()
                dma("sp", r[:, 0:n + 4], XBC[cc * 128:(cc + 1) * 128, c0 - 2:c0 + n + 2])
                op("dve", "tensor_scalar", out=xc[:, tok0:tok0 + n], in0=r[:, 0:n], scalar1=cvc(CV_LRU_CW + cc), scalar2=cvc(CV_LRU_CB + cc), op0=ALU.mult, op1=ALU.add)
                for k in range(1, 5):
                    op("dve", "scalar_tensor_tensor", out=xc[:, tok0:tok0 + n], in0=r[:, k:k + n], scalar=cvc(CV_LRU_CW + k * 8 + cc),
                       in1=xc[:, tok0:tok0 + n], op0=ALU.mult, op1=ALU.add)
            op("pool", "tensor_copy", out=xcb[:], in_=xc[:])
            for ax in range(2):
                for d in range(2):
                    dma("pool", wa[:, ax, d, :], lru_bd[ax, d, cc])
            for d in range(2):
                if d == 0:
                    order = spans
                else:
                    order = spans[:nctx_sp][::-1] + spans[nctx_sp:][::-1]
                hh = hf if d == 0 else hb
                for si, (s0, n) in enumerate(order):
                    for q0 in range(0, n, 512):
                        nq = min(512, n - q0)
                        op("pe", "matmul", pr[:, q0:q0 + nq], wa[:, 0, d, :], xcb[:, s0 + q0:s0 + q0 + nq], start=True, stop=True)
                        op("pe", "matmul", pi[:, q0:q0 + nq], wa[:, 1, d, :], xcb[:, s0 + q0:s0 + q0 + nq], start=True, stop=True)
                    op("act", "activation", out=rb[:, 0:n], in_=pr[:, 0:n], func=AF.Sigmoid, bias=cvc(CV_BA + d * 8 + cc))
                    op("act", "activation", out=ib[:, 0:n], in_=pi[:, 0:n], func=AF.Sigmoid, bias=cvc(CV_BX + d * 8 + cc))
                    op("act", "activation", out=sb2[:, 0:n], in_=rb[:, 0:n], func=AF.Exp, scale=lb2[:, d * 8 + cc:d * 8 + cc + 1])
                    op("act", "activation", out=rb[:, 0:n], in_=rb[:, 0:n], func=AF.Exp, scale=lb[:, d * 8 + cc:d * 8 + cc + 1])
                    op("act", "activation", out=sb2[:, 0:n], in_=sb2[:, 0:n], func=AF.Sqrt, scale=-1.0, bias=1.0)
                    op("dve", "tensor_tensor", out=ib[:, 0:n], in0=ib[:, 0:n], in1=sb2[:, 0:n], op=ALU.mult)
                    op("dve", "tensor_tensor", out=ib[:, 0:n], in0=ib[:, 0:n], in1=xc[:, s0:s0 + n], op=ALU.mult)
                    if d == 0:
                        init = 0.0 if s0 == 0 else hf[:, s0 - 1:s0]
                        op("dve", "tensor_tensor_scan", out=hf[:, s0:s0 + n], data0=rb[:, 0:n], data1=ib[:, 0:n], initial=init, op0=ALU.mult, op1=ALU.add)
                    else:
                        if s0 + n == CTXN:
                            init = 0.0
                        elif s0 + n == T:
                            init = hb[:, 0:1]
                        else:
                            init = hb[:, s0 + n:s0 + n + 1]
                        op("dve", "tensor_tensor_scan", out=rev(hb[:, s0:s0 + n], n), data0=rev(rb[:, 0:n], n), data1=rev(ib[:, 0:n], n),
                           initial=init, op0=ALU.mult, op1=ALU.add)
            for (s0, n) in [(CTXN + c0, min(1024, L - c0)) for c0 in range(0, L, 1024)]:
                g = gt.next()
                dma("sp", g[:, 0:n], GATE[cc, :, s0:s0 + n])
                op("pool", "tensor_tensor", out=hf[:, s0:s0 + n], in0=hf[:, s0:s0 + n], in1=hb[:, s0:s0 + n], op=ALU.add)
                y = yo.next()
                op("dve", "tensor_tensor", out=y[:, 0:n], in0=hf[:, s0:s0 + n], in1=g[:, 0:n], op=ALU.mult)
                dma("act", YT[cc, :, s0:s0 + n], y[:, 0:n])
        S.barrier()
        sc.close()

    phase_consts()
    phase_wconv()
    phase_mod(0)
    G0 = Scope(nc, "l0")
    GL = {"dtall": G0.sb("dtall", [128, NT, 32], F32), "dAall": G0.sb("dAall", [128, NT, 32], F32)}
    phase_inproj(0, GL)
    phase_conv0()
    phase_ssd(GL)
    G0.close()
    phase_attn(0)
    phase_oproj(0)
    phase_moe(0)
    phase_mod(1)
    phase_inproj(1, None)
    phase_lru()
    phase_select()
    phase_attn_local()
    phase_oproj(1)
    phase_moe(1)
    S.barrier()
    G.close()
    S.close()
    return nc


def _host_consts(L):
    t = np.arange(128)
    tt, ii = np.meshgrid(t, t, indexing="ij")
    m = np.zeros((8, 128, 128), np.float32)
    m[0] = tt > ii
    m[1] = tt <= ii
    m[2] = ii >= tt
    m[3] = tt < ii
    m[4] = tt >= ii
    m[5] = tt >= ii
    m[6] = tt >= ii
    m[7] = tt <= ii
    rows = L // GRID_W
    row = np.repeat(np.arange(rows), GRID_W).astype(np.float32)
    col = np.tile(np.arange(GRID_W), rows).astype(np.float32)
    n_freq = 32
    inv = (10000.0 ** (-np.arange(n_freq, dtype=np.float32) / n_freq)).astype(np.float32)
    ang = np.concatenate([row[:, None] * inv, col[:, None] * inv], axis=-1).astype(np.float32)
    cos = np.cos(ang).astype(np.float32).T
    sin = np.sin(ang).astype(np.float32).T
    cos2 = np.concatenate([cos, cos], axis=0)
    sin2 = np.concatenate([-sin, sin], axis=0)
    return m, np.ascontiguousarray(cos2), np.ascontiguousarray(sin2)


def _swap_heads(w, nheads):
    w = w.reshape(w.shape[0], nheads, 2, 64)
    return np.ascontiguousarray(w[:, :, ::-1, :]).reshape(w.shape[0], nheads * 128)


def make_inputs(inp, b, NL, NCT):
    f = lambda a: np.ascontiguousarray(np.asarray(a, dtype=np.float32))
    L = NL * 128
    m = {}
    m["xin"] = f(np.concatenate([inp["ctx"][b], inp["x"][b]], axis=0))
    c16 = np.zeros((16, 128), np.float32)
    c16[0::2] = f(inp["c"][b]).reshape(8, 128)
    c16[1::2] = f(inp["c_ctx"]).reshape(8, 128)
    m["cv16"] = c16
    m["w_mod"] = f(inp["w_mod"])
    m["b_mod"] = f(inp["b_mod"])
    rv = np.zeros((1, RV_N), np.float32)
    rv[0, RV_GMIX:RV_GMIX + 2048] = f(inp["g_mix"]).reshape(-1)
    rv[0, RV_GFFN:RV_GFFN + 2048] = f(inp["g_ffn"]).reshape(-1)
    rv[0, RV_GFIN:RV_GFIN + 1024] = f(inp["g_final"])
    rv[0, RV_NORMG:RV_NORMG + 1024] = f(inp["ssd_norm_g"][0])
    rv[0, RV_SSD_D:RV_SSD_D + 16] = f(inp["ssd_d"][0])
    rv[0, RV_DTB:RV_DTB + 32] = f(inp["ssd_dt_bias"][0]).reshape(-1)
    rv[0, RV_ALOG:RV_ALOG + 32] = f(inp["ssd_a_log"][0]).reshape(-1)
    for l in range(2):
        rv[0, RV_BROUTE + l * 20:RV_BROUTE + l * 20 + 4] = f(inp["moe_b_grp"][l])
        rv[0, RV_BROUTE + l * 20 + 4:RV_BROUTE + l * 20 + 20] = f(inp["moe_b_rt"][l])
    rv[0, RV_SINK:RV_SINK + 8] = f(inp["swa_sink"][0])
    m["rowv"] = rv
    cvv = np.zeros((CV_N, 128), np.float32)
    cvv[CV_SSD_CW:CV_SSD_CW + 50] = f(inp["ssd_conv_w"][0]).reshape(5, 10, 128).reshape(50, 128)
    cvv[CV_SSD_CB:CV_SSD_CB + 10] = f(inp["ssd_conv_b"][0]).reshape(10, 128)
    qg = f(inp["att_q_g"][0])
    kg = f(inp["att_k_g"][0])
    cvv[CV_QG] = qg
    cvv[CV_KG] = kg
    cvv[CV_QGS] = np.concatenate([qg[64:], qg[:64]])
    cvv[CV_KGS] = np.concatenate([kg[64:], kg[:64]])
    cvv[CV_LRU_CW:CV_LRU_CW + 40] = f(inp["lru_conv_w"][0]).reshape(5, 8, 128).reshape(40, 128)
    cvv[CV_LRU_CB:CV_LRU_CB + 8] = f(inp["lru_conv_b"][0]).reshape(8, 128)
    cvv[CV_BA:CV_BA + 16] = f(inp["lru_b_a"][0]).reshape(16, 128)
    cvv[CV_BX:CV_BX + 16] = f(inp["lru_b_x"][0]).reshape(16, 128)
    cvv[CV_LAM:CV_LAM + 16] = f(inp["lru_lam"][0]).reshape(16, 128)
    m["colv"] = cvv
    cm, cos2, sin2 = _host_consts(L)
    m["cmask"] = cm
    m["cos2"] = cos2
    m["sin2"] = sin2
    ab = f(inp["ab_w_in"][0])
    cd = f(inp["cd_w_in"][0])
    m["ab_w_in"] = ab
    m["cd_w_in"] = cd
    m["ab_w_sw"] = np.concatenate([_swap_heads(ab[:, 2336:3360], 8), _swap_heads(ab[:, 3360:3616], 2)], axis=1)
    m["cd_w_sw"] = np.concatenate([_swap_heads(cd[:, 2048:3072], 8), _swap_heads(cd[:, 3072:3328], 2)], axis=1)
    m["ab_w_out"] = f(inp["ab_w_out"][0])
    m["cd_w_out"] = f(inp["cd_w_out"][0])
    bd = np.zeros((2, 2, 8, 128, 128), np.float32)
    for ax, key in enumerate(("lru_w_a", "lru_w_x")):
        w = f(inp[key][0])
        for d in range(2):
            for cc in range(8):
                bd[ax, d, cc, 0:64, 0:64] = w[d, 2 * cc]
                bd[ax, d, cc, 64:128, 64:128] = w[d, 2 * cc + 1]
    m["lru_bd"] = bd
    m["moe_wr"] = np.ascontiguousarray(np.concatenate([f(inp["moe_w_grp"]), f(inp["moe_w_rt"])], axis=-1))
    m["moe_w1"] = f(inp["moe_w1"])
    m["moe_w3"] = f(inp["moe_w3"])
    m["moe_w2"] = f(inp["moe_w2"])
    return m


def rank_flags(r):
    fl = np.zeros((1, 8), np.float32)
    fl[0, r] = 1.0
    fl[0, 4] = 1.0 if r > 0 else 0.0
    fl[0, 5] = 1.0 if r < 3 else 0.0
    return fl


_NC_CACHE = {}


def kernel(**inputs):
    B, L, _ = inputs["x"].shape
    NL = L // 128
    NCT = inputs["ctx"].shape[1] // 128
    key = (NL, NCT)
    if key not in _NC_CACHE:
        _NC_CACHE[key] = build(NL, NCT)
    nc = _NC_CACHE[key]
    maps = [make_inputs(inputs, b, NL, NCT) for b in range(B)]
    in_maps = []
    for i in range(8):
        m = dict(maps[(i // 4) % B])
        m["rflag"] = rank_flags(i % 4)
        in_maps.append(m)
    res = run_bass_kernel_spmd(nc, in_maps, core_ids=list(range(8)))
    LQ = L // 4
    full = np.zeros((B, L, D), np.float32)
    for i in range(8):
        b, r = (i // 4) % B, i % 4
        if i // 4 < B:
            full[b, r * LQ:(r + 1) * LQ] = np.asarray(res.results[i]["out"], dtype=np.float32)
    return full
```

```python
from contextlib import ExitStack
import numpy as np
import concourse.bass as bass
import concourse.mybir as mybir
from concourse.bass_utils import run_bass_kernel_spmd

F32 = mybir.dt.float32
BF16 = mybir.dt.bfloat16
ALU = mybir.AluOpType
AF = mybir.ActivationFunctionType
AX = mybir.AxisListType

D = 1024
EPS = 1e-6
ATT_SCALE = 128 ** -0.5
GRID_W = 64
BIG = 1.0e9


class _Res:
    __slots__ = ("lw", "rd")

    def __init__(self):
        self.lw = None
        self.rd = {}


class _TL:
    def __init__(self, name, sem, unit):
        self.name = name
        self.sem = sem
        self.unit = unit
        self.cnt = 0
        self.vc = {}
        self.snaps = {}


class AutoSync:
    def __init__(self, nc, n_dma_sems=8):
        self.nc = nc
        self.res = {}
        self.tl = {}
        self._stack = []
        for name in ("pe", "dve", "act", "pool", "sp"):
            cm = nc.semaphore("s_" + name)
            sem = cm.__enter__()
            self._stack.append(cm)
            self.tl[name] = _TL(name, sem, 1)
        self.eng = {"pe": nc.tensor, "dve": nc.vector, "act": nc.scalar, "pool": nc.gpsimd, "sp": nc.sync}
        self.dq = {}
        for q in ("sp", "act", "pool"):
            lst = []
            for i in range(n_dma_sems):
                cm = nc.semaphore("d_%s%d" % (q, i))
                sem = cm.__enter__()
                self._stack.append(cm)
                t = _TL("d_%s%d" % (q, i), sem, 16)
                self.tl[t.name] = t
                lst.append(t)
            self.dq[q] = [lst, 0]
        self.n_wait = 0
        self.n_inst = 0

    def close(self):
        for cm in reversed(self._stack):
            cm.__exit__(None, None, None)

    def _key(self, ap):
        t = ap.tensor
        if type(t).__name__.startswith("DRam"):
            return None
        return t.name

    def _r(self, key):
        r = self.res.get(key)
        if r is None:
            r = self.res[key] = _Res()
        return r

    def _need(self, reads, writes):
        deps = {}

        def add(t, v):
            if deps.get(t, 0) < v:
                deps[t] = v

        for k in reads:
            lw = self._r(k).lw
            if lw:
                add(*lw)
        for k in writes:
            r = self._r(k)
            if r.lw:
                add(*r.lw)
            for t, v in r.rd.items():
                add(t, v)
        return deps

    def _wait(self, E, deps):
        T = self.tl[E]
        for t, v in deps.items():
            if E == "pe" and t == "pe":
                continue
            if T.vc.get(t, 0) >= v:
                continue
            tl = self.tl[t]
            self.eng[E].wait_ge(tl.sem, v * tl.unit)
            self.n_wait += 1
            snap = tl.snaps.get(v)
            if snap:
                for a, b in snap.items():
                    if T.vc.get(a, 0) < b:
                        T.vc[a] = b
            if T.vc.get(t, 0) < v:
                T.vc[t] = v

    def _mark(self, ev, reads, writes):
        t, v = ev
        for k in reads:
            r = self._r(k)
            if r.rd.get(t, 0) < v:
                r.rd[t] = v
        for k in writes:
            r = self._r(k)
            r.lw = ev
            r.rd = {}

    def _classify(self, args, kwargs):
        reads, writes = [], []
        first = True
        for a in args:
            if isinstance(a, bass.AP):
                k = self._key(a)
                if k is not None:
                    (writes if first else reads).append(k)
            first = False
        for n, a in kwargs.items():
            if isinstance(a, bass.AP):
                k = self._key(a)
                if k is not None:
                    (writes if n in ("out", "accum_out") else reads).append(k)
        return reads, writes

    def op(self, E, name, *args, **kwargs):
        reads, writes = self._classify(args, kwargs)
        self._wait(E, self._need(reads, writes))
        inst = getattr(self.eng[E], name)(*args, **kwargs)
        T = self.tl[E]
        T.cnt += 1
        inst.then_inc(T.sem, 1)
        T.snaps[T.cnt] = dict(T.vc)
        self._mark((E, T.cnt), reads, writes)
        self.n_inst += 1
        return inst

    def dma(self, Q, out, in_, **kwargs):
        reads = [k for k in [self._key(in_)] if k is not None]
        writes = [k for k in [self._key(out)] if k is not None]
        self._wait(Q, self._need(reads, writes))
        lst, idx = self.dq[Q]
        tl = lst[idx % len(lst)]
        self.dq[Q][1] = idx + 1
        if tl.cnt > 0:
            self._wait(Q, {tl.name: tl.cnt})
        inst = self.eng[Q].dma_start(out=out, in_=in_, **kwargs)
        tl.cnt += 1
        inst.then_inc(tl.sem, 16)
        tl.snaps[tl.cnt] = dict(self.tl[Q].vc)
        self._mark((tl.name, tl.cnt), reads, writes)
        self.n_inst += 1
        return inst

    def barrier(self):
        now = {t: tl.cnt for t, tl in self.tl.items() if tl.cnt > 0}
        for E in ("sp", "act", "pool", "dve", "pe"):
            self._wait(E, {t: v for t, v in now.items() if t != E})
            if E != "pe" and now.get(E, 0) > 0:
                self._wait(E, {E: now[E]})


class Scope:
    _n = 0

    def __init__(self, nc, tag):
        self.nc = nc
        self.es = ExitStack()
        Scope._n += 1
        self.tag = "%s%d_" % (tag, Scope._n)

    def sb(self, name, shape, dt):
        return self.es.enter_context(self.nc.sbuf_tensor(self.tag + name, list(shape), dt))

    def ps(self, name, shape, dt=F32):
        return self.es.enter_context(self.nc.psum_tensor(self.tag + name, list(shape), dt))

    def close(self):
        self.es.close()


class Rot:
    def __init__(self, items):
        self.items = items
        self.i = 0

    def next(self):
        x = self.items[self.i % len(self.items)]
        self.i += 1
        return x


def rev(ap2, n):
    return bass.AP(ap2.tensor, ap2.offset + (n - 1), [[ap2.ap[0][0], ap2.ap[0][1]], [-1, n]])


def bc(ap, axis, shape):
    return ap.unsqueeze(axis).to_broadcast(list(shape))


CV_SSD_CW = 0
CV_SSD_CB = 50
CV_QG = 60
CV_KG = 61
CV_QGS = 62
CV_KGS = 63
CV_LRU_CW = 64
CV_LRU_CB = 104
CV_BA = 112
CV_BX = 128
CV_LAM = 144
CV_N = 160
RV_GMIX = 0
RV_GFFN = 2048
RV_GFIN = 4096
RV_NORMG = 5120
RV_SSD_D = 6144
RV_DTB = 6160
RV_ALOG = 6192
RV_BROUTE = 6224
RV_SINK = 6264
RV_N = 6272


def build(NL, NCT, dbg=False):
    nc = bass.Bass("TRN2", target_bir_lowering=False)
    NT = NL + NCT
    T = NT * 128
    L = NL * 128
    CTXN = NCT * 128
    W = T + 8
    COL_CTX = 2
    COL_LAT = CTXN + 6

    def din(name, shape, dt=F32):
        return nc.dram_tensor(name, list(shape), dt, kind="ExternalInput").ap()

    def dsc(name, shape, dt):
        return nc.dram_tensor(name, list(shape), dt, kind="ExternalOutput" if dbg else "Internal").ap()

    xin = din("xin", [T, D])
    cv16 = din("cv16", [16, 128])
    w_mod = din("w_mod", [2, D, 6 * D])
    b_mod = din("b_mod", [2, 6 * D])
    rowv = din("rowv", [1, RV_N])
    colv = din("colv", [CV_N, 128])
    cmask = din("cmask", [8, 128, 128])
    cos2 = din("cos2", [128, L])
    sin2 = din("sin2", [128, L])
    w_in = [din("ab_w_in", [D, 3872]), din("cd_w_in", [D, 3584])]
    w_sw = [din("ab_w_sw", [D, 1280]), din("cd_w_sw", [D, 1280])]
    w_out = [din("ab_w_out", [2048, D]), din("cd_w_out", [2048, D])]
    lru_bd = din("lru_bd", [2, 2, 8, 128, 128])
    moe_wr = din("moe_wr", [2, D, 20])
    moe_w1 = din("moe_w1", [2, 16, D, 256])
    moe_w3 = din("moe_w3", [2, 16, D, 256])
    moe_w2 = din("moe_w2", [2, 16, 256, D])
    rflag = din("rflag", [1, 8])
    out = nc.dram_tensor("out", [NL // 4 * 128, D], F32, kind="ExternalOutput").ap()

    XRES = dsc("XRES", [T, D], F32)
    MODV = dsc("MODV", [2, 2, 6 * D], F32)
    XBC = dsc("XBC", [1280, W], F32)
    XS = dsc("XS", [T, D], BF16)
    ZT = dsc("ZT", [T, D], BF16)
    BTOK = dsc("BTOK", [T, 128], BF16)
    BT = dsc("BT", [2, 64, T], BF16)
    CTs = dsc("CT", [2, 64, T], BF16)
    QT = dsc("QT", [8, 128, T], BF16)
    KT = dsc("KT", [2, 128, T + 128], BF16)
    VT = dsc("VT", [T + 128, 256], BF16)
    NLQ = NL // 4
    LQ = NLQ * 128
    QTq = dsc("QTq", [8, 128, LQ], BF16)
    KTq = dsc("KTq", [2, 128, LQ + 256], BF16)
    VTq = dsc("VTq", [LQ + 256, 256], BF16)
    YTL = dsc("YTL", [16, 128, LQ], BF16)
    Xq = dsc("Xq", [LQ, D], F32)
    XQ1 = dsc("XQ1", [LQ, D], F32)
    HTFq = dsc("HTFq", [8, 128, LQ], BF16)
    YF = dsc("YF", [T, D], F32)
    YT = dsc("YT", [16, 128, T], BF16)
    HTF = dsc("HTF", [8, 128, T], BF16)
    GATE = dsc("GATE", [8, 128, T], BF16)
    W1B = dsc("W1B", [2, 16, 128, 2048], BF16)
    W3B = dsc("W3B", [2, 16, 128, 2048], BF16)

    S = AutoSync(nc)
    op = S.op
    dma = S.dma

    G = Scope(nc, "g")
    ident_f = G.sb("identf", [128, 128], F32)
    ident_b = G.sb("identb", [128, 128], BF16)
    ones_b = G.sb("onesb", [128, 128], BF16)
    ones_f = G.sb("onesf", [128, 128], F32)
    masks = G.sb("masks", [128, 8, 128], F32)
    colT = G.sb("colT", [128, CV_N], F32)
    gates = G.sb("gates", [128, NT, 16], F32)

    def cvc(r):
        return colT[:, r:r + 1]

    MK_LF, MK_UF, MK_MF, MK_LB, MK_UB, MK_MB, MK_WP, MK_WN = range(8)

    blocks = [(0, NCT, True)]
    t0 = NCT
    while t0 < NT:
        n = min(4, NT - t0)
        blocks.append((t0, n, False))
        t0 += n

    def phase_consts():
        sc = Scope(nc, "c")
        cst = sc.sb("cst", [128, 2, 128], F32)
        ptr = sc.ps("ptr", [128, 256], F32)
        dma("sp", masks[:], cmask.rearrange("m p f -> p m f"))
        op("dve", "tensor_tensor", out=ident_f[:], in0=masks[:, MK_UF, :], in1=masks[:, MK_UB, :], op=ALU.mult)
        op("dve", "tensor_copy", out=ident_b[:], in_=ident_f[:])
        op("pool", "memset", ones_b[:], 1.0)
        op("pool", "memset", ones_f[:], 1.0)
        dma("sp", cst[:, 0, :], colv[0:128, :])
        dma("sp", cst[0:CV_N - 128, 1, :], colv[128:CV_N, :])
        op("pe", "transpose", ptr[:, 0:128], cst[:, 0, :], ident_f[:])
        op("pe", "transpose", ptr[:, 128:128 + (CV_N - 128)], cst[0:CV_N - 128, 1, :], ident_f[0:CV_N - 128, 0:CV_N - 128])
        op("dve", "tensor_copy", out=colT[:], in_=ptr[:, 0:CV_N])
        zpad = sc.sb("zpad", [128, 256], BF16)
        op("pool", "memset", zpad[:], 0.0)
        for g in range(2):
            dma("act", KT[g][:, T:T + 128], zpad[:, 0:128])
        dma("act", VT[T:T + 128, :], zpad[:])
        S.barrier()
        sc.close()

    def phase_mod(l):
        sc = Scope(nc, "m")
        c16 = sc.sb("c16", [16, 128], F32)
        cT = sc.sb("cT", [128, 16], F32)
        pc = sc.ps("pc", [128, 16], F32)
        wm = Rot([sc.sb("wm%d" % i, [128, 8, 512], F32) for i in range(2)])
        pm = Rot([sc.ps("pm%d" % i, [2, 512], F32) for i in range(2)])
        modrow = sc.sb("modrow", [2, 6 * D], F32)
        bm = sc.sb("bm", [2, 6 * D], F32)
        gm = sc.sb("gm", [2, 2, D], F32)
        res = sc.sb("res", [2, 6 * D], F32)
        dma("sp", c16[:], cv16)
        op("act", "activation", out=c16[:], in_=c16[:], func=AF.Silu)
        op("pe", "transpose", pc[:], c16[:], ident_f[0:16, 0:16])
        op("dve", "tensor_copy", out=cT[:], in_=pc[:])
        dma("sp", bm[:], b_mod[l:l + 1, :].to_broadcast([2, 6 * D]))
        dma("sp", gm[:, 0, :], rowv[:, RV_GMIX + l * D:RV_GMIX + (l + 1) * D].to_broadcast([2, D]))
        dma("sp", gm[:, 1, :], rowv[:, RV_GFFN + l * D:RV_GFFN + (l + 1) * D].to_broadcast([2, D]))
        for nb in range(12):
            w = wm.next()
            dma("sp", w[:], w_mod[l, :, nb * 512:(nb + 1) * 512].rearrange("(c p) n -> p c n", p=128))
            p = pm.next()
            for c in range(8):
                op("pe", "matmul", p[:], cT[:, 2 * c:2 * c + 2], w[:, c, :], start=(c == 0), stop=(c == 7))
            op("dve", "tensor_tensor", out=modrow[:, nb * 512:(nb + 1) * 512], in0=p[:], in1=bm[:, nb * 512:(nb + 1) * 512], op=ALU.add)
        for half in range(2):
            b = half * 3 * D
            op("dve", "scalar_tensor_tensor", out=res[:, b:b + D], in0=modrow[:, b + D:b + 2 * D], scalar=1.0,
               in1=gm[:, half, :], op0=ALU.add, op1=ALU.mult)
            op("dve", "tensor_copy", out=res[:, b + D:b + 2 * D], in_=modrow[:, b:b + D])
            op("dve", "tensor_copy", out=res[:, b + 2 * D:b + 3 * D], in_=modrow[:, b + 2 * D:b + 3 * D])
        dma("act", MODV[l], res[:])
        S.barrier()
        sc.close()

    def load_mod(sc, l, idx, name):
        tl = []
        for s in range(2):
            t = sc.sb("%s%d" % (name, s), [128, D], F32)
            dma("sp", t[:], MODV[l, s:s + 1, idx * D:(idx + 1) * D].to_broadcast([128, D]))
            tl.append(t)
        return tl

    def load_row(sc, off, n, name):
        t = sc.sb(name, [128, n], F32)
        dma("sp", t[:], rowv[:, off:off + n].to_broadcast([128, n]))
        return t

    def rstd_from_ss(sc_tiles, ss, n):
        t1, t2 = sc_tiles
        op("dve", "tensor_scalar", out=t1[:], in0=ss, scalar1=1.0 / n, scalar2=EPS, op0=ALU.mult, op1=ALU.add)
        op("act", "activation", out=t1[:], in_=t1[:], func=AF.Sqrt)
        op("dve", "reciprocal", out=t2[:], in_=t1[:])
        return t2

    def phase_wconv():
        sc = Scope(nc, "w")
        st = Rot([sc.sb("st%d" % i, [128, 2048], F32) for i in range(3)])
        sb_ = Rot([sc.sb("sb%d" % i, [128, 2048], BF16) for i in range(3)])
        i = 0
        for l in range(2):
            for e in range(16):
                for src, dst in ((moe_w1, W1B), (moe_w3, W3B)):
                    a = st.next()
                    b = sb_.next()
                    dma("sp", a[:].rearrange("p (c f) -> p c f", c=8), src[l, e].rearrange("(c p) f -> p c f", p=128))
                    op(("pool", "dve", "act")[i % 3], "tensor_copy" if i % 3 != 2 else "copy", out=b[:], in_=a[:])
                    dma("act", dst[l, e], b[:])
                    i += 1
        S.barrier()
        sc.close()

    def phase_inproj(l, GL):
        sc = Scope(nc, "a")
        NW = 3872 if l == 0 else 3584
        Wb = sc.sb("Wb", [128, 8, NW + 1280], BF16)
        for c in range(8):
            dma("pool", Wb[:, c, 0:NW], w_in[l][c * 128:(c + 1) * 128, :])
            dma("pool", Wb[:, c, NW:NW + 1280], w_sw[l][c * 128:(c + 1) * 128, :])
        gsc = load_mod(sc, l, 0, "gsc")
        sh = load_mod(sc, l, 1, "sh")
        xt = Rot([sc.sb("xt%d" % i, [128, D], F32) for i in range(2)])
        junk = sc.sb("junk", [128, D], BF16)
        ss = sc.sb("ss", [128, 1], F32)
        r1 = sc.sb("r1", [128, 1], F32)
        r2 = sc.sb("r2", [128, 1], F32)
        tmp = sc.sb("tmp", [128, D], F32)
        htok = Rot([sc.sb("htok%d" % i, [128, D], BF16) for i in range(2)])
        hTs = [sc.sb("hT%d" % i, [128, 8, 512], BF16) for i in range(2)]
        ptp = sc.ps("ptp", [128, 8, 128], BF16)
        pf = [sc.ps("pf%d" % i, [128, 512], F32) for i in range(4)]
        pss = sc.ps("pss", [128, 512], F32)
        pz = sc.ps("pz", [128, 1024], F32)
        pdv = pss
        stg = Rot([sc.sb("stg%d" % i, [128, 512], F32) for i in range(2)])
        stb = Rot([sc.sb("stb%d" % i, [128, 512], BF16) for i in range(2)])
        zst = Rot([sc.sb("zst%d" % i, [128, D], BF16) for i in range(2)])
        vst = Rot([sc.sb("vst%d" % i, [128, 256], BF16) for i in range(2)])
        sq = sc.sb("sq", [128, 512], BF16)
        rs1 = sc.sb("rs1", [128, 512], F32)
        rs2 = sc.sb("rs2", [128, 512], F32)
        qn = sc.sb("qn", [128, 512], F32)
        qs = sc.sb("qs", [128, 512], F32)
        cosb = sc.sb("cosb", [128, 512], F32)
        sinb = sc.sb("sinb", [128, 512], F32)
        if l == 0:
            dtb = load_row(sc, RV_DTB, 32, "dtb")
            alog = load_row(sc, RV_ALOG, 32, "alog")
            aneg = sc.sb("aneg", [128, 32], F32)
            op("act", "activation", out=aneg[:], in_=alog[:], func=AF.Exp)
            op("dve", "tensor_scalar", out=aneg[:], in0=aneg[:], scalar1=-1.0, scalar2=None, op0=ALU.mult)
            d1 = sc.sb("d1", [128, 32], F32)
            d2 = sc.sb("d2", [128, 32], F32)
            d3 = sc.sb("d3", [128, 32], F32)
        if l == 0:
            zc = sc.sb("zc", [128, 4], F32)
            op("pool", "memset", zc[:], 0.0)
            for r0 in range(0, 1280, 128):
                dma("act", XBC[r0:r0 + 128, 0:2], zc[:, 0:2])
                dma("act", XBC[r0:r0 + 128, CTXN + 2:CTXN + 6], zc[:, 0:4])
                dma("act", XBC[r0:r0 + 128, W - 2:W], zc[:, 0:2])
        xsrc = xin if l == 0 else XRES
        if l == 0:
            fm = [("raw", 1024 + 128 * i, i) for i in range(10)]
            qcol, kcol, vcol = 2336, 3360, 3616
        else:
            fm = [("gelu", 128 * i, i) for i in range(8)] + [("raw", 1024 + 128 * i, i) for i in range(8)]
            qcol, kcol, vcol = 2048, 3072, 3328
        def prologue(bi):
            tile0, n, is_ctx = blocks[bi]
            hT = hTs[bi % 2]
            s = 1 if is_ctx else 0
            for j in range(n):
                x = xt.next()
                dma("sp", x[:], xsrc[(tile0 + j) * 128:(tile0 + j + 1) * 128, :])
                op("act", "activation", out=junk[:], in_=x[:], func=AF.Square, accum_out=ss[:])
                rstd = rstd_from_ss((r1, r2), ss[:], D)
                op("dve", "scalar_tensor_tensor", out=tmp[:], in0=x[:], scalar=rstd[:, 0:1], in1=gsc[s][:], op0=ALU.mult, op1=ALU.mult)
                h = htok.next()
                op("pool", "tensor_tensor", out=h[:], in0=tmp[:], in1=sh[s][:], op=ALU.add)
                for c in range(8):
                    op("pe", "transpose", ptp[:, c, :], h[:, c * 128:(c + 1) * 128], ident_b[:])
                op("act", "copy", out=hT[:, :, j * 128:(j + 1) * 128], in_=ptp[:])

        def body(bi):
            tile0, n, is_ctx = blocks[bi]
            hT = hTs[bi % 2]
            NB = n * 128
            s = 1 if is_ctx else 0
            tok0 = tile0 * 128
            if not is_ctx:
                lt0 = tok0 - CTXN
                dma("sp", cosb[:, 0:NB], cos2[:, lt0:lt0 + NB])
                dma("sp", sinb[:, 0:NB], sin2[:, lt0:lt0 + NB])
            colbase = (COL_CTX + tok0) if is_ctx else (COL_LAT + tok0 - CTXN)

            def mm_fm(p, col):
                for c in range(8):
                    op("pe", "matmul", p[:, 0:NB], Wb[:, c, col:col + 128], hT[:, c, 0:NB], start=(c == 0), stop=(c == 7))

            for (kind, col, idx) in fm:
                p = pf[idx % 4]
                mm_fm(p, col)
                if kind == "raw":
                    st = stg.next()
                    op("act", "copy", out=st[:, 0:NB], in_=p[:, 0:NB])
                    dma("act", XBC[idx * 128:(idx + 1) * 128, colbase:colbase + NB], st[:, 0:NB])
                else:
                    st = stb.next()
                    op("act", "activation", out=st[:, 0:NB], in_=p[:, 0:NB], func=AF.Gelu)
                    dma("act", GATE[idx, :, tok0:tok0 + NB], st[:, 0:NB])
            for hh in range(10):
                isq = hh < 8
                if l == 1 and is_ctx and isq:
                    continue
                col = (qcol + hh * 128) if isq else (kcol + (hh - 8) * 128)
                cols = NW + hh * 128
                pq, pw = (pf[0], pf[1]) if hh % 2 == 0 else (pf[2], pf[3])
                mm_fm(pq, col)
                mm_fm(pw, cols)
                if l == 0:
                    op("act", "activation", out=sq[:, 0:NB], in_=pq[:, 0:NB], func=AF.Square)
                    op("pe", "matmul", pss[:, 0:NB], ones_b[:], sq[:, 0:NB], start=True, stop=True)
                    op("act", "activation", out=rs1[:, 0:NB], in_=pss[:, 0:NB], func=AF.Sqrt, scale=1.0 / 128, bias=EPS)
                    op("dve", "reciprocal", out=rs2[:, 0:NB], in_=rs1[:, 0:NB])
                    g_ = cvc(CV_QG if isq else CV_KG)
                    gs_ = cvc(CV_QGS if isq else CV_KGS)
                    op("dve", "scalar_tensor_tensor", out=qn[:, 0:NB], in0=pq[:, 0:NB], scalar=g_, in1=rs2[:, 0:NB], op0=ALU.mult, op1=ALU.mult)
                    op("dve", "scalar_tensor_tensor", out=qs[:, 0:NB], in0=pw[:, 0:NB], scalar=gs_, in1=rs2[:, 0:NB], op0=ALU.mult, op1=ALU.mult)
                    a_n, a_s = qn, qs
                    e1, e2 = "pool", "pool"
                else:
                    a_n, a_s = pq, pw
                    e1, e2 = "dve", "dve"
                st = stb.next()
                if is_ctx:
                    op("act", "copy", out=st[:, 0:NB], in_=a_n[:, 0:NB])
                else:
                    o1, o2 = (qn, qs) if l == 0 else (rs1, rs2)
                    op(e1, "tensor_tensor", out=o1[:, 0:NB], in0=a_n[:, 0:NB], in1=cosb[:, 0:NB], op=ALU.mult)
                    op(e2, "tensor_tensor", out=o2[:, 0:NB], in0=a_s[:, 0:NB], in1=sinb[:, 0:NB], op=ALU.mult)
                    op("dve" if l == 0 else "pool", "tensor_tensor", out=st[:, 0:NB], in0=o1[:, 0:NB], in1=o2[:, 0:NB], op=ALU.add)
                dst = QT[hh] if isq else KT[hh - 8]
                dma("act", dst[:, tok0:tok0 + NB], st[:, 0:NB])
            for j in range(n):
                ti = tile0 + j
                if l == 0:
                    for half in range(2):
                        for c in range(8):
                            op("pe", "matmul", pz[:, half * 512:(half + 1) * 512], hT[:, c, j * 128:(j + 1) * 128],
                               Wb[:, c, half * 512:(half + 1) * 512], start=(c == 0), stop=(c == 7))
                    z = zst.next()
                    op("act", "activation", out=z[:], in_=pz[:], func=AF.Silu)
                    dma("act", ZT[ti * 128:(ti + 1) * 128, :], z[:])
                    for c in range(8):
                        op("pe", "matmul", pdv[:, 0:32], hT[:, c, j * 128:(j + 1) * 128], Wb[:, c, 2304:2336], start=(c == 0), stop=(c == 7))
                    op("dve", "tensor_tensor", out=d1[:], in0=pdv[:, 0:32], in1=dtb[:], op=ALU.add)
                    op("dve", "scalar_tensor_tensor", out=d2[:], in0=d1[:], scalar=-1.0, in1=d1[:], op0=ALU.mult, op1=ALU.max)
                    op("act", "activation", out=d2[:], in_=d2[:], func=AF.Exp, scale=-1.0)
                    op("act", "activation", out=d3[:], in_=d2[:], func=AF.Ln, bias=1.0)
                    op("dve", "scalar_tensor_tensor", out=GL["dtall"][:, ti, :], in0=d1[:], scalar=0.0, in1=d3[:], op0=ALU.max, op1=ALU.add)
                    op("dve", "tensor_tensor", out=GL["dAall"][:, ti, :], in0=GL["dtall"][:, ti, :], in1=aneg[:], op=ALU.mult)
                for c in range(8):
                    op("pe", "matmul", pdv[:, 256:512], hT[:, c, j * 128:(j + 1) * 128], Wb[:, c, vcol:vcol + 256], start=(c == 0), stop=(c == 7))
                v = vst.next()
                op("act", "copy", out=v[:], in_=pdv[:, 256:512])
                dma("act", VT[ti * 128:(ti + 1) * 128, :], v[:])
        prologue(0)
        for bi in range(len(blocks)):
            if bi + 1 < len(blocks):
                prologue(bi + 1)
            body(bi)
        S.barrier()
        sc.close()

    def conv_spans(maxn):
        sp = []
        for c0 in range(0, CTXN, maxn):
            n = min(maxn, CTXN - c0)
            sp.append((COL_CTX + c0, n, c0))
        for c0 in range(0, L, maxn):
            n = min(maxn, L - c0)
            sp.append((COL_LAT + c0, n, CTXN + c0))
        return sp

    def conv5(raw, acc, n, wrow0, stride, cc, brow):
        op("dve", "tensor_scalar", out=acc[:, 0:n], in0=raw[:, 0:n], scalar1=cvc(wrow0 + cc), scalar2=cvc(brow + cc), op0=ALU.mult, op1=ALU.add)
        for k in range(1, 5):
            op("dve", "scalar_tensor_tensor", out=acc[:, 0:n], in0=raw[:, k:k + n], scalar=cvc(wrow0 + k * stride + cc), in1=acc[:, 0:n], op0=ALU.mult, op1=ALU.add)

    def phase_conv0():
        sc = Scope(nc, "v")
        raw = Rot([sc.sb("raw%d" % i, [128, 1028], F32) for i in range(2)])
        acc = Rot([sc.sb("acc%d" % i, [128, 1024], F32) for i in range(2)])
        act = Rot([sc.sb("act%d" % i, [128, 1024], BF16) for i in range(2)])
        ptp = Rot([sc.ps("ptp%d" % i, [128, 4, 128], BF16) for i in range(2)])
        xst = Rot([sc.sb("xst%d" % i, [128, 4, 128], BF16) for i in range(3)])
        for cc in range(10):
            for (c0, n, tok0) in conv_spans(1024):
                r = raw.next()
                dma("sp", r[:, 0:n + 4], XBC[cc * 128:(cc + 1) * 128, c0 - 2:c0 + n + 2])
                a = acc.next()
                conv5(r, a, n, CV_SSD_CW, 10, cc, CV_SSD_CB)
                b = act.next()
                op("act", "activation", out=b[:, 0:n], in_=a[:, 0:n], func=AF.Silu)
                if cc >= 8:
                    dst = BT if cc == 8 else CTs
                    dma("act", dst[0, :, tok0:tok0 + n], b[0:64, 0:n])
                    dma("act", dst[1, :, tok0:tok0 + n], b[64:128, 0:n])
                if cc <= 8:
                    for j0 in range(0, n // 128, 4):
                        nj = min(4, n // 128 - j0)
                        p = ptp.next()
                        for j in range(nj):
                            op("pe", "transpose", p[:, j, :], b[:, (j0 + j) * 128:(j0 + j + 1) * 128], ident_b[:])
                        x = xst.next()
                        if (j0 // 4) % 2:
                            op("act", "copy", out=x[:, 0:nj, :], in_=p[:, 0:nj, :])
                        else:
                            op("dve", "tensor_copy", out=x[:, 0:nj, :], in_=p[:, 0:nj, :])
                        r0 = tok0 + j0 * 128
                        if cc < 8:
                            dma("act", XS[r0:r0 + nj * 128, cc * 128:(cc + 1) * 128].rearrange("(j p) c -> p j c", p=128), x[:, 0:nj, :])
                        else:
                            dma("act", BTOK[r0:r0 + nj * 128, :].rearrange("(j p) c -> p j c", p=128), x[:, 0:nj, :])
        S.barrier()
        sc.close()

    def phase_ssd(GL):
        sc = Scope(nc, "s")
        dtall, dAall = GL["dtall"], GL["dAall"]
        dsk = load_row(sc, RV_SSD_D, 16, "dsk")
        normg = load_row(sc, RV_NORMG, D, "normg")
        Hf = sc.sb("Hf", [64, 2, 512], F32)
        Hb = sc.sb("Hb", [64, 2, 512], BF16)
        tmpH = sc.sb("tmpH", [64, 2, 512], F32)
        xs_ = Rot([sc.sb("xs%d" % i, [128, D], BF16) for i in range(3)])
        btok_ = Rot([sc.sb("btok%d" % i, [128, 128], BF16) for i in range(3)])
        bt_ = Rot([sc.sb("bt%d" % i, [64, 2, 128], BF16) for i in range(3)])
        ct_ = Rot([sc.sb("ct%d" % i, [64, 2, 128], BF16) for i in range(3)])
        psm_ = Rot([sc.ps("psm", [128, 512], F32)])
        psm = psm_.items[0]
        pcb = psm[:, 0:256].rearrange("p (g i) -> p g i", g=2)
        pac = psm[:, 256:288]
        ptb = sc.ps("ptb", [128, 8, 128], BF16)
        pseg = sc.ps("pseg", [128, 1024], F32)
        pyd = sc.ps("pyd", [128, 1024], F32)
        pch = sc.ps("pch", [128, 1024], F32)
        cbm_ = Rot([sc.sb("cbm%d" % i, [128, 2, 128], F32) for i in range(3)])
        Z_ = Rot([sc.sb("Z%d" % i, [128, 16, 128], F32) for i in range(2)])
        e__ = Rot([sc.sb("e%d" % i, [128, 16, 128], F32) for i in range(3)])
        wT_ = Rot([sc.sb("wT%d" % i, [128, 16, 128], BF16) for i in range(2)])
        sm_ = Rot([tuple(sc.sb("sm%s%d" % (nm, i), [128, w], F32) for nm, w in (("a", 32), ("e", 16), ("c", 16), ("t", 16))) for i in range(3)])
        y1_ = Rot([sc.sb("y1%d" % i, [128, D], F32) for i in range(2)])
        y2 = Rot([sc.sb("y2%d" % i, [128, D], F32) for i in range(2)])
        xw_ = Rot([sc.sb("xw%d" % i, [128, D], BF16) for i in range(3)])
        yf_ = Rot([sc.sb("yf%d" % i, [128, D], F32) for i in range(2)])
        zs_ = Rot([sc.sb("zs%d" % i, [128, D], BF16) for i in range(2)])
        ss = sc.sb("ss", [128, 1], F32)
        r1 = sc.sb("r1", [128, 1], F32)
        r2 = sc.sb("r2", [128, 1], F32)
        junk = sc.sb("junk", [128, D], BF16)
        ytok = sc.sb("ytok", [128, D], BF16)
        yst = Rot([sc.sb("yst%d" % i, [128, 8, 128], BF16) for i in range(2)])

        Ssb_ = Rot([sc.sb("Ssb%d" % i, [64, 2, 512], F32) for i in range(2)])

        def partA1(ti, d):
            mL, mU, mM = (MK_LF, MK_UF, MK_MF) if d == 0 else (MK_LB, MK_UB, MK_MB)
            r0 = ti * 128
            xs = xs_.next()
            dma("sp", xs[:], XS[r0:r0 + 128, :])
            btok = btok_.next()
            dma("sp", btok[:], BTOK[r0:r0 + 128, :])
            bt = bt_.next()
            dma("sp", bt[:], BT[:, :, r0:r0 + 128].rearrange("g n t -> n g t"))
            ct = ct_.next()
            dma("sp", ct[:], CTs[:, :, r0:r0 + 128].rearrange("g n t -> n g t"))
            dA = dAall[:, ti, d * 16:(d + 1) * 16]
            dt = dtall[:, ti, d * 16:(d + 1) * 16]
            cbm, Z, e_, sm, xw = cbm_.next(), Z_.next(), e__.next(), sm_.next(), xw_.next()
            acs, eacs, ecd, te = sm[0][:], sm[1][:], sm[2][:], sm[3][:]
            op("pool", "tensor_tensor", out=Z[:], in0=bc(masks[:, mU, :], 1, [128, 16, 128]), in1=bc(dA, 2, [128, 16, 128]), op=ALU.mult)
            op("pe", "matmul", pac[:, 0:16], masks[:, mU, :], dA, start=True, stop=True)
            op("pe", "matmul", pac[:, 16:32], ones_f[:], dA, start=True, stop=True)
            for g in range(2):
                op("pe", "matmul", pcb[:, g, :], bt[:, g, :], ct[:, g, :], start=True, stop=True)
            op("dve", "tensor_copy", out=acs, in_=pac)
            op("dve", "tensor_tensor", out=te, in0=acs[:, 16:32], in1=acs[:, 0:16], op=ALU.subtract)
            op("act", "activation", out=te, in_=te, func=AF.Exp)
            op("act", "activation", out=eacs, in_=acs[:, 0:16], func=AF.Exp)
            op("act", "activation", out=ecd, in_=acs[:, 16:32], func=AF.Exp)
            op("dve", "tensor_tensor", out=te, in0=te, in1=dt, op=ALU.mult)
            op("dve", "tensor_tensor", out=cbm[:], in0=pcb, in1=bc(masks[:, mM, :], 1, [128, 2, 128]), op=ALU.mult)
            op("pool", "tensor_tensor", out=xw[:].rearrange("p (h q) -> p h q", h=16), in0=xs[:].rearrange("p (h q) -> p h q", h=16),
               in1=bc(te, 2, [128, 16, 64]), op=ALU.mult)
            Zf = Z[:].rearrange("p h i -> p (h i)")
            ef = e_[:].rearrange("p h i -> p (h i)")
            for hf_ in range(2):
                for q in range(2):
                    c0 = hf_ * 1024 + q * 512
                    op("pe", "matmul", pseg[:, q * 512:(q + 1) * 512], masks[:, mL, :], Zf[:, c0:c0 + 512], start=True, stop=True)
                op("act", "activation", out=ef[:, hf_ * 1024:(hf_ + 1) * 1024], in_=pseg[:], func=AF.Exp)
            return dict(ti=ti, xs=xs, ct=ct, btok=btok, sm=sm, cbm=cbm, e=e_, xw=xw, dt=dt)

        def partA2(st):
            xs, btok, cbm, e_, xw, dt = st["xs"], st["btok"], st["cbm"], st["e"], st["xw"], st["dt"]
            wT, y1, Ssb = wT_.next(), y1_.next(), Ssb_.next()
            op("dve", "tensor_tensor", out=e_[:], in0=e_[:], in1=bc(dt, 2, [128, 16, 128]), op=ALU.mult)
            op("dve", "tensor_tensor", out=wT[:].rearrange("p (g h) i -> p g h i", g=2), in0=e_[:].rearrange("p (g h) i -> p g h i", g=2),
               in1=bc(cbm[:], 2, [128, 2, 8, 128]), op=ALU.mult)
            for h in range(16):
                op("pe", "matmul", pyd[:, h * 64:(h + 1) * 64], wT[:, h, :], xs[:, h * 64:(h + 1) * 64], start=True, stop=True)
            op("act", "copy", out=y1[:], in_=pyd[:])
            pS = pyd[0:64, 0:1024].rearrange("p (g n) -> p g n", g=2)
            for g in range(2):
                op("pe", "matmul", pS[:, g, :], btok[:, g * 64:(g + 1) * 64], xw[:, g * 512:(g + 1) * 512], start=True, stop=True)
            op("act", "copy", out=Ssb[:], in_=pS)
            st["y1"] = y1
            st["Ssb"] = Ssb
            return st

        def partB(st):
            ct, sm, y1, Ssb = st["ct"], st["sm"], st["y1"], st["Ssb"]
            eacs = sm[1][:]
            for g in range(2):
                op("pe", "matmul", pch[:, g * 512:(g + 1) * 512], ct[:, g, :], Hb[:, g, :], start=True, stop=True)
            op("dve", "tensor_tensor", out=tmpH[:].rearrange("p g (h q) -> p (g h) q", h=8), in0=Hf[:].rearrange("p g (h q) -> p (g h) q", h=8),
               in1=bc(sm[2][0:64, :], 2, [64, 16, 64]), op=ALU.mult)
            op("dve", "tensor_tensor", out=Hf[:], in0=tmpH[:], in1=Ssb[:], op=ALU.add)
            op("act", "copy", out=Hb[:], in_=Hf[:])
            y = y2.next()
            op("dve", "tensor_tensor", out=y[:].rearrange("p (h q) -> p h q", h=16), in0=pch[:].rearrange("p (h q) -> p h q", h=16),
               in1=bc(eacs, 2, [128, 16, 64]), op=ALU.mult)
            op("pool", "tensor_tensor", out=y[:], in0=y[:], in1=y1[:], op=ALU.add)
            return y

        def zero_state():
            op("pool", "memset", Hf[:], 0.0)
            op("pool", "memset", Hb[:], 0.0)

        def fin_fwd(st, y):
            ti = st["ti"]
            dma("act", YF[ti * 128:(ti + 1) * 128, :], y[:])

        def fin_bwd(st, y):
            ti, xs = st["ti"], st["xs"]
            r0 = ti * 128
            yf = yf_.next()
            dma("sp", yf[:], YF[r0:r0 + 128, :])
            zs = zs_.next()
            dma("sp", zs[:], ZT[r0:r0 + 128, :])
            op("pool", "tensor_tensor", out=y[:], in0=y[:], in1=yf[:], op=ALU.add)
            op("dve", "tensor_tensor", out=yf[:].rearrange("p (h q) -> p h q", h=16), in0=xs[:].rearrange("p (h q) -> p h q", h=16),
               in1=bc(dsk[:], 2, [128, 16, 64]), op=ALU.mult)
            op("pool", "tensor_tensor", out=y[:], in0=y[:], in1=yf[:], op=ALU.add)
            op("dve", "tensor_tensor", out=y[:], in0=y[:], in1=zs[:], op=ALU.mult)
            op("act", "activation", out=junk[:], in_=y[:], func=AF.Square, accum_out=ss[:])
            rstd = rstd_from_ss((r1, r2), ss[:], D)
            op("dve", "scalar_tensor_tensor", out=ytok[:], in0=y[:], scalar=rstd[:, 0:1], in1=normg[:], op0=ALU.mult, op1=ALU.mult)
            yT = yst.next()
            for c in range(8):
                op("pe", "transpose", ptb[:, c, :], ytok[:, c * 128:(c + 1) * 128], ident_b[:])
            op("act", "copy", out=yT[:], in_=ptb[:])
            dma("act", YT[0:8, :, r0:r0 + 128].rearrange("c p t -> p c t"), yT[:])

        def run(order, d, fin):
            zero_state()
            n = len(order)
            s1 = {}
            s2 = {}
            s1[0] = partA1(order[0], d)
            if n > 1:
                s1[1] = partA1(order[1], d)
            s2[0] = partA2(s1.pop(0))
            for k in range(n):
                st = s2.pop(k)
                y = partB(st)
                fin(st, y)
                if k + 1 < n:
                    s2[k + 1] = partA2(s1.pop(k + 1))
                if k + 2 < n:
                    s1[k + 2] = partA1(order[k + 2], d)

        run(list(range(NT)), 0, fin_fwd)
        S.barrier()
        run(list(range(NCT - 1, -1, -1)) + list(range(NT - 1, NCT - 1, -1)), 1, fin_bwd)
        S.barrier()
        sc.close()

    def phase_attn(l):
        sc = Scope(nc, "t")
        KTs = sc.sb("KTs", [128, 2, T], BF16)
        Vs = sc.sb("Vs", [128, NT, 256], BF16)
        for g in range(2):
            dma("sp", KTs[:, g, :], KT[g][:, 0:T])
        dma("sp", Vs[:], VT[0:T, :].rearrange("(n p) c -> p n c", p=128))
        q_ = Rot([sc.sb("q%d" % i, [128, 4, 128], BF16) for i in range(3)])
        ps_s = Rot([sc.ps("pss%d" % i, [128, 512], F32) for i in range(3)])
        pT_ = Rot([sc.sb("pT%d" % i, [128, 512], BF16) for i in range(4)])
        po_ = Rot([sc.ps("po%d" % i, [128, 512], F32) for i in range(2)])
        pm_ = Rot([sc.ps("pm%d" % i, [128, 512], F32) for i in range(2)])
        ac_ = Rot([sc.sb("ac%d" % i, [128, 512], F32) for i in range(2)])
        rs_ = Rot([sc.sb("rs%d" % i, [128, 512], F32) for i in range(2)])
        o_ = Rot([sc.sb("o%d" % i, [128, 4, 128], BF16) for i in range(2)])
        if l == 1:
            sk = load_row(sc, RV_SINK, 8, "sk")
            op("act", "activation", out=sk[:], in_=sk[:], func=AF.Exp)
        groups = []
        for ti in range(NT):
            is_ctx = ti < NCT
            if l == 1 and is_ctx:
                continue
            if l == 0:
                keys = [(k, None) for k in (range(NCT) if is_ctx else range(NT))]
            else:
                keys = [(k, None) for k in range(NCT)]
                if ti - 1 >= NCT:
                    keys.append((ti - 1, MK_WP))
                keys.append((ti, None))
                if ti + 1 < NT:
                    keys.append((ti + 1, MK_WN))
            for g in range(2):
                groups.append((ti, g, keys))
        items = []
        for gi, (ti, g, keys) in enumerate(groups):
            for i, (kt, mk) in enumerate(keys):
                items.append((gi, i, kt, mk, i == 0, i == len(keys) - 1))
        qbuf = {}

        def load_q(gi):
            if gi < len(groups) and gi not in qbuf:
                ti, g, _ = groups[gi]
                q = q_.next()
                dma("sp", q[:], QT[g * 4:(g + 1) * 4, :, ti * 128:(ti + 1) * 128].rearrange("h d t -> d h t"))
                qbuf[gi] = q

        def issue_S(n):
            gi, i, kt, mk, first, last = items[n]
            load_q(gi)
            if first:
                load_q(gi + 1)
            g = groups[gi][1]
            p = ps_s.next()
            op("pe", "matmul", p[:], KTs[:, g, kt * 128:(kt + 1) * 128], qbuf[gi][:].rearrange("d h t -> d (h t)"), start=True, stop=True)
            return p

        LOOK = 2
        pend = [issue_S(n) for n in range(min(LOOK, len(items)))]
        acc = {}
        for n, (gi, i, kt, mk, first, last) in enumerate(items):
            if n + LOOK < len(items):
                pend.append(issue_S(n + LOOK))
            p = pend.pop(0)
            ti, g, keys = groups[gi]
            if first:
                acc[gi] = (po_.next(), pm_.next(), ac_.next())
            po, pm, ac = acc[gi]
            pT = pT_.next()
            op("act", "activation", out=pT[:], in_=p[:], func=AF.Exp, scale=ATT_SCALE)
            if mk is not None:
                op("pool", "tensor_tensor", out=pT[:].rearrange("k (h t) -> k h t", h=4), in0=pT[:].rearrange("k (h t) -> k h t", h=4),
                   in1=bc(masks[:, mk, :], 1, [128, 4, 128]), op=ALU.mult)
            op("pe", "matmul", po[:], Vs[:, kt, g * 128:(g + 1) * 128], pT[:], start=first, stop=last)
            nk = len(keys)
            use_dve = (nk >= 4) and (i % 2 == 1)
            if use_dve:
                if i == 1:
                    op("dve", "tensor_copy", out=ac[:], in_=pT[:])
                else:
                    op("dve", "tensor_tensor", out=ac[:], in0=ac[:], in1=pT[:], op=ALU.add)
            else:
                op("pe", "matmul", pm[:], ones_b[:], pT[:], start=first, stop=(last and nk < 4))
            if last:
                if nk >= 4:
                    op("pe", "matmul", pm[:], ones_f[:], ac[:], start=False, stop=True)
                rs = rs_.next()
                if l == 1:
                    op("dve", "tensor_tensor", out=rs[:].rearrange("d (h t) -> d h t", h=4), in0=pm[:].rearrange("d (h t) -> d h t", h=4),
                       in1=bc(sk[:, g * 4:(g + 1) * 4], 2, [128, 4, 128]), op=ALU.add)
                    op("dve", "reciprocal", out=rs[:], in_=rs[:])
                else:
                    op("dve", "reciprocal", out=rs[:], in_=pm[:])
                o = o_.next()
                op("dve", "tensor_tensor", out=o[:].rearrange("d h t -> d (h t)"), in0=po[:], in1=rs[:], op=ALU.mult)
                dma("act", YT[8 + g * 4:8 + (g + 1) * 4, :, ti * 128:(ti + 1) * 128].rearrange("h d t -> d h t"), o[:])
                del acc[gi]
                qbuf.pop(gi, None)
        S.barrier()
        sc.close()

    def phase_select():
        sc = Scope(nc, "q")
        fl = sc.sb("fl", [128, 8], F32)
        dma("sp", fl[:], rflag.to_broadcast([128, 8]))
        zt = sc.sb("zt", [128, 256], BF16)
        cb_ = Rot([sc.sb("cb%d" % i, [128, LQ + 256], BF16) for i in range(3)])
        ab_ = Rot([sc.sb("ab%d" % i, [128, LQ + 256], BF16) for i in range(2)])
        cf_ = Rot([sc.sb("cf%d" % i, [128, D], F32) for i in range(3)])
        af_ = Rot([sc.sb("af%d" % i, [128, D], F32) for i in range(2)])
        cnt = [0]

        def sel(dst, cands, n, f32=False):
            acc = (af_ if f32 else ab_).next()
            e = "dve"
            cnt[0] += 1
            for j, c in enumerate(cands):
                t = (cf_ if f32 else cb_).next()
                dma("sp", t[:, 0:n], c)
                if j == 0:
                    op(e, "tensor_scalar", out=acc[:, 0:n], in0=t[:, 0:n], scalar1=fl[:, 0:1], scalar2=None, op0=ALU.mult)
                else:
                    op(e, "scalar_tensor_tensor", out=acc[:, 0:n], in0=t[:, 0:n], scalar=fl[:, j:j + 1], in1=acc[:, 0:n], op0=ALU.mult, op1=ALU.add)
            dma("act", dst, acc[:, 0:n])

        for h in range(8):
            sel(QTq[h], [QT[h][:, CTXN + j * LQ:CTXN + (j + 1) * LQ] for j in range(4)], LQ)
            sel(YTL[h], [YT[h][:, CTXN + j * LQ:CTXN + (j + 1) * LQ] for j in range(4)], LQ)
        for g in range(2):
            sel(KTq[g], [KT[g][:, CTXN + j * LQ - 128:CTXN + (j + 1) * LQ + 128] for j in range(4)], LQ + 256)
        for tl in range(NLQ + 2):
            sel(VTq[tl * 128:(tl + 1) * 128, :], [VT[CTXN + j * LQ - 128 + tl * 128:CTXN + j * LQ + tl * 128, :] for j in range(4)], 256)
        for tl in range(NLQ):
            sel(Xq[tl * 128:(tl + 1) * 128, :], [XRES[CTXN + j * LQ + tl * 128:CTXN + j * LQ + (tl + 1) * 128, :] for j in range(4)], D, f32=True)
        S.barrier()
        sc.close()

    def phase_attn_local():
        sc = Scope(nc, "u")
        fl = sc.sb("fl", [128, 8], F32)
        dma("sp", fl[:], rflag.to_broadcast([128, 8]))
        NE = NLQ + 2
        Kc = sc.sb("Kc", [128, 2, CTXN], BF16)
        Vc = sc.sb("Vc", [128, NCT, 256], BF16)
        Kq = sc.sb("Kq", [128, 2, NE * 128], BF16)
        Vq = sc.sb("Vq", [128, NE, 256], BF16)
        for g in range(2):
            dma("sp", Kc[:, g, :], KT[g][:, 0:CTXN])
            dma("sp", Kq[:, g, :], KTq[g])
        dma("sp", Vc[:], VT[0:CTXN, :].rearrange("(n p) c -> p n c", p=128))
        dma("sp", Vq[:], VTq.rearrange("(n p) c -> p n c", p=128))
        mfirst = sc.sb("mfirst", [128, 128], F32)
        mlast = sc.sb("mlast", [128, 128], F32)
        op("dve", "tensor_scalar", out=mfirst[:], in0=masks[:, MK_WP, :], scalar1=fl[:, 4:5], scalar2=None, op0=ALU.mult)
        op("dve", "tensor_scalar", out=mlast[:], in0=masks[:, MK_WN, :], scalar1=fl[:, 5:6], scalar2=None, op0=ALU.mult)
        q_ = Rot([sc.sb("q%d" % i, [128, 4, 128], BF16) for i in range(3)])
        ps_s = Rot([sc.ps("pss%d" % i, [128, 512], F32) for i in range(3)])
        pT_ = Rot([sc.sb("pT%d" % i, [128, 512], BF16) for i in range(4)])
        po_ = Rot([sc.ps("po%d" % i, [128, 512], F32) for i in range(2)])
        pm_ = Rot([sc.ps("pm%d" % i, [128, 512], F32) for i in range(2)])
        rs_ = Rot([sc.sb("rs%d" % i, [128, 512], F32) for i in range(2)])
        o_ = Rot([sc.sb("o%d" % i, [128, 4, 128], BF16) for i in range(2)])
        sk = load_row(sc, RV_SINK, 8, "sk")
        op("act", "activation", out=sk[:], in_=sk[:], func=AF.Exp)
        groups = []
        for j in range(NLQ):
            keys = [("c", k, None) for k in range(NCT)]
            keys.append(("q", j, mfirst[:] if j == 0 else masks[:, MK_WP, :]))
            keys.append(("q", j + 1, None))
            keys.append(("q", j + 2, mlast[:] if j == NLQ - 1 else masks[:, MK_WN, :]))
            for g in range(2):
                groups.append((j, g, keys))
        items = []
        for gi, (j, g, keys) in enumerate(groups):
            for i, (src, kt, mk) in enumerate(keys):
                items.append((gi, i, src, kt, mk, i == 0, i == len(keys) - 1))
        qbuf = {}

        def load_q(gi):
            if gi < len(groups) and gi not in qbuf:
                j, g, _ = groups[gi]
                q = q_.next()
                dma("sp", q[:], QTq[g * 4:(g + 1) * 4, :, j * 128:(j + 1) * 128].rearrange("h d t -> d h t"))
                qbuf[gi] = q

        def issue_S(n):
            gi, i, src, kt, mk, first, last = items[n]
            load_q(gi)
            if first:
                load_q(gi + 1)
            g = groups[gi][1]
            kk = Kc if src == "c" else Kq
            p = ps_s.next()
            op("pe", "matmul", p[:], kk[:, g, kt * 128:(kt + 1) * 128], qbuf[gi][:].rearrange("d h t -> d (h t)"), start=True, stop=True)
            return p

        LOOK = 2
        pend = [issue_S(n) for n in range(min(LOOK, len(items)))]
        acc = {}
        for n, (gi, i, src, kt, mk, first, last) in enumerate(items):
            if n + LOOK < len(items):
                pend.append(issue_S(n + LOOK))
            p = pend.pop(0)
            j, g, keys = groups[gi]
            if first:
                acc[gi] = (po_.next(), pm_.next())
            po, pm = acc[gi]
            pT = pT_.next()
            op("act", "activation", out=pT[:], in_=p[:], func=AF.Exp, scale=ATT_SCALE)
            if mk is not None:
                op("pool", "tensor_tensor", out=pT[:].rearrange("k (h t) -> k h t", h=4), in0=pT[:].rearrange("k (h t) -> k h t", h=4),
                   in1=bc(mk, 1, [128, 4, 128]), op=ALU.mult)
            vv = Vc if src == "c" else Vq
            op("pe", "matmul", po[:], vv[:, kt, g * 128:(g + 1) * 128], pT[:], start=first, stop=last)
            op("pe", "matmul", pm[:], ones_b[:], pT[:], start=first, stop=last)
            if last:
                rs = rs_.next()
                op("dve", "tensor_tensor", out=rs[:].rearrange("d (h t) -> d h t", h=4), in0=pm[:].rearrange("d (h t) -> d h t", h=4),
                   in1=bc(sk[:, g * 4:(g + 1) * 4], 2, [128, 4, 128]), op=ALU.add)
                op("dve", "reciprocal", out=rs[:], in_=rs[:])
                o = o_.next()
                op("dve", "tensor_tensor", out=o[:].rearrange("d h t -> d (h t)"), in0=po[:], in1=rs[:], op=ALU.mult)
                dma("act", YTL[8 + g * 4:8 + (g + 1) * 4, :, j * 128:(j + 1) * 128].rearrange("h d t -> d h t"), o[:])
                del acc[gi]
                qbuf.pop(gi, None)
        S.barrier()
        sc.close()

    def phase_oproj(l):
        sc = Scope(nc, "o")
        Wo = sc.sb("Wo", [128, 16, D], BF16)
        for c in range(16):
            dma("pool", Wo[:, c, :], w_out[l][c * 128:(c + 1) * 128, :])
        g1 = load_mod(sc, l, 2, "g1")
        gsc2 = load_mod(sc, l, 3, "gsc2")
        sh2 = load_mod(sc, l, 4, "sh2")
        wr = sc.sb("wr", [128, 8, 20], F32)
        dma("sp", wr[:], moe_wr[l].rearrange("(c p) n -> p c n", p=128))
        brow = load_row(sc, RV_BROUTE + l * 20, 20, "brow")
        y_ = Rot([sc.sb("y%d" % i, [128, 16, 128], BF16) for i in range(2)])
        x_ = Rot([sc.sb("x%d" % i, [128, D], F32) for i in range(2)])
        po = sc.ps("po", [128, 1024], F32)
        tmp = sc.sb("tmp", [128, D], F32)
        xn_ = Rot([sc.sb("xn%d" % i, [128, D], F32) for i in range(2)])
        junk = sc.sb("junk", [128, D], BF16)
        ss = sc.sb("ss", [128, 1], F32)
        r1 = sc.sb("r1", [128, 1], F32)
        r2 = sc.sb("r2", [128, 1], F32)
        h32 = sc.sb("h32", [128, D], F32)
        pt32 = sc.ps("pt32", [128, 8, 128], F32)
        hT32 = sc.sb("hT32", [128, 8, 128], F32)
        hTb = Rot([sc.sb("hTb%d" % i, [128, 8, 128], BF16) for i in range(2)])
        plog = sc.ps("plog", [128, 32], F32)
        lg = sc.sb("lg", [128, 20], F32)
        sm = sc.sb("sm", [128, 16], F32)
        oh = sc.sb("oh", [128, 4], F32)
        eg = sc.sb("eg", [128, 4], F32)
        pen = sc.sb("pen", [128, 4], F32)
        elm = sc.sb("elm", [128, 16], F32)
        mk1 = sc.sb("mk1", [128, 16], F32)
        el2 = sc.sb("el2", [128, 16], F32)
        mk2 = sc.sb("mk2", [128, 16], F32)
        local = (l == 1)
        if not local:
            xsrc, ysrc, xdst, hdst = xin, YT, XRES, HTF
            tiles = list(range(NT))
            nctx = NCT
        else:
            xsrc, ysrc, xdst, hdst = Xq, YTL, XQ1, HTFq
            tiles = list(range(NLQ))
            nctx = 0
        t_lo = 0
        NTP = len(tiles)
        LG = sc.sb("LG", [128, NT, 20], F32)
        po2 = [po, sc.ps("po_b", [128, 1024], F32)]
        pend = {}

        def mm(k):
            ti = tiles[k]
            r0 = ti * 128
            y = y_.next()
            dma("sp", y[:], ysrc[:, :, r0:r0 + 128].rearrange("c p t -> p c t"))
            x = x_.next()
            dma("sp", x[:], xsrc[r0:r0 + 128, :])
            p = po2[k % 2]
            for half in range(2):
                for c in range(16):
                    op("pe", "matmul", p[:, half * 512:(half + 1) * 512], y[:, c, :], Wo[:, c, half * 512:(half + 1) * 512], start=(c == 0), stop=(c == 15))
            pend[k] = (p, x)

        mm(0)
        for k, ti in enumerate(tiles):
            if k + 1 < len(tiles):
                mm(k + 1)
            p, x = pend.pop(k)
            s = 1 if ti < nctx else 0
            r0 = ti * 128
            op("dve", "tensor_tensor", out=tmp[:], in0=p[:], in1=g1[s][:], op=ALU.mult)
            xn = xn_.next()
            op("pool", "tensor_tensor", out=xn[:], in0=tmp[:], in1=x[:], op=ALU.add)
            dma("act", xdst[r0:r0 + 128, :], xn[:])
            op("act", "activation", out=junk[:], in_=xn[:], func=AF.Square, accum_out=ss[:])
            rstd = rstd_from_ss((r1, r2), ss[:], D)
            op("dve", "scalar_tensor_tensor", out=tmp[:], in0=xn[:], scalar=rstd[:, 0:1], in1=gsc2[s][:], op0=ALU.mult, op1=ALU.mult)
            op("dve", "tensor_tensor", out=h32[:], in0=tmp[:], in1=sh2[s][:], op=ALU.add)
            for c in range(8):
                op("pe", "transpose", pt32[:, c, :], h32[:, c * 128:(c + 1) * 128], ident_f[:])
            op("act", "copy", out=hT32[:], in_=pt32[:])
            hb = hTb.next()
            op("pool", "tensor_copy", out=hb[:], in_=hT32[:])
            dma("act", hdst[:, :, r0:r0 + 128].rearrange("c p t -> p c t"), hb[:])
            for c in range(8):
                op("pe", "matmul", plog[:, 0:20], hT32[:, c, :], wr[:, c, :], start=(c == 0), stop=(c == 7))
            op("dve", "tensor_tensor", out=LG[:, ti, :], in0=plog[:, 0:20], in1=brow[:], op=ALU.add)
        R = sc.sb("R", [128, 10, NT], F32)
        gmax, gsum, pgrp, m1, m2, dm, ed, w1p, w2p = [R[:, i, 0:NTP] for i in range(9)]
        OH = sc.sb("OH", [128, NT, 4], F32)
        ELM = sc.sb("ELM", [128, NT, 16], F32)
        MK1 = sc.sb("MK1", [128, NT, 16], F32)
        EL2 = sc.sb("EL2", [128, NT, 16], F32)
        MK2 = sc.sb("MK2", [128, NT, 16], F32)
        GLv = LG[:, 0:NTP, 0:4]
        ELv = LG[:, 0:NTP, 4:20]
        oh = OH[:, 0:NTP, :]
        elm, mk1, el2, mk2 = [t[:, 0:NTP, :] for t in (ELM, MK1, EL2, MK2)]
        op("dve", "reduce_max", out=gmax, in_=GLv, axis=AX.X)
        op("dve", "tensor_tensor", out=oh, in0=GLv, in1=bc(gmax, 2, [128, NTP, 4]), op=ALU.is_ge)
        op("dve", "tensor_tensor", out=elm[:, :, 0:4], in0=GLv, in1=bc(gmax, 2, [128, NTP, 4]), op=ALU.subtract)
        op("act", "activation", out=elm[:, :, 0:4], in_=elm[:, :, 0:4], func=AF.Exp)
        op("dve", "reduce_sum", out=gsum, in_=elm[:, :, 0:4], axis=AX.X)
        op("dve", "reciprocal", out=pgrp, in_=gsum)
        op("dve", "tensor_scalar", out=oh, in0=oh, scalar1=1.0, scalar2=BIG, op0=ALU.subtract, op1=ALU.mult)
        op("dve", "tensor_tensor", out=elm.rearrange("p t (g k) -> p t g k", g=4), in0=ELv.rearrange("p t (g k) -> p t g k", g=4),
           in1=bc(oh, 3, [128, NTP, 4, 4]), op=ALU.add)
        op("dve", "reduce_max", out=m1, in_=elm, axis=AX.X)
        op("dve", "tensor_tensor", out=mk1, in0=elm, in1=bc(m1, 2, [128, NTP, 16]), op=ALU.is_ge)
        op("dve", "scalar_tensor_tensor", out=el2, in0=mk1, scalar=-BIG, in1=elm, op0=ALU.mult, op1=ALU.add)
        op("dve", "reduce_max", out=m2, in_=el2, axis=AX.X)
        op("dve", "tensor_tensor", out=mk2, in0=el2, in1=bc(m2, 2, [128, NTP, 16]), op=ALU.is_ge)
        op("dve", "tensor_tensor", out=dm, in0=m2, in1=m1, op=ALU.subtract)
        op("act", "activation", out=ed, in_=dm, func=AF.Exp)
        op("dve", "tensor_scalar", out=w1p, in0=ed, scalar1=1.0, scalar2=None, op0=ALU.add)
        op("dve", "reciprocal", out=w1p, in_=w1p)
        op("dve", "tensor_tensor", out=w1p, in0=w1p, in1=pgrp, op=ALU.mult)
        op("dve", "tensor_tensor", out=w2p, in0=w1p, in1=ed, op=ALU.mult)
        op("dve", "tensor_tensor", out=mk1, in0=mk1, in1=bc(w1p, 2, [128, NTP, 16]), op=ALU.mult)
        op("dve", "tensor_tensor", out=mk2, in0=mk2, in1=bc(w2p, 2, [128, NTP, 16]), op=ALU.mult)
        op("dve", "tensor_tensor", out=gates[:, 0:NTP, :], in0=mk1, in1=mk2, op=ALU.add)
        S.barrier()
        sc.close()

    def phase_moe(l):
        sc = Scope(nc, "e")
        last = (l == 1)
        W2b = sc.sb("W2b", [128, 32, D], BF16)
        for e in range(16):
            dma("pool", W2b[:, 2 * e:2 * e + 2, :], moe_w2[l, e].rearrange("(fc p) d -> p fc d", p=128))
        g2 = load_mod(sc, l, 5, "g2")
        if last:
            gfin = load_row(sc, RV_GFIN, D, "gfin")
        hT_ = Rot([sc.sb("hT%d" % i, [128, 8, 512], BF16) for i in range(1)])
        gB_ = Rot([sc.sb("gateB%d" % i, [128, 4, 512], BF16) for i in range(2)])
        hid = sc.sb("hid", [128, 32, 512], BF16)
        w1_ = Rot([sc.sb("w1%d" % i, [128, 8, 256], BF16) for i in range(2)])
        w3_ = Rot([sc.sb("w3%d" % i, [128, 8, 256], BF16) for i in range(2)])
        pa_ = Rot([sc.ps("pa%d" % i, [128, 512], F32) for i in range(2)])
        pu_ = Rot([sc.ps("pu%d" % i, [128, 512], F32) for i in range(2)])
        po_ = Rot([sc.ps("po%d" % i, [128, 512], F32) for i in range(2)])
        sg_ = Rot([sc.sb("sg%d" % i, [128, 512], BF16) for i in range(2)])
        t1_ = Rot([sc.sb("t1%d" % i, [128, 512], BF16) for i in range(2)])
        x_ = Rot([sc.sb("x%d" % i, [128, D], F32) for i in range(1)])
        xn_ = Rot([sc.sb("xn%d" % i, [128, D], F32) for i in range(1)])
        tmp = sc.sb("tmp", [128, 512], F32)
        junk = sc.sb("junk", [128, D], BF16)
        ss = sc.sb("ss", [128, 1], F32)
        r1 = sc.sb("r1", [128, 1], F32)
        r2 = sc.sb("r2", [128, 1], F32)
        if not last:
            blks, hsrc, xsrc2 = blocks, HTF, XRES
        else:
            blks, hsrc, xsrc2 = [(t0_, min(4, NLQ - t0_), False) for t0_ in range(0, NLQ, 4)], HTFq, XQ1
        for (tile0, n, is_ctx) in blks:
            NB = n * 128
            s = 1 if is_ctx else 0
            tok0 = tile0 * 128
            hT = hT_.next()
            dma("sp", hT[:, :, 0:NB], hsrc[:, :, tok0:tok0 + NB].rearrange("c p t -> p c t"))
            for e in range(16):
                if e % 4 == 0:
                    gateB = gB_.next()
                    for j in range(n):
                        p = po_.next()
                        for k in range(4):
                            op("pe", "matmul", p[:, k * 128:(k + 1) * 128], gates[:, tile0 + j, e + k:e + k + 1].to_broadcast([128, 128]),
                               ident_f[:], start=True, stop=True)
                        op("act", "copy", out=gateB[:, :, j * 128:(j + 1) * 128], in_=p[:].rearrange("p (k t) -> p k t", k=4))
                w1 = w1_.next()
                w3 = w3_.next()
                dma("sp", w1[:].rearrange("p c f -> p (c f)"), W1B[l, e])
                dma("sp", w3[:].rearrange("p c f -> p (c f)"), W3B[l, e])
                for fc in range(2):
                    pa = pa_.next()
                    pu = pu_.next()
                    for c in range(8):
                        op("pe", "matmul", pa[:, 0:NB], w1[:, c, fc * 128:(fc + 1) * 128], hT[:, c, 0:NB], start=(c == 0), stop=(c == 7))
                    for c in range(8):
                        op("pe", "matmul", pu[:, 0:NB], w3[:, c, fc * 128:(fc + 1) * 128], hT[:, c, 0:NB], start=(c == 0), stop=(c == 7))
                    sg = sg_.next()
                    op("act", "activation", out=sg[:, 0:NB], in_=pa[:, 0:NB], func=AF.Silu)
                    t1 = t1_.next()
                    op("dve", "tensor_tensor", out=t1[:, 0:NB], in0=pu[:, 0:NB], in1=gateB[:, e % 4, 0:NB], op=ALU.mult)
                    op("pool", "tensor_tensor", out=hid[:, 2 * e + fc, 0:NB], in0=sg[:, 0:NB], in1=t1[:, 0:NB], op=ALU.mult)
            for j in range(n):
                ti = tile0 + j
                r0 = ti * 128
                x = x_.next()
                dma("sp", x[:], xsrc2[r0:r0 + 128, :])
                xn = xn_.next()
                for half in range(2):
                    po = po_.next()
                    for k in range(32):
                        op("pe", "matmul", po[:], hid[:, k, j * 128:(j + 1) * 128], W2b[:, k, half * 512:(half + 1) * 512], start=(k == 0), stop=(k == 31))
                    op("dve", "tensor_tensor", out=tmp[:], in0=po[:], in1=g2[s][:, half * 512:(half + 1) * 512], op=ALU.mult)
                    op("pool", "tensor_tensor", out=xn[:, half * 512:(half + 1) * 512], in0=tmp[:], in1=x[:, half * 512:(half + 1) * 512], op=ALU.add)
                if not last:
                    dma("act", XRES[r0:r0 + 128, :], xn[:])
                else:
                    op("act", "activation", out=junk[:], in_=xn[:], func=AF.Square, accum_out=ss[:])
                    rstd = rstd_from_ss((r1, r2), ss[:], D)
                    op("dve", "scalar_tensor_tensor", out=x[:], in0=xn[:], scalar=rstd[:, 0:1], in1=gfin[:], op0=ALU.mult, op1=ALU.mult)
                    dma("act", out[ti * 128:(ti + 1) * 128, :], x[:])
        S.barrier()
        sc.close()

    def phase_lru():
        sc = Scope(nc, "r")
        xc = sc.sb("xc", [128, T], F32)
        xcb = sc.sb("xcb", [128, T], BF16)
        hf = sc.sb("hf", [128, T], F32)
        hb = sc.sb("hb", [128, T], F32)
        raw = Rot([sc.sb("raw%d" % i, [128, 1028], F32) for i in range(1)])
        wa = sc.sb("wa", [128, 2, 2, 128], BF16)
        SP = 1024
        prs = [sc.ps("pr%d" % i, [128, SP], F32) for i in range(2)]
        pis = [sc.ps("pi%d" % i, [128, SP], F32) for i in range(2)]
        rbs = [sc.sb("rb%d" % i, [128, SP], F32) for i in range(2)]
        ibs = [sc.sb("ib%d" % i, [128, SP], F32) for i in range(2)]
        sbs = [sc.sb("sb2%d" % i, [128, SP], F32) for i in range(2)]
        gt = Rot([sc.sb("gt%d" % i, [128, 1024], BF16) for i in range(2)])
        yo = Rot([sc.sb("yo%d" % i, [128, 1024], BF16) for i in range(2)])
        lb = sc.sb("lb", [128, 16], F32)
        lb2 = sc.sb("lb2", [128, 16], F32)
        op("act", "activation", out=lb[:], in_=colT[:, CV_LAM:CV_LAM + 16], func=AF.Exp, scale=-1.0)
        op("act", "activation", out=lb[:], in_=lb[:], func=AF.Ln, bias=1.0)
        op("dve", "tensor_scalar", out=lb2[:], in0=lb[:], scalar1=-16.0, scalar2=None, op0=ALU.mult)
        op("dve", "tensor_scalar", out=lb[:], in0=lb[:], scalar1=-8.0, scalar2=None, op0=ALU.mult)
        cspans = [(c0, min(SP, CTXN - c0)) for c0 in range(0, CTXN, SP)]
        lspans = [(CTXN + c0, min(SP, L - c0)) for c0 in range(0, L, SP)]
        orders = [cspans + lspans, cspans[::-1] + lspans[::-1]]
        for cc in range(8):
            for (c0, n, tok0) in conv_spans(1024):
                r = raw.next()
                dma("sp", r[:, 0:n + 4], XBC[cc * 128:(cc + 1) * 128, c0 - 2:c0 + n + 2])
                op("dve", "tensor_scalar", out=xc[:, tok0:tok0 + n], in0=r[:, 0:n], scalar1=cvc(CV_LRU_CW + cc), scalar2=cvc(CV_LRU_CB + cc), op0=ALU.mult, op1=ALU.add)
                for k in range(1, 5):
                    op("dve", "scalar_tensor_tensor", out=xc[:, tok0:tok0 + n], in0=r[:, k:k + n], scalar=cvc(CV_LRU_CW + k * 8 + cc),
                       in1=xc[:, tok0:tok0 + n], op0=ALU.mult, op1=ALU.add)
            op("pool", "tensor_copy", out=xcb[:], in_=xc[:])
            for ax in range(2):
                for d in range(2):
                    dma("pool", wa[:, ax, d, :], lru_bd[ax, d, cc])
            for k in range(len(orders[0])):
                sn = [orders[0][k], orders[1][k]]
                for d in range(2):
                    s0, n = sn[d]
                    for q0 in range(0, n, 512):
                        nq = min(512, n - q0)
                        op("pe", "matmul", prs[d][:, q0:q0 + nq], wa[:, 0, d, :], xcb[:, s0 + q0:s0 + q0 + nq], start=True, stop=True)
                        op("pe", "matmul", pis[d][:, q0:q0 + nq], wa[:, 1, d, :], xcb[:, s0 + q0:s0 + q0 + nq], start=True, stop=True)
                for d in range(2):
                    s0, n = sn[d]
                    op("act", "activation", out=rbs[d][:, 0:n], in_=prs[d][:, 0:n], func=AF.Sigmoid, bias=cvc(CV_BA + d * 8 + cc))
                    op("act", "activation", out=ibs[d][:, 0:n], in_=pis[d][:, 0:n], func=AF.Sigmoid, bias=cvc(CV_BX + d * 8 + cc))
                for d in range(2):
                    s0, n = sn[d]
                    op("act", "activation", out=sbs[d][:, 0:n], in_=rbs[d][:, 0:n], func=AF.Exp, scale=lb2[:, d * 8 + cc:d * 8 + cc + 1])
                    op("act", "activation", out=rbs[d][:, 0:n], in_=rbs[d][:, 0:n], func=AF.Exp, scale=lb[:, d * 8 + cc:d * 8 + cc + 1])
                for d in range(2):
                    s0, n = sn[d]
                    op("act", "activation", out=sbs[d][:, 0:n], in_=sbs[d][:, 0:n], func=AF.Sqrt, scale=-1.0, bias=1.0)
                for d in range(2):
                    s0, n = sn[d]
                    rb, ib, sb2 = rbs[d], ibs[d], sbs[d]
                    op("dve", "tensor_tensor", out=ib[:, 0:n], in0=ib[:, 0:n], in1=sb2[:, 0:n], op=ALU.mult)
                    op("dve", "tensor_tensor", out=ib[:, 0:n], in0=ib[:, 0:n], in1=xc[:, s0:s0 + n], op=ALU.mult)
                    if d == 0:
                        init = 0.0 if s0 == 0 else hf[:, s0 - 1:s0]
                        op("dve", "tensor_tensor_scan", out=hf[:, s0:s0 + n], data0=rb[:, 0:n], data1=ib[:, 0:n], initial=init, op0=ALU.mult, op1=ALU.add)
                    else:
                        if s0 + n == CTXN:
                            init = 0.0
                        elif s0 + n == T:
                            init = hb[:, 0:1]
                        else:
                            init = hb[:, s0 + n:s0 + n + 1]
                        op("dve", "tensor_tensor_scan", out=rev(hb[:, s0:s0 + n], n), data0=rev(rb[:, 0:n], n), data1=rev(ib[:, 0:n], n),
                           initial=init, op0=ALU.mult, op1=ALU.add)
            for (s0, n) in [(CTXN + c0, min(1024, L - c0)) for c0 in range(0, L, 1024)]:
                g = gt.next()
                dma("sp", g[:, 0:n], GATE[cc, :, s0:s0 + n])
                op("pool", "tensor_tensor", out=hf[:, s0:s0 + n], in0=hf[:, s0:s0 + n], in1=hb[:, s0:s0 + n], op=ALU.add)
                y = yo.next()
                op("dve", "tensor_tensor", out=y[:, 0:n], in0=hf[:, s0:s0 + n], in1=g[:, 0:n], op=ALU.mult)
                dma("act", YT[cc, :, s0:s0 + n], y[:, 0:n])
        S.barrier()
        sc.close()

    phase_consts()
    phase_wconv()
    phase_mod(0)
    G0 = Scope(nc, "l0")
    GL = {"dtall": G0.sb("dtall", [128, NT, 32], F32), "dAall": G0.sb("dAall", [128, NT, 32], F32)}
    phase_inproj(0, GL)
    phase_conv0()
    phase_ssd(GL)
    G0.close()
    phase_attn(0)
    phase_oproj(0)
    phase_moe(0)
    phase_mod(1)
    phase_inproj(1, None)
    phase_lru()
    phase_select()
    phase_attn_local()
    phase_oproj(1)
    phase_moe(1)
    S.barrier()
    G.close()
    S.close()
    return nc


def _host_consts(L):
    t = np.arange(128)
    tt, ii = np.meshgrid(t, t, indexing="ij")
    m = np.zeros((8, 128, 128), np.float32)
    m[0] = tt > ii
    m[1] = tt <= ii
    m[2] = ii >= tt
    m[3] = tt < ii
    m[4] = tt >= ii
    m[5] = tt >= ii
    m[6] = tt >= ii
    m[7] = tt <= ii
    rows = L // GRID_W
    row = np.repeat(np.arange(rows), GRID_W).astype(np.float32)
    col = np.tile(np.arange(GRID_W), rows).astype(np.float32)
    n_freq = 32
    inv = (10000.0 ** (-np.arange(n_freq, dtype=np.float32) / n_freq)).astype(np.float32)
    ang = np.concatenate([row[:, None] * inv, col[:, None] * inv], axis=-1).astype(np.float32)
    cos = np.cos(ang).astype(np.float32).T
    sin = np.sin(ang).astype(np.float32).T
    cos2 = np.concatenate([cos, cos], axis=0)
    sin2 = np.concatenate([-sin, sin], axis=0)
    return m, np.ascontiguousarray(cos2), np.ascontiguousarray(sin2)


def _swap_heads(w, nheads):
    w = w.reshape(w.shape[0], nheads, 2, 64)
    return np.ascontiguousarray(w[:, :, ::-1, :]).reshape(w.shape[0], nheads * 128)


def make_inputs(inp, b, NL, NCT):
    f = lambda a: np.ascontiguousarray(np.asarray(a, dtype=np.float32))
    L = NL * 128
    m = {}
    m["xin"] = f(np.concatenate([inp["ctx"][b], inp["x"][b]], axis=0))
    c16 = np.zeros((16, 128), np.float32)
    c16[0::2] = f(inp["c"][b]).reshape(8, 128)
    c16[1::2] = f(inp["c_ctx"]).reshape(8, 128)
    m["cv16"] = c16
    m["w_mod"] = f(inp["w_mod"])
    m["b_mod"] = f(inp["b_mod"])
    rv = np.zeros((1, RV_N), np.float32)
    rv[0, RV_GMIX:RV_GMIX + 2048] = f(inp["g_mix"]).reshape(-1)
    rv[0, RV_GFFN:RV_GFFN + 2048] = f(inp["g_ffn"]).reshape(-1)
    rv[0, RV_GFIN:RV_GFIN + 1024] = f(inp["g_final"])
    rv[0, RV_NORMG:RV_NORMG + 1024] = f(inp["ssd_norm_g"][0])
    rv[0, RV_SSD_D:RV_SSD_D + 16] = f(inp["ssd_d"][0])
    rv[0, RV_DTB:RV_DTB + 32] = f(inp["ssd_dt_bias"][0]).reshape(-1)
    rv[0, RV_ALOG:RV_ALOG + 32] = f(inp["ssd_a_log"][0]).reshape(-1)
    for l in range(2):
        rv[0, RV_BROUTE + l * 20:RV_BROUTE + l * 20 + 4] = f(inp["moe_b_grp"][l])
        rv[0, RV_BROUTE + l * 20 + 4:RV_BROUTE + l * 20 + 20] = f(inp["moe_b_rt"][l])
    rv[0, RV_SINK:RV_SINK + 8] = f(inp["swa_sink"][0])
    m["rowv"] = rv
    cvv = np.zeros((CV_N, 128), np.float32)
    cvv[CV_SSD_CW:CV_SSD_CW + 50] = f(inp["ssd_conv_w"][0]).reshape(5, 10, 128).reshape(50, 128)
    cvv[CV_SSD_CB:CV_SSD_CB + 10] = f(inp["ssd_conv_b"][0]).reshape(10, 128)
    qg = f(inp["att_q_g"][0])
    kg = f(inp["att_k_g"][0])
    cvv[CV_QG] = qg
    cvv[CV_KG] = kg
    cvv[CV_QGS] = np.concatenate([qg[64:], qg[:64]])
    cvv[CV_KGS] = np.concatenate([kg[64:], kg[:64]])
    cvv[CV_LRU_CW:CV_LRU_CW + 40] = f(inp["lru_conv_w"][0]).reshape(5, 8, 128).reshape(40, 128)
    cvv[CV_LRU_CB:CV_LRU_CB + 8] = f(inp["lru_conv_b"][0]).reshape(8, 128)
    cvv[CV_BA:CV_BA + 16] = f(inp["lru_b_a"][0]).reshape(16, 128)
    cvv[CV_BX:CV_BX + 16] = f(inp["lru_b_x"][0]).reshape(16, 128)
    cvv[CV_LAM:CV_LAM + 16] = f(inp["lru_lam"][0]).reshape(16, 128)
    m["colv"] = cvv
    cm, cos2, sin2 = _host_consts(L)
    m["cmask"] = cm
    m["cos2"] = cos2
    m["sin2"] = sin2
    ab = f(inp["ab_w_in"][0])
    cd = f(inp["cd_w_in"][0])
    m["ab_w_in"] = ab
    m["cd_w_in"] = cd
    m["ab_w_sw"] = np.concatenate([_swap_heads(ab[:, 2336:3360], 8), _swap_heads(ab[:, 3360:3616], 2)], axis=1)
    m["cd_w_sw"] = np.concatenate([_swap_heads(cd[:, 2048:3072], 8), _swap_heads(cd[:, 3072:3328], 2)], axis=1)
    m["ab_w_out"] = f(inp["ab_w_out"][0])
    m["cd_w_out"] = f(inp["cd_w_out"][0])
    bd = np.zeros((2, 2, 8, 128, 128), np.float32)
    for ax, key in enumerate(("lru_w_a", "lru_w_x")):
        w = f(inp[key][0])
        for d in range(2):
            for cc in range(8):
                bd[ax, d, cc, 0:64, 0:64] = w[d, 2 * cc]
                bd[ax, d, cc, 64:128, 64:128] = w[d, 2 * cc + 1]
    m["lru_bd"] = bd
    m["moe_wr"] = np.ascontiguousarray(np.concatenate([f(inp["moe_w_grp"]), f(inp["moe_w_rt"])], axis=-1))
    m["moe_w1"] = f(inp["moe_w1"])
    m["moe_w3"] = f(inp["moe_w3"])
    m["moe_w2"] = f(inp["moe_w2"])
    return m


def rank_flags(r):
    fl = np.zeros((1, 8), np.float32)
    fl[0, r] = 1.0
    fl[0, 4] = 1.0 if r > 0 else 0.0
    fl[0, 5] = 1.0 if r < 3 else 0.0
    return fl


_NC_CACHE = {}


def kernel(**inputs):
    B, L, _ = inputs["x"].shape
    NL = L // 128
    NCT = inputs["ctx"].shape[1] // 128
    key = (NL, NCT)
    if key not in _NC_CACHE:
        _NC_CACHE[key] = build(NL, NCT)
    nc = _NC_CACHE[key]
    maps = [make_inputs(inputs, b, NL, NCT) for b in range(B)]
    in_maps = []
    for i in range(8):
        m = dict(maps[(i // 4) % B])
        m["rflag"] = rank_flags(i % 4)
        in_maps.append(m)
    res = run_bass_kernel_spmd(nc, in_maps, core_ids=list(range(8)))
    LQ = L // 4
    full = np.zeros((B, L, D), np.float32)
    for i in range(8):
        b, r = (i // 4) % B, i % 4
        if i // 4 < B:
            full[b, r * LQ:(r + 1) * LQ] = np.asarray(res.results[i]["out"], dtype=np.float32)
    return full
```
